# Optimizing a Trainium2 kernel written in Bass

```python
import math
import jax, jax.numpy as jnp
from jax import lax
import numpy as np

D_MODEL = 1024
BATCH = 16
SEQ = 4096
DEPTH = 2

CTX_LEN = 256
GRID_W = 64
N_BRANCH = 4
MIX_W = 256
GLA_HEADS = 4
GLA_DK = 32
GLA_DV = 64
GLA_RANK = 16
GLA_TAU = 16.0
HY_WIDTH = 256
HY_ORDER = 2
HY_BANDS = 16
HY_FEAT = 1 + 2 * HY_BANDS
HY_FFN = 64
HY_SHIFT = 0.05
HG_HEADS = 4
HG_DK = 64
HG_DV = 64
DA_HEADS = 4
DA_DK = 32
DA_DV = 64
ROPE_BASE = 10000.0
Q_BLOCK = 128
CHUNK = 64
N_EXPERTS = 16
EXPERT_FF = 1024
CAPACITY_FACTOR = 2
ADA_CHUNKS = 6
EPS = 1e-6
F_TINY = 1e-20

IN_NAMES = ('gla_q', 'gla_k', 'gla_v', 'gla_af', 'gla_ab', 'gla_g', 'hy',
            'hg_q', 'hg_ff', 'hg_fb', 'hg_i', 'hg_g', 'da_q', 'da_k', 'da_v', 'merge')
IN_WIDTHS = (GLA_HEADS * GLA_DK, GLA_HEADS * GLA_DK, GLA_HEADS * GLA_DV, GLA_RANK, GLA_RANK, GLA_HEADS * GLA_DV,
             (1 + HY_ORDER) * HY_WIDTH,
             HG_HEADS * HG_DK, HG_HEADS * HG_DK, HG_HEADS * HG_DK, HG_HEADS * HG_DV, HG_HEADS * HG_DV,
             DA_HEADS * 2 * DA_DK, DA_HEADS * 2 * DA_DK, DA_HEADS * DA_DV,
             N_BRANCH * D_MODEL)
N_IN = sum(IN_WIDTHS)

kernel_name = 'hybrid_flow_backbone'


def rms_norm(x, g):
    xf = x.astype(jnp.float32)
    y = xf * lax.rsqrt(jnp.mean(xf * xf, axis=-1, keepdims=True) + EPS)
    return (y * g.astype(jnp.float32)).astype(x.dtype)


def modulate(x, g, shift, scale):
    return rms_norm(x, g) * (1 + scale) + shift


def heads(t, n):
    B, L, W = t.shape
    return t.reshape(B, L, n, W // n).transpose(0, 2, 1, 3)


def merge_heads(t):
    B, H, L, d = t.shape
    return t.transpose(0, 2, 1, 3).reshape(B, L, H * d)


def short_conv(u, w):
    up = jnp.pad(u, ((0, 0), (1, 1), (0, 0)))
    return up[:, :-2] * w[0] + up[:, 1:-1] * w[1] + up[:, 2:] * w[2]


def chunk_recurrence(q, k, v, log_g, s0):
    B, H, L, dk = q.shape
    dv = v.shape[-1]
    n = L // CHUNK

    def to_chunks(t):
        return t.astype(jnp.float32).reshape(B, H, n, CHUNK, t.shape[-1]).transpose(2, 0, 1, 3, 4)

    mask = jnp.tril(jnp.ones((CHUNK, CHUNK), bool))[:, :, None]

    def step(S, xs):
        qc, kc, vc, gc = xs
        b = jnp.cumsum(gc, axis=-2)
        o_inter = jnp.einsum('bhtd,bhde->bhte', qc * jnp.exp(b), S)
        diff = b[:, :, :, None, :] - b[:, :, None, :, :]
        decay = jnp.where(mask, jnp.exp(jnp.where(mask, diff, 0.0)), 0.0)
        scores = jnp.einsum('bhtd,bhsd,bhtsd->bhts', qc, kc, decay)
        o_intra = jnp.einsum('bhts,bhse->bhte', scores, vc)
        b_end = b[:, :, -1:, :]
        S_new = jnp.exp(b_end[:, :, 0, :, None]) * S + jnp.einsum('bhsd,bhse->bhde', kc * jnp.exp(b_end - b), vc)
        return S_new, o_inter + o_intra

    S_fin, o = lax.scan(step, s0, (to_chunks(q), to_chunks(k), to_chunks(v), to_chunks(log_g)))
    o = o.transpose(1, 2, 0, 3, 4).reshape(B, H, L, dv)
    return o.astype(v.dtype), S_fin


def bidir_recurrence(ctx_in, lat_in):
    qc, kfc, kbc, vc, gfc, gbc = ctx_in
    ql, kfl, kbl, vl, gfl, gbl = lat_in
    B, H, _, dk = qc.shape
    s0 = jnp.zeros((B, H, dk, vc.shape[-1]), jnp.float32)
    flip = lambda t: jnp.flip(t, axis=2)
    oc_f, sc_f = chunk_recurrence(qc, kfc, vc, gfc, s0)
    ol_f, _ = chunk_recurrence(ql, kfl, vl, gfl, sc_f)
    oc_b, sc_b = chunk_recurrence(flip(qc), flip(kbc), flip(vc), flip(gbc), s0)
    ol_b, _ = chunk_recurrence(flip(ql), flip(kbl), flip(vl), flip(gbl), sc_b)
    return oc_f + flip(oc_b), ol_f + flip(ol_b)


def gla_inputs(parts, wa2, ba):
    q = heads(parts['gla_q'], GLA_HEADS) * (GLA_DK ** -0.5)
    k = heads(parts['gla_k'], GLA_HEADS)
    v = heads(parts['gla_v'], GLA_HEADS)
    gf = heads(jax.nn.log_sigmoid((parts['gla_af'] @ wa2[0] + ba[0]).astype(jnp.float32)) / GLA_TAU, GLA_HEADS)
    gb = heads(jax.nn.log_sigmoid((parts['gla_ab'] @ wa2[1] + ba[1]).astype(jnp.float32)) / GLA_TAU, GLA_HEADS)
    return (q, k, k, v, gf, gb)


def hgrn_inputs(parts, lb):
    q = heads(jax.nn.silu(parts['hg_q']), HG_HEADS)
    v = heads(parts['hg_i'], HG_HEADS)
    res = []
    for d, name in enumerate(('hg_ff', 'hg_fb')):
        z = parts[name].astype(jnp.float32)
        f = lb[d] + (1 - lb[d]) * jax.nn.sigmoid(z)
        log_f = jnp.log(jnp.maximum(f, F_TINY))
        k = (1 - lb[d]) * jax.nn.sigmoid(-z)
        res.append((heads(k, HG_HEADS), heads(log_f, HG_HEADS)))
    (kf, gf), (kb, gb) = res
    return (q, kf, kb, v, gf, gb)


def gated_branch(o, g, gate):
    return merge_heads(rms_norm(o, g)) * jax.nn.silu(gate)


def hyena_filters(L, p):
    j = jnp.arange(L, dtype=jnp.float32)
    t = j / max(L - 1, 1)
    w = 2 * math.pi * j / L
    f = jnp.linspace(1e-4, HY_BANDS - 1, HY_BANDS, dtype=jnp.float32)
    feats = jnp.concatenate([t[:, None], jnp.cos(w[:, None] * f), -jnp.sin(w[:, None] * f)], axis=-1)
    h = jnp.sin(p['hy_freq'][0] * (feats @ p['hy_w1'] + p['hy_b1']))
    h = jnp.sin(p['hy_freq'][1] * (h @ p['hy_w2'] + p['hy_b2']))
    h = (h @ p['hy_w3']).astype(jnp.float32)
    dist = jnp.abs(j - L // 2) / (L // 2)
    h = h * (jnp.exp(-dist[:, None] * jnp.abs(p['hy_decay'].astype(jnp.float32))) + HY_SHIFT)
    h = h / jnp.sum(jnp.abs(h), axis=0, keepdims=True)
    return h.reshape(L, HY_ORDER, HY_WIDTH)


def fft_conv(u, h):
    L = u.shape[1]
    n = 2 * L
    U = jnp.fft.rfft(u.astype(jnp.float32), n=n, axis=1)
    Hf = jnp.fft.rfft(h, n=n, axis=0)
    y = jnp.fft.irfft(U * Hf[None], n=n, axis=1)[:, L // 2: L // 2 + L]
    return y.astype(u.dtype)


def hyena_branch(u3, p):
    L = u3.shape[1]
    v, x1, x2 = jnp.split(short_conv(u3, p['hy_conv_w']), 1 + HY_ORDER, axis=-1)
    h = hyena_filters(L, p)
    z = v
    for o, gate in enumerate((x1, x2)):
        z = gate * (fft_conv(z, h[:, o]) + p['hy_bias'][o] * z)
    return z


def rope_axis(t, pos):
    half = t.shape[-1] // 2
    freqs = ROPE_BASE ** (-jnp.arange(half, dtype=jnp.float32) / half)
    ang = pos.astype(jnp.float32)[:, None] * freqs
    cos, sin = jnp.cos(ang).astype(t.dtype), jnp.sin(ang).astype(t.dtype)
    t1, t2 = t[..., :half], t[..., half:]
    return jnp.concatenate([t1 * cos - t2 * sin, t1 * sin + t2 * cos], axis=-1)


def rope2d(t, row, col):
    a = t.shape[-1] // 2
    return jnp.concatenate([rope_axis(t[..., :a], row), rope_axis(t[..., a:], col)], axis=-1)


def da_qk(t, g):
    B, L, _ = t.shape
    t = rms_norm(t.reshape(B, L, DA_HEADS, 2, DA_DK), g).transpose(0, 2, 1, 3, 4)
    return t[..., 0, :], t[..., 1, :]


def diff_attend(q1, q2, k1, k2, v, lam):
    B, H, Lq, d = q1.shape
    nb = Lq // Q_BLOCK
    scale = d ** -0.5

    def blocks(t):
        return t.reshape(B, H, nb, Q_BLOCK, d).transpose(2, 0, 1, 3, 4)

    def one(qs):
        qa, qb = qs
        p1 = jax.nn.softmax(jnp.einsum('bhqd,bhkd->bhqk', qa, k1).astype(jnp.float32) * scale, axis=-1)
        p2 = jax.nn.softmax(jnp.einsum('bhqd,bhkd->bhqk', qb, k2).astype(jnp.float32) * scale, axis=-1)
        return jnp.einsum('bhqk,bhkd->bhqd', (p1 - lam * p2).astype(v.dtype), v)

    o = lax.map(one, (blocks(q1), blocks(q2)))
    return o.transpose(1, 2, 0, 3, 4).reshape(B, H, Lq, v.shape[-1])


def merge_branches(outs, gate_cols, w_branch, w_out):
    B, L, _ = gate_cols.shape
    gates = jax.nn.sigmoid(gate_cols.reshape(B, L, N_BRANCH, D_MODEL))
    merged = gates[:, :, 0] * (outs[0] @ w_branch[0])
    for i in range(1, N_BRANCH):
        merged = merged + gates[:, :, i] * (outs[i] @ w_branch[i])
    return merged @ w_out


def token_mixer(hc, hx, p, lb, row, col, lam_init, last):
    split_idx = np.cumsum(IN_WIDTHS)[:-1].tolist()
    pc = dict(zip(IN_NAMES, jnp.split(hc @ p['w_in'], split_idx, axis=-1)))
    px = dict(zip(IN_NAMES, jnp.split(hx @ p['w_in'], split_idx, axis=-1)))
    gla_c, gla_x = bidir_recurrence(gla_inputs(pc, p['gla_wa2'], p['gla_ba']), gla_inputs(px, p['gla_wa2'], p['gla_ba']))
    hg_c, hg_x = bidir_recurrence(hgrn_inputs(pc, lb), hgrn_inputs(px, lb))
    q1c, q2c = da_qk(pc['da_q'], p['da_qnorm_g'])
    k1c, k2c = da_qk(pc['da_k'], p['da_knorm_g'])
    vc = heads(pc['da_v'], DA_HEADS)
    q1x, q2x = [rope2d(t, row, col) for t in da_qk(px['da_q'], p['da_qnorm_g'])]
    k1x, k2x = [rope2d(t, row, col) for t in da_qk(px['da_k'], p['da_knorm_g'])]
    vx = heads(px['da_v'], DA_HEADS)
    lp = p['da_lam'].astype(jnp.float32)
    lam = jnp.exp(jnp.sum(lp[0] * lp[1])) - jnp.exp(jnp.sum(lp[2] * lp[3])) + lam_init
    cat = lambda a, b: jnp.concatenate([a, b], axis=2)
    da_x = diff_attend(q1x, q2x, cat(k1c, k1x), cat(k2c, k2x), cat(vc, vx), lam)

    def finish(parts, gla_o, hg_o, da_o):
        outs = (gated_branch(gla_o, p['gla_norm_g'], parts['gla_g']),
                hyena_branch(parts['hy'], p),
                gated_branch(hg_o, p['hg_norm_g'], parts['hg_g']),
                merge_heads(rms_norm(da_o, p['da_norm_g']) * (1 - lam_init)))
        return merge_branches(outs, parts['merge'], p['w_branch'], p['w_out'])

    mx = finish(px, gla_x, hg_x, da_x)
    if last:
        return None, mx
    da_c = diff_attend(q1c, q2c, k1c, k2c, vc, lam)
    return finish(pc, gla_c, hg_c, da_c), mx


def expert_choice_moe(h, router, w1, w3, w2):
    B, L, D = h.shape
    cap = CAPACITY_FACTOR * L // N_EXPERTS
    aff = jax.nn.softmax((h @ router).astype(jnp.float32), axis=-1)
    g, idx = lax.top_k(aff.transpose(0, 2, 1), cap)
    xg = jax.vmap(lambda hb, ib: hb[ib])(h, idx)
    a = jnp.einsum('becd,edf->becf', xg, w1)
    b = jnp.einsum('becd,edf->becf', xg, w3)
    y = jnp.einsum('becf,efd->becd', jax.nn.silu(a) * b, w2) * g[..., None].astype(h.dtype)
    return jax.vmap(lambda yb, ib: jnp.zeros((L, D), h.dtype).at[ib.reshape(-1)].add(yb.reshape(-1, D)))(y, idx)


def setup_inputs(seed: int = 0) -> dict:
    key = jax.random.key(seed)
    ks = jax.random.split(key, 34)
    D = D_MODEL
    nrm = lambda k, shape, s: jax.random.normal(k, shape, jnp.float32) * s
    return {
        'x': nrm(ks[0], (BATCH, SEQ, D), 1.0),
        'c': nrm(ks[1], (BATCH, D), 1.0),
        'ctx': nrm(ks[2], (BATCH, CTX_LEN, D), 1.0),
        'c_ctx': nrm(ks[3], (D,), 1.0),
        'ada_w': nrm(ks[4], (DEPTH, D, ADA_CHUNKS * D), 0.5 * D ** -0.5),
        'ada_b': nrm(ks[5], (DEPTH, ADA_CHUNKS * D), 0.02),
        'norm1_g': 1 + nrm(ks[6], (DEPTH, D), 0.02),
        'norm2_g': 1 + nrm(ks[7], (DEPTH, D), 0.02),
        'w_in': nrm(ks[8], (DEPTH, D, N_IN), D ** -0.5),
        'gla_wa2': nrm(ks[9], (DEPTH, 2, GLA_RANK, GLA_HEADS * GLA_DK), GLA_RANK ** -0.5),
        'gla_ba': nrm(ks[10], (DEPTH, 2, GLA_HEADS * GLA_DK), 0.1),
        'gla_norm_g': 1 + nrm(ks[11], (DEPTH, GLA_DV), 0.02),
        'hy_conv_w': nrm(ks[12], (DEPTH, 3, (1 + HY_ORDER) * HY_WIDTH), 3 ** -0.5),
        'hy_w1': nrm(ks[13], (DEPTH, HY_FEAT, HY_FFN), HY_FEAT ** -0.5),
        'hy_b1': nrm(ks[14], (DEPTH, HY_FFN), 0.1),
        'hy_w2': nrm(ks[15], (DEPTH, HY_FFN, HY_FFN), HY_FFN ** -0.5),
        'hy_b2': nrm(ks[16], (DEPTH, HY_FFN), 0.1),
        'hy_w3': nrm(ks[17], (DEPTH, HY_FFN, HY_ORDER * HY_WIDTH), HY_FFN ** -0.5),
        'hy_freq': 1 + nrm(ks[18], (DEPTH, 2, HY_FFN), 0.02),
        'hy_decay': jnp.linspace(3.0, 15.0, HY_ORDER * HY_WIDTH, dtype=jnp.float32)[None] + nrm(ks[19], (DEPTH, HY_ORDER * HY_WIDTH), 0.1),
        'hy_bias': nrm(ks[20], (DEPTH, HY_ORDER, HY_WIDTH), 0.5),
        'hg_lower': nrm(ks[21], (DEPTH, 2, HG_HEADS * HG_DK), 0.1),
        'hg_norm_g': 1 + nrm(ks[22], (DEPTH, HG_DV), 0.02),
        'da_qnorm_g': 1 + nrm(ks[23], (DEPTH, DA_DK), 0.02),
        'da_knorm_g': 1 + nrm(ks[24], (DEPTH, DA_DK), 0.02),
        'da_lam': nrm(ks[25], (DEPTH, 4, DA_DK), 0.1),
        'da_norm_g': 1 + nrm(ks[26], (DEPTH, DA_DV), 0.02),
        'w_branch': nrm(ks[27], (DEPTH, N_BRANCH, MIX_W, D), MIX_W ** -0.5),
        'w_out': nrm(ks[28], (DEPTH, D, D), D ** -0.5),
        'moe_router': nrm(ks[29], (DEPTH, D, N_EXPERTS), D ** -0.5),
        'moe_w1': nrm(ks[30], (DEPTH, N_EXPERTS, D, EXPERT_FF), D ** -0.5),
        'moe_w3': nrm(ks[31], (DEPTH, N_EXPERTS, D, EXPERT_FF), D ** -0.5),
        'moe_w2': nrm(ks[32], (DEPTH, N_EXPERTS, EXPERT_FF, D), EXPERT_FF ** -0.5),
    }


def reference(x, c, ctx, c_ctx, ada_w, ada_b, norm1_g, norm2_g, w_in, gla_wa2, gla_ba, gla_norm_g,
              hy_conv_w, hy_w1, hy_b1, hy_w2, hy_b2, hy_w3, hy_freq, hy_decay, hy_bias,
              hg_lower, hg_norm_g, da_qnorm_g, da_knorm_g, da_lam, da_norm_g, w_branch, w_out,
              moe_router, moe_w1, moe_w3, moe_w2):
    L = x.shape[1]
    ROWS = L // GRID_W
    row = jnp.repeat(jnp.arange(ROWS), GRID_W)
    col = jnp.tile(jnp.arange(GRID_W), ROWS)
    P = jax.nn.softmax(hg_lower.astype(jnp.float32), axis=0)
    lower = jnp.cumsum(P, axis=0) - P[0]
    sc = jax.nn.silu(c)
    scc = jax.nn.silu(c_ctx)
    xc, xx = ctx, x
    for l in range(DEPTH):
        last = l == DEPTH - 1
        lam_init = 0.8 - 0.6 * math.exp(-0.3 * l)
        p = {'w_in': w_in[l], 'gla_wa2': gla_wa2[l], 'gla_ba': gla_ba[l], 'gla_norm_g': gla_norm_g[l],
             'hy_conv_w': hy_conv_w[l], 'hy_w1': hy_w1[l], 'hy_b1': hy_b1[l], 'hy_w2': hy_w2[l], 'hy_b2': hy_b2[l],
             'hy_w3': hy_w3[l], 'hy_freq': hy_freq[l], 'hy_decay': hy_decay[l], 'hy_bias': hy_bias[l],
             'hg_norm_g': hg_norm_g[l], 'da_qnorm_g': da_qnorm_g[l], 'da_knorm_g': da_knorm_g[l],
             'da_lam': da_lam[l], 'da_norm_g': da_norm_g[l], 'w_branch': w_branch[l], 'w_out': w_out[l]}
        mod_x = jnp.split((sc @ ada_w[l] + ada_b[l])[:, None, :], ADA_CHUNKS, axis=-1)
        mod_c = jnp.split((scc @ ada_w[l] + ada_b[l])[None, None, :], ADA_CHUNKS, axis=-1)
        hx = modulate(xx, norm1_g[l], mod_x[0], mod_x[1])
        hc = modulate(xc, norm1_g[l], mod_c[0], mod_c[1])
        mc, mx = token_mixer(hc, hx, p, lower[l], row, col, lam_init, last)
        xx = xx + mod_x[2] * mx
        hx = modulate(xx, norm2_g[l], mod_x[3], mod_x[4])
        xx = xx + mod_x[5] * expert_choice_moe(hx, moe_router[l], moe_w1[l], moe_w3[l], moe_w2[l])
        if not last:
            xc = xc + mod_c[2] * mc
            hc = modulate(xc, norm2_g[l], mod_c[3], mod_c[4])
            xc = xc + mod_c[5] * expert_choice_moe(hc, moe_router[l], moe_w1[l], moe_w3[l], moe_w2[l])
    return xx
```

```python
import math
import numpy as np
import ml_dtypes
import concourse.bass as bass
import concourse.mybir as mybir
from concourse.bass_utils import run_bass_kernel_spmd

F32 = mybir.dt.float32
BF16 = mybir.dt.bfloat16
I32 = mybir.dt.int32
U32 = mybir.dt.uint32
U8 = mybir.dt.uint8
AF = mybir.ActivationFunctionType
ALU = mybir.AluOpType
AX = mybir.AxisListType
NPBF = ml_dtypes.bfloat16

D = 1024
TC = 256
TL = 4096
TT = TC + TL
NB = 2
NCORES = 8
DEPTH = 2
NIN = 7712
EPS = 1e-6
PAGE = 2048

OFF = {}
_o = 0
for _n, _w in (("gla_q", 128), ("gla_k", 128), ("gla_v", 256), ("gla_af", 16), ("gla_ab", 16), ("gla_g", 256),
               ("hy", 768), ("hg_q", 256), ("hg_ff", 256), ("hg_fb", 256), ("hg_i", 256), ("hg_g", 256),
               ("da_q", 256), ("da_k", 256), ("da_v", 256), ("merge", 4096)):
    OFF[_n] = (_o, _w)
    _o += _w
assert _o == NIN

COMPUTE = ("pe", "dve", "act", "pool")
DMAQ = ("sp", "pq")
NSEM_DMA = {"sp": 36, "pq": 20}
EPOCH = 16000
NEPOCH = 9


class Ins:
    __slots__ = ("eng", "fn", "deps", "flag", "dma", "idx")

    def __init__(self, eng, fn, dma):
        self.eng = eng
        self.fn = fn
        self.deps = set()
        self.flag = False
        self.dma = dma


class Sched:
    def __init__(self, nc):
        self.nc = nc
        self.ins = []
        self.lastw = {}
        self.readers = {}

    def _add(self, eng, fn, reads, writes, dma=False):
        phys = "pool" if eng == "pq" else eng
        I = Ins(eng, fn, dma)
        I.idx = len(self.ins)
        lastw = self.lastw
        readers = self.readers
        raw = set()
        for k in reads:
            w = lastw.get(k)
            if w is not None:
                raw.add(w)
        other = set()
        for k in writes:
            w = lastw.get(k)
            if w is not None:
                other.add(w)
            rs = readers.get(k)
            if rs:
                other.update(rs)
        if dma:
            I.deps = raw | other
        else:
            deps = set()
            for d in raw:
                dphys = "pool" if d.eng == "pq" else d.eng
                if d.dma or dphys != phys or phys != "pe":
                    deps.add(d)
            for d in other:
                dphys = "pool" if d.eng == "pq" else d.eng
                if d.dma or dphys != phys:
                    deps.add(d)
            I.deps = deps
        I.deps.discard(I)
        for k in writes:
            lastw[k] = I
            readers[k] = []
        wset = set(writes)
        for k in reads:
            if k not in wset:
                readers.setdefault(k, []).append(I)
        self.ins.append(I)
        return I

    def pe(self, fn, reads, writes):
        return self._add("pe", fn, reads, writes)

    def dve(self, fn, reads, writes):
        return self._add("dve", fn, reads, writes)

    def act(self, fn, reads, writes):
        return self._add("act", fn, reads, writes)

    def pool(self, fn, reads, writes):
        return self._add("pool", fn, reads, writes)

    def dma(self, fn, reads, writes, q="sp"):
        return self._add(q, fn, reads, writes, dma=True)

    def emit(self, final_keys=()):
        nc = self.nc
        for I in self.ins:
            for d in I.deps:
                d.flag = True
        finals = []
        for k in final_keys:
            if k in self.lastw and self.lastw[k] not in finals:
                finals.append(self.lastw[k])
        for f in finals:
            f.flag = True
        sems = {e: [nc.alloc_semaphore("s_%s%d" % (e, i)) for i in range(NEPOCH)] for e in COMPUTE}
        dsem = {q: [nc.alloc_semaphore("d_%s%d" % (q, i)) for i in range(NSEM_DMA[q])] for q in DMAQ}
        dcnt = {q: [0] * NSEM_DMA[q] for q in DMAQ}
        dnext = {q: 0 for q in DMAQ}
        ccnt = {e: 0 for e in COMPUTE}
        ev = {}
        guard = {}
        for I in self.ins:
            if I.dma:
                q = I.eng
                i = dnext[q]
                dnext[q] = (i + 1) % NSEM_DMA[q]
                prev = dcnt[q][i]
                dcnt[q][i] += 16
                ev[I] = (dsem[q][i], dcnt[q][i])
                guard[I] = (dsem[q][i], prev) if prev > 0 else None
            elif I.flag:
                ccnt[I.eng] += 1
                ep = (ccnt[I.eng] - 1) // EPOCH
                assert ep < NEPOCH, "too many semaphore epochs"
                ev[I] = (sems[I.eng][ep], (ccnt[I.eng] - 1) % EPOCH + 1)
        streams = {"pe": [], "dve": [], "act": [], "pool": [], "sp": []}
        for I in self.ins:
            streams["pool" if I.eng == "pq" else I.eng].append(I)
        self.stats = {k: len(v) for k, v in streams.items()}
        self.stats["semmax"] = dict(ccnt)

        def run_stream(phys, eng):
            waited = {}
            for I in streams[phys]:
                need = {}
                for d in I.deps:
                    s, v = ev[d]
                    key = id(s)
                    if key not in need or need[key][1] < v:
                        need[key] = (s, v)
                if I.dma and guard[I] is not None:
                    s, v = guard[I]
                    key = id(s)
                    if key not in need or need[key][1] < v:
                        need[key] = (s, v)
                for key, (s, v) in need.items():
                    if waited.get(key, 0) < v:
                        eng.wait_ge(s, v)
                        waited[key] = v
                bi = I.fn(eng)
                if I.dma:
                    bi.then_inc(ev[I][0], 16)
                elif I.flag:
                    bi.then_inc(ev[I][0], 1)
            if phys == "sp":
                for f in finals:
                    s, v = ev[f]
                    eng.wait_ge(s, v)

        with nc.Block() as block:
            @block.tensor
            def _(e):
                run_stream("pe", e)

            @block.vector
            def _(e):
                run_stream("dve", e)

            @block.scalar
            def _(e):
                run_stream("act", e)

            @block.gpsimd
            def _(e):
                run_stream("pool", e)

            @block.sync
            def _(e):
                run_stream("sp", e)


class Tn:
    def __init__(self, ap, off, nbytes, esize):
        self.ap = ap
        self.off = off
        self.nbytes = nbytes
        self.esize = esize

    def k(self, lo=0, hi=None):
        b0 = self.off + lo * self.esize
        b1 = self.off + (self.nbytes if hi is None else hi * self.esize)
        return [("sb", p) for p in range(b0 // PAGE, (b1 - 1) // PAGE + 1)]


ESZ = {F32: 4, BF16: 2, I32: 4, U32: 4, U8: 1}


class Mem:
    def __init__(self, nc, nbytes):
        self.ar = nc.alloc_sbuf_tensor("arena", [128, nbytes], U8).ap()
        self.nbytes = nbytes
        self.top = 0
        self.peak = 0

    def alloc(self, shape, dtype, parts=128):
        es = ESZ[dtype]
        n = 1
        for s in shape:
            n *= s
        nb = n * es
        off = (self.top + 63) // 64 * 64
        assert off + nb <= self.nbytes, "SBUF arena overflow: need %d have %d" % (off + nb, self.nbytes)
        self.top = off + nb
        self.peak = max(self.peak, self.top)
        ap = self.ar[0:parts, off:off + nb].bitcast(dtype)
        if len(shape) == 2:
            ap = ap.rearrange("p (a b) -> p a b", a=shape[0])
        elif len(shape) == 3:
            ap = ap.rearrange("p (a b c) -> p a b c", a=shape[0], b=shape[1])
        return Tn(ap, off, nb, es)

    def mark(self):
        return self.top

    def release(self, m):
        self.top = m


def dkeys(name, lo, hi, gran):
    return [(name, i) for i in range(lo // gran, (hi - 1) // gran + 1)]


def host_consts():
    c = {}
    c["ident_bf"] = np.eye(128, dtype=np.float32).astype(NPBF)
    c["ident_f"] = np.eye(128, dtype=np.float32)
    c["ones_f"] = np.ones((128, 128), dtype=np.float32)
    c["ones_row"] = np.ones((1, 512), dtype=np.float32)
    tpos = np.arange(TL)
    pos = np.stack([tpos // 64, tpos % 64], 1).astype(np.float32)
    fr = (10000.0 ** (-np.arange(8, dtype=np.float32) / 8.0)).astype(np.float32)
    ang = pos[:, :, None] * fr[None, None, :]
    c["rope"] = np.stack([np.cos(ang), np.sin(ang)], 1).astype(np.float32).reshape(TL, 32)
    gm = np.zeros((128, 4), np.float32)
    for r_ in range(4):
        gm[r_ * 32:(r_ + 1) * 32, r_] = 1.0
    c["gm4"] = gm
    for L in (TL, TC):
        n = 2 * L
        N1 = n // 128
        K1 = N1 // 2
        tg = "%d" % L
        j = np.arange(L, dtype=np.float64)
        tt_ = j / max(L - 1, 1)
        w_ = 2 * np.pi * j / L
        f_ = np.linspace(1e-4, 15, 16)
        feats = np.concatenate([tt_[:, None], np.cos(w_[:, None] * f_), -np.sin(w_[:, None] * f_)], axis=-1)
        c["hy_featsT" + tg] = np.ascontiguousarray(feats.T).astype(np.float32)
        dist = np.abs(j - L // 2) / (L // 2)
        c["hy_negdist" + tg] = np.ascontiguousarray((-dist).reshape(L // 128, 128).T).astype(np.float32)
        t1 = np.arange(K1)[:, None]
        f1 = np.arange(N1)[None, :]
        W1 = np.exp(-2j * np.pi * f1 * t1 / N1 - 1j * np.pi * t1 / N1)
        c["hy_W1_" + tg] = np.concatenate([W1.real, W1.imag], 1).astype(NPBF)
        E = np.exp(2j * np.pi * f1.T * t1.T / N1 + 1j * np.pi * t1.T / N1)
        c["hy_E_" + tg] = np.concatenate([(2.0 / n) * E.real, -(2.0 / n) * E.imag], 0).astype(NPBF)
        t2 = np.arange(128)[:, None]
        f2 = np.arange(64)[None, :]
        PK = np.zeros((N1, 128, 6, 128), np.float64)
        PKH = np.zeros((N1, 128, 4, 128), np.float64)
        for a in range(N1):
            f = a + N1 * f2
            ang = np.pi * (2 * f + 1) * t2 / n
            Gc, Gs = np.cos(ang), -np.sin(ang)
            PK[a, :, 0, :] = np.concatenate([Gc, Gs], 1)
            PK[a, :, 1, :] = np.concatenate([-Gs, Gc], 1)
            PK[a, :, 2, :] = np.concatenate([Gs, Gc], 1)
            PK[a, :, 3, :] = np.concatenate([Gc, -Gs], 1)
            PK[a, :, 4, :] = np.concatenate([Gc.T, Gs.T], 0)
            PK[a, :, 5, :] = np.concatenate([-Gs.T, Gc.T], 0)
            rot = np.exp(1j * np.pi * (2 * f + 1) / 4)
            Gp = (Gc + 1j * Gs) * rot
            Gpc, Gps = Gp.real, Gp.imag
            PKH[a, :, 0, :] = np.concatenate([Gpc, Gpc], 1)
            PKH[a, :, 1, :] = np.concatenate([-Gps, -Gps], 1)
            PKH[a, :, 2, :] = np.concatenate([-Gps, Gps], 1)
            PKH[a, :, 3, :] = np.concatenate([-Gpc, Gpc], 1)
        c["hy_PK_" + tg] = PK.astype(NPBF)
        c["hy_PKH_" + tg] = PKH.astype(NPBF)
    for C, hp in ((128, 4), (32, 2)):
        s_ = np.arange(C)[:, None]
        t_ = np.arange(C)[None, :]
        trif = (s_ <= t_).astype(np.float32)
        trib = (s_ >= t_).astype(np.float32)
        c["tri%d" % C] = np.stack([trif, trib], 1).copy()
        c["msk%d" % C] = np.stack([np.tile(trif, (1, hp)), np.tile(trib, (1, hp))], 1).astype(np.float32)
        dk = 128 // hp
        hm = np.zeros((128, hp, C), np.float32)
        bd = np.zeros((128, hp, 64), np.float32)
        for hh in range(hp):
            hm[hh * dk:(hh + 1) * dk, hh, :] = 1.0
            bd[hh * dk:(hh + 1) * dk, hh, :] = 1.0
        c["hm%d" % C] = hm
        c["bd%d" % C] = bd
    return c


class K:
    pass


def build_program(dbg=(), stop=None, feed=(), plan=None, need=None):
    nc = bass.Bass("TRN2", target_bir_lowering=False)
    S = Sched(nc)
    mem = Mem(nc, 206 * 1024)
    g = K()
    g.nc, g.S, g.mem = nc, S, mem
    g.dbg = {}

    def din(name, shape, dt=F32):
        if need is not None and name not in need:
            return nc.dram_tensor(name, list(shape), dt, kind="Internal").ap()
        return nc.dram_tensor(name, list(shape), dt, kind="ExternalInput").ap()

    def dscr(name, shape, dt=F32):
        kind = "ExternalOutput" if name in dbg else ("ExternalInput" if name in feed else "Internal")
        t = nc.dram_tensor(name, list(shape), dt, kind=kind).ap()
        if name in dbg:
            g.dbg[name] = t
        return t
    g.dscr = dscr

    I = {}
    I["x"] = din("x", [NB, TL, D])
    I["ctx"] = din("ctx", [NB, TC, D])
    I["ccT"] = din("ccT", [128, 8, 3])
    I["ada_w"] = din("ada_w", [DEPTH, D, 6 * D])
    I["ada_b"] = din("ada_b", [DEPTH, 6 * D])
    I["norm1_g"] = din("norm1_g", [DEPTH, D])
    I["norm2_g"] = din("norm2_g", [DEPTH, D])
    I["w_in"] = din("w_in", [DEPTH, D, NIN])
    for nm, shp in (("gla_wa2", [DEPTH, 2, 16, 128]), ("gla_ba", [DEPTH, 2, 128]), ("gla_norm_g", [DEPTH, 64]),
                    ("hy_conv_w", [DEPTH, 3, 768]), ("hy_w1", [DEPTH, 33, 64]), ("hy_b1", [DEPTH, 64]),
                    ("hy_w2", [DEPTH, 64, 64]), ("hy_b2", [DEPTH, 64]), ("hy_w3", [DEPTH, 64, 512]),
                    ("hy_freq", [DEPTH, 2, 64]), ("hy_decay", [DEPTH, 512]), ("hy_bias", [DEPTH, 2, 256]),
                    ("hg_lower", [DEPTH, 2, 256]), ("hg_norm_g", [DEPTH, 64]), ("da_qnorm_g", [DEPTH, 32]),
                    ("da_knorm_g", [DEPTH, 32]), ("da_lam", [DEPTH, 4, 32]), ("da_norm_g", [DEPTH, 64]),
                    ("w_branch", [DEPTH, 4, 256, D]), ("w_out", [DEPTH, D, D]), ("moe_router", [DEPTH, D, 16]),
                    ("moe_w1", [DEPTH, 16, D, D]), ("moe_w3", [DEPTH, 16, D, D]), ("moe_w2", [DEPTH, 16, D, D])):
        I[nm] = din(nm, shp)
    cst = host_consts()
    for k, v in cst.items():
        I[k] = din(k, v.shape, BF16 if v.dtype == NPBF else F32)
    g.I = I
    OUT = nc.dram_tensor("out", [NB, TL, D], F32, kind="ExternalOutput").ap()
    g.OUT = OUT
    g.XC = dscr("XC", [NB, TC, D])
    g.MODS = dscr("MODS", [DEPTH, 3, 6 * D])
    g.HTOK = dscr("HTOK", [TT, D], BF16)
    g.QK_GLA = dscr("QK_GLA", [256, TT])
    g.AFB = dscr("AFB", [32, TT])
    g.V_GLA = dscr("V_GLA", [TT, 256], BF16)
    g.G_GLA = dscr("G_GLA", [TT, 256], BF16)
    g.HY = dscr("HY", [TT, 768])
    g.Q_HG = dscr("Q_HG", [256, TT])
    g.ZT_HG = dscr("ZT_HG", [512, TT])
    g.Z_HG = dscr("Z_HG", [TT, 512])
    g.VG_HG = dscr("VG_HG", [TT, 512], BF16)
    g.QK_DA = dscr("QK_DA", [TT, 512])
    g.V_DA = dscr("V_DA", [TT, 256], BF16)
    g.MG = dscr("MG", [4096, TT], BF16)
    g.O_GLA = dscr("O_GLA", [2, TT, 256])
    g.O_HG = dscr("O_HG", [2, TT, 256])
    g.O_DA = dscr("O_DA", [1, TT, 256])
    g.BRT = dscr("BRT", [4, 256, TT], BF16)
    g.HFILT = {TL: dscr("HFILT_X", [TL, 512], BF16), TC: dscr("HFILT_C", [TC, 512], BF16)}
    g.HAB = {TL: dscr("HAB_X", [2, 64, 128, 512]), TC: dscr("HAB_C", [2, 4, 128, 512])}
    g.UCONV = dscr("UCONV", [TT, 768])
    g.ZB = dscr("ZB", [2, TT, 256], BF16)
    g.ZF = dscr("ZF", [TT, 256])
    g.AD = dscr("AD", [128, 128, 512], BF16)
    g.ZD = dscr("ZD", [128, 128, 256], BF16)

    psall = nc.alloc_psum_tensor("psall", [128, 8 * 512], F32).ap()
    g.PS = [psall[:, b * 512:(b + 1) * 512] for b in range(8)]
    g.PSB = [psall[:, b * 512:(b + 1) * 512].bitcast(BF16) for b in range(8)]
    g.psk = [[("ps", b)] for b in range(8)]

    g.ident_bf = mem.alloc([128], BF16)
    g.ident_f = mem.alloc([128], F32)
    g.ones_f = mem.alloc([128], F32)
    S.dma(lambda e: e.dma_start(out=g.ident_bf.ap, in_=I["ident_bf"]), [], g.ident_bf.k())
    S.dma(lambda e: e.dma_start(out=g.ident_f.ap, in_=I["ident_f"]), [], g.ident_f.k())
    S.dma(lambda e: e.dma_start(out=g.ones_f.ap, in_=I["ones_f"]), [], g.ones_f.k())
    g.hT = mem.alloc([8, TT], BF16)

    for b in range(NB):
        for q in range(4):
            S.dma(lambda e, b=b, q=q: e.dma_start(out=OUT[b, q * 1024:(q + 1) * 1024, :], in_=I["x"][b, q * 1024:(q + 1) * 1024, :]),
                  [], dkeys(("XX", b), q * 1024, (q + 1) * 1024, 128))
        S.dma(lambda e, b=b: e.dma_start(out=g.XC[b], in_=I["ctx"][b]), [], dkeys(("XC", b), 0, TC, 128))

    if plan is not None:
        for (fn, args) in plan:
            globals()[fn](g, *args)
        return finish(g)
    stage_mods_all(g)
    for l in range(DEPTH):
        last = (l == DEPTH - 1)
        lam_init = 0.8 - 0.6 * math.exp(-0.3 * l)
        stage_hy_filter(g, l, TL)
        if not last:
            stage_hy_filter(g, l, TC)
        tok_lo = TC if last else 0
        for b in range(NB):
            stage_norm(g, l, b, 1)
            stage_proj(g, l, b)
            stage_recur(g, l, b, "gla")
            stage_recur(g, l, b, "hg")
            stage_da(g, l, b, not last)
            stage_hy(g, l, b, not last)
            stage_branch_norm(g, l, b, "O_GLA", "O_GLA", 2, "G_GLA", 0, "G_GLA", "gla_norm_g", 1.0, 0, tok_lo)
            stage_branch_norm(g, l, b, "O_HG", "O_HG", 2, "VG_HG", 256, "VG_HG", "hg_norm_g", 1.0, 2, tok_lo)
            stage_branch_norm(g, l, b, "O_DA", "O_DA", 1, None, 0, None, "da_norm_g", 1.0 - lam_init, 3, tok_lo)
            stage_merge(g, l, b, tok_lo)
            stage_norm(g, l, b, 2, segs=("x",) if last else ("c", "x"))
            stage_moe(g, l, b, not last)
    return finish(g)


def finish(g):
    fk = []
    for b in range(NB):
        fk += dkeys(("XX", b), 0, TL, 128)
    for name in g.dbg:
        fk.append(("DBG", name))
    for k in list(g.S.lastw.keys()):
        if isinstance(k, tuple) and len(k) >= 1 and isinstance(k[0], str) and k[0] in g.dbg:
            fk.append(k)
    g.S.emit(final_keys=fk)
    return g


def stage_mods_all(g):
    nc, S, mem, I = g.nc, g.S, g.mem, g.I
    m0 = mem.mark()
    scT = mem.alloc([8, 3], F32)
    S.dma(lambda e: e.dma_start(out=scT.ap, in_=I["ccT"]), [], scT.k())
    S.act(lambda e: e.activation(out=scT.ap, in_=scT.ap, func=AF.Silu), scT.k(), scT.k())
    modrow = mem.alloc([6 * D], F32, parts=3)
    bias = mem.alloc([6 * D], F32, parts=3)
    gbc = mem.alloc([2, D], F32, parts=3)
    wsl = [mem.alloc([8, 512], F32) for _ in range(2)]
    for l in range(DEPTH):
        S.dma(lambda e, l=l: e.dma_start(out=bias.ap, in_=I["ada_b"][l:l + 1, :].partition_broadcast(3)), [], bias.k())
        S.dma(lambda e, l=l: e.dma_start(out=gbc.ap[:, 0, :], in_=I["norm1_g"][l:l + 1, :].partition_broadcast(3)), [], gbc.k())
        S.dma(lambda e, l=l: e.dma_start(out=gbc.ap[:, 1, :], in_=I["norm2_g"][l:l + 1, :].partition_broadcast(3)), [], gbc.k())
        for n in range(12):
            w = wsl[n % 2]
            src = I["ada_w"][l].rearrange("(k p) n -> p k n", p=128)[:, :, n * 512:(n + 1) * 512]
            S.dma(lambda e, w=w, src=src: e.dma_start(out=w.ap, in_=src), [], w.k())
            bank = n % 2
            for k in range(8):
                S.pe(lambda e, w=w, k=k, bank=bank: e.matmul(g.PS[bank][0:3, :], lhsT=scT.ap[:, k, :], rhs=w.ap[:, k, :],
                                                            start=(k == 0), stop=(k == 7)),
                     scT.k() + w.k(), g.psk[bank])
            S.dve(lambda e, n=n, bank=bank: e.tensor_tensor(out=modrow.ap[:, n * 512:(n + 1) * 512], in0=g.PS[bank][0:3, :],
                                                              in1=bias.ap[:, n * 512:(n + 1) * 512], op=ALU.add),
                  g.psk[bank] + bias.k(), modrow.k(n * 512, (n + 1) * 512))
        for (ch, gi) in ((1, 0), (4, 1)):
            S.dve(lambda e, ch=ch, gi=gi: e.scalar_tensor_tensor(out=modrow.ap[:, ch * D:(ch + 1) * D], in0=modrow.ap[:, ch * D:(ch + 1) * D],
                                                                  scalar=1.0, in1=gbc.ap[:, gi, :], op0=ALU.add, op1=ALU.mult),
                  modrow.k(ch * D, (ch + 1) * D) + gbc.k(), modrow.k(ch * D, (ch + 1) * D))
        S.dma(lambda e, l=l: e.dma_start(out=g.MODS[l], in_=modrow.ap), modrow.k(), [("MODS", l)])
    mem.release(m0)


def load_mod_bc(g, l, j, ch, dst):
    src = g.MODS[l, j:j + 1, ch * D:(ch + 1) * D].partition_broadcast(128)
    g.S.dma(lambda e: e.dma_start(out=dst.ap, in_=src), [("MODS", l)], dst.k())


def stage_norm(g, l, b, which, segs=("c", "x")):
    nc, S, mem = g.nc, g.S, g.mem
    m0 = mem.mark()
    sbc = mem.alloc([D], F32)
    shbc = mem.alloc([D], F32)
    xts = [mem.alloc([D], F32) for _ in range(2)]
    t1s = [mem.alloc([D], F32) for _ in range(2)]
    hbs = [mem.alloc([D], BF16) for _ in range(2)]
    junk = mem.alloc([D], BF16)
    st = [mem.alloc([2], F32) for _ in range(2)]
    cnt = 0
    for seg in segs:
        j = 2 if seg == "c" else b
        load_mod_bc(g, l, j, 1 if which == 1 else 4, sbc)
        load_mod_bc(g, l, j, 0 if which == 1 else 3, shbc)
        ntile = TC // 128 if seg == "c" else TL // 128
        for i in range(ntile):
            tok0 = i * 128 if seg == "c" else TC + i * 128
            xt, t1, hb, s_ = xts[cnt % 2], t1s[cnt % 2], hbs[cnt % 2], st[cnt % 2]
            bank = 2 + cnt % 2
            cnt += 1
            if seg == "c":
                src, sk = g.XC[b, i * 128:(i + 1) * 128, :], [(("XC", b), i)]
            else:
                src, sk = g.OUT[b, i * 128:(i + 1) * 128, :], [(("XX", b), i)]
            S.dma(lambda e, xt=xt, src=src: e.dma_start(out=xt.ap, in_=src), sk, xt.k())
            S.act(lambda e, xt=xt, s_=s_: e.activation(out=junk.ap, in_=xt.ap, func=AF.Square, accum_out=s_.ap[:, 0:1]),
                  xt.k(), junk.k() + s_.k())
            S.act(lambda e, s_=s_: e.activation(out=s_.ap[:, 1:2], in_=s_.ap[:, 0:1], func=AF.Sqrt, bias=EPS, scale=1.0 / D),
                  s_.k(), s_.k())
            S.dve(lambda e, s_=s_: e.reciprocal(out=s_.ap[:, 1:2], in_=s_.ap[:, 1:2]), s_.k(), s_.k())
            S.dve(lambda e, xt=xt, t1=t1, s_=s_: e.scalar_tensor_tensor(out=t1.ap, in0=xt.ap, scalar=s_.ap[:, 1:2], in1=sbc.ap,
                                                                         op0=ALU.mult, op1=ALU.mult),
                  xt.k() + s_.k() + sbc.k(), t1.k())
            S.pool(lambda e, t1=t1, hb=hb: e.tensor_tensor(out=hb.ap, in0=t1.ap, in1=shbc.ap, op=ALU.add),
                   t1.k() + shbc.k(), hb.k())
            if which == 2:
                S.dma(lambda e, hb=hb, tok0=tok0: e.dma_start(out=g.HTOK[tok0:tok0 + 128, :], in_=hb.ap),
                      hb.k(), [("HTOK", tok0 // 128)])
            for c in range(8):
                S.pe(lambda e, hb=hb, c=c, bank=bank: e.transpose(out=g.PSB[bank][:, c * 128:(c + 1) * 128],
                                                                  in_=hb.ap[:, c * 128:(c + 1) * 128], identity=g.ident_bf.ap),
                     hb.k() + g.ident_bf.k(), g.psk[bank])
            dst = g.hT.ap[:, :, tok0:tok0 + 128]
            hk = []
            for c in range(8):
                hk += g.hT.k(c * TT + tok0, c * TT + tok0 + 128)
            srcp = g.PSB[bank].rearrange("p (c t) -> p c t", c=8)
            if cnt % 2 == 0:
                S.act(lambda e, dst=dst, srcp=srcp: e.activation(out=dst, in_=srcp, func=AF.Copy), g.psk[bank], hk)
            else:
                S.dve(lambda e, dst=dst, srcp=srcp: e.tensor_copy(out=dst, in_=srcp), g.psk[bank], hk)
    mem.release(m0)


def load_w_bf16(g, dst, src_ap, nk=8):
    src = src_ap.rearrange("(k p) n -> p k n", p=128)
    g.S.dma(lambda e: e.dma_start(out=dst.ap, in_=src), [], dst.k(), q="pq")


def stage_proj(g, l, b):
    nc, S, mem, I = g.nc, g.S, g.mem, g.I
    m0 = mem.mark()
    W = I["w_in"][l]
    wsl = [mem.alloc([8, 512], BF16) for _ in range(2)]
    stg = [mem.alloc([512], F32) for _ in range(4)]
    wcnt = [0]
    scnt = [0]
    pcnt = [0]
    slabs = [(s * 512, min(512, TT - s * 512)) for s in range((TT + 511) // 512)]

    def next_w(c0, width):
        w = wsl[wcnt[0] % 2]
        wcnt[0] += 1
        wv = Tn(w.ap[:, :, 0:width], w.off, w.nbytes, w.esize)
        src = W[:, c0:c0 + width].rearrange("(k p) n -> p k n", p=128)
        S.dma(lambda e: e.dma_start(out=wv.ap, in_=src), [], w.k(), q="pq")
        return w

    def evac(kind, out_ap, ps_ap, rk, wk, scale=1.0):
        if kind == "copy":
            if scnt[0] % 2 == 0:
                S.dve(lambda e: e.tensor_copy(out=out_ap, in_=ps_ap), rk, wk)
            else:
                S.act(lambda e: e.activation(out=out_ap, in_=ps_ap, func=AF.Copy), rk, wk)
        elif kind == "scale":
            S.act(lambda e: e.activation(out=out_ap, in_=ps_ap, func=AF.Copy, scale=scale), rk, wk)
        elif kind == "silu":
            S.act(lambda e: e.activation(out=out_ap, in_=ps_ap, func=AF.Silu), rk, wk)
        elif kind == "sigmoid":
            S.act(lambda e: e.activation(out=out_ap, in_=ps_ap, func=AF.Sigmoid), rk, wk)

    fm = [
        (OFF["gla_q"][0], 128, g.QK_GLA, 0, "scale", F32, "QK_GLA"),
        (OFF["gla_k"][0], 128, g.QK_GLA, 128, "copy", F32, "QK_GLA"),
        (OFF["gla_af"][0], 32, g.AFB, 0, "copy", F32, "AFB"),
        (OFF["hg_q"][0], 256, g.Q_HG, 0, "silu", F32, "Q_HG"),
        (OFF["hg_ff"][0], 512, g.ZT_HG, 0, "copy", F32, "ZT_HG"),
        (OFF["merge"][0], 4096, g.MG, 0, "sigmoid", BF16, "MG"),
    ]
    for (c0, ncols, dst, r0, kind, dt, kn) in fm:
        for cs in range(0, ncols, 512):
            cw = min(512, ncols - cs)
            w = next_w(c0 + cs, cw)
            for ch in range(0, cw, 128):
                m = min(128, cw - ch)
                for (t0, tw) in slabs:
                    bank = 4 + pcnt[0] % 4
                    pcnt[0] += 1
                    hk = []
                    for k in range(8):
                        hk += g.hT.k(k * TT + t0, k * TT + t0 + tw)
                    for k in range(8):
                        S.pe(lambda e, w=w, k=k, ch=ch, m=m, bank=bank, t0=t0, tw=tw:
                             e.matmul(g.PS[bank][0:m, 0:tw], lhsT=w.ap[:, k, ch:ch + m], rhs=g.hT.ap[:, k, t0:t0 + tw],
                                      start=(k == 0), stop=(k == 7)),
                             w.k() + hk, g.psk[bank])
                    st = stg[scnt[0] % 4]
                    scnt[0] += 1
                    if dt == BF16:
                        sv = st.ap.bitcast(BF16)[0:m, 0:tw]
                    else:
                        sv = st.ap[0:m, 0:tw]
                    evac(kind, sv, g.PS[bank][0:m, 0:tw], g.psk[bank], st.k(), scale=32 ** -0.5)
                    rr = r0 + cs + ch
                    S.dma(lambda e, dst=dst, rr=rr, m=m, t0=t0, tw=tw, sv=sv: e.dma_start(out=dst[rr:rr + m, t0:t0 + tw], in_=sv),
                          st.k(), [(kn, rr // 128, t0 // 512)])
    tmg = [
        (OFF["gla_v"][0], 256, g.V_GLA, 0, "copy", BF16, "V_GLA"),
        (OFF["gla_g"][0], 256, g.G_GLA, 0, "silu", BF16, "G_GLA"),
        (OFF["hy"][0], 512, g.HY, 0, "copy", F32, "HY"),
        (OFF["hy"][0] + 512, 256, g.HY, 512, "copy", F32, "HY"),
        (OFF["hg_ff"][0], 512, g.Z_HG, 0, "copy", F32, "Z_HG"),
        (OFF["hg_i"][0], 256, g.VG_HG, 0, "copy", BF16, "VG_HG"),
        (OFF["hg_g"][0], 256, g.VG_HG, 256, "silu", BF16, "VG_HG"),
        (OFF["da_q"][0], 512, g.QK_DA, 0, "copy", F32, "QK_DA"),
        (OFF["da_v"][0], 256, g.V_DA, 0, "copy", BF16, "V_DA"),
    ]
    for (c0, ncols, dst, dc0, kind, dt, kn) in tmg:
        w = next_w(c0, ncols)
        for i in range(TT // 128):
            t0 = i * 128
            bank = 4 + pcnt[0] % 4
            pcnt[0] += 1
            hk = []
            for k in range(8):
                hk += g.hT.k(k * TT + t0, k * TT + t0 + 128)
            for k in range(8):
                S.pe(lambda e, w=w, k=k, bank=bank, t0=t0, ncols=ncols:
                     e.matmul(g.PS[bank][:, 0:ncols], lhsT=g.hT.ap[:, k, t0:t0 + 128], rhs=w.ap[:, k, 0:ncols],
                              start=(k == 0), stop=(k == 7)),
                     w.k() + hk, g.psk[bank])
            st = stg[scnt[0] % 4]
            scnt[0] += 1
            if dt == BF16:
                sv = st.ap.bitcast(BF16)[:, 0:ncols]
            else:
                sv = st.ap[:, 0:ncols]
            evac(kind, sv, g.PS[bank][:, 0:ncols], g.psk[bank], st.k())
            S.dma(lambda e, dst=dst, t0=t0, dc0=dc0, ncols=ncols, sv=sv: e.dma_start(out=dst[t0:t0 + 128, dc0:dc0 + ncols], in_=sv),
                  st.k(), [(kn, i, dc0)])
    mem.release(m0)


_CACHE = {}


def make_in_maps(inputs):
    cst = host_consts()
    maps = []
    x = np.ascontiguousarray(inputs["x"], dtype=np.float32)
    ctx = np.ascontiguousarray(inputs["ctx"], dtype=np.float32)
    c = np.asarray(inputs["c"], dtype=np.float32)
    c_ctx = np.asarray(inputs["c_ctx"], dtype=np.float32)
    shared = {}
    for k in inputs:
        if k not in ("x", "c", "ctx", "c_ctx"):
            shared[k] = np.ascontiguousarray(inputs[k], dtype=np.float32)
    for core in range(NCORES):
        b0 = core * NB
        cc = np.stack([c[b0], c[b0 + 1], c_ctx], axis=1)
        ccT = np.ascontiguousarray(cc.reshape(8, 128, 3).transpose(1, 0, 2))
        m = {"x": x[b0:b0 + NB], "ctx": ctx[b0:b0 + NB], "ccT": ccT}
        m.update(shared)
        m.update(cst)
        maps.append(m)
    return maps


def kernel(**inputs):
    if "prog" not in _CACHE:
        _CACHE["prog"] = build_program()
    g = _CACHE["prog"]
    maps = make_in_maps(inputs)
    res = run_bass_kernel_spmd(g.nc, maps, core_ids=list(range(NCORES)))
    out = np.concatenate([np.asarray(r["out"]) for r in res.results], axis=0)
    return out.astype(np.float32)


def stage_recur(g, l, b, kind):
    nc, S, mem, I = g.nc, g.S, g.mem, g.I
    m0 = mem.mark()
    if kind == "gla":
        dk, C, hp, nt = 32, 128, 4, 1
        O = g.O_GLA
        okn = "O_GLA"
    else:
        dk, C, hp, nt = 64, 32, 2, 2
        O = g.O_HG
        okn = "O_HG"
    HD = 4 * dk
    BLK = 512
    tri = mem.alloc([2, C], F32, parts=C)
    msk = mem.alloc([2, hp * C], F32, parts=C)
    hm = mem.alloc([hp, C], F32)
    bd = mem.alloc([hp, 64], F32)
    S.dma(lambda e: e.dma_start(out=tri.ap, in_=I["tri%d" % C]), [], tri.k())
    S.dma(lambda e: e.dma_start(out=msk.ap, in_=I["msk%d" % C]), [], msk.k())
    S.dma(lambda e: e.dma_start(out=hm.ap, in_=I["hm%d" % C]), [], hm.k())
    S.dma(lambda e: e.dma_start(out=bd.ap, in_=I["bd%d" % C]), [], bd.k())
    if kind == "gla":
        S.dve(lambda e: e.tensor_scalar(out=tri.ap, in0=tri.ap, scalar1=-1.0 / 16.0, scalar2=None, op0=ALU.mult), tri.k(), tri.k())
        wa2b = mem.alloc([2, 128], F32, parts=17)
        for d_ in range(2):
            S.dma(lambda e, d_=d_: e.dma_start(out=wa2b.ap[0:16, d_, :], in_=I["gla_wa2"][l, d_]), [], wa2b.k())
            S.dma(lambda e, d_=d_: e.dma_start(out=wa2b.ap[16:17, d_, :], in_=I["gla_ba"][l, d_:d_ + 1, :]), [], wa2b.k())
    else:
        lbb = mem.alloc([2, 256], F32, parts=C)
        omb = mem.alloc([2, 256], F32, parts=C)
        lbp = mem.alloc([2, 2], F32)
        omp = mem.alloc([2, 2], F32)
        if l == 0:
            S.dve(lambda e: e.memset(lbb.ap, 0.0), [], lbb.k())
            S.dve(lambda e: e.memset(omb.ap, 1.0), [], omb.k())
            S.dve(lambda e: e.memset(lbp.ap, 0.0), [], lbp.k())
            S.dve(lambda e: e.memset(omp.ap, 1.0), [], omp.k())
        else:
            t0_ = mem.alloc([2, 256], F32, parts=C)
            S.dma(lambda e: e.dma_start(out=lbb.ap, in_=I["hg_lower"][1:2].rearrange("o d e -> o (d e)").partition_broadcast(C)
                                        .rearrange("p o (d e) -> p (o d) e", d=2)), [], lbb.k())
            S.dma(lambda e: e.dma_start(out=t0_.ap, in_=I["hg_lower"][0:1].rearrange("o d e -> o (d e)").partition_broadcast(C)
                                        .rearrange("p o (d e) -> p (o d) e", d=2)), [], t0_.k())
            S.dve(lambda e: e.tensor_tensor(out=lbb.ap, in0=lbb.ap, in1=t0_.ap, op=ALU.subtract), lbb.k() + t0_.k(), lbb.k())
            S.act(lambda e: e.activation(out=lbb.ap, in_=lbb.ap, func=AF.Sigmoid), lbb.k(), lbb.k())
            S.dve(lambda e: e.tensor_scalar(out=omb.ap, in0=lbb.ap, scalar1=-1.0, scalar2=1.0, op0=ALU.mult, op1=ALU.add), lbb.k(), omb.k())
            t1_ = mem.alloc([2, 2], F32)
            S.dma(lambda e: e.dma_start(out=lbp.ap, in_=I["hg_lower"][1].rearrange("d (t p) -> p d t", p=128), allow_slow_non_contiguous=True), [], lbp.k())
            S.dma(lambda e: e.dma_start(out=t1_.ap, in_=I["hg_lower"][0].rearrange("d (t p) -> p d t", p=128), allow_slow_non_contiguous=True), [], t1_.k())
            S.dve(lambda e: e.tensor_tensor(out=lbp.ap, in0=lbp.ap, in1=t1_.ap, op=ALU.subtract), lbp.k() + t1_.k(), lbp.k())
            S.act(lambda e: e.activation(out=lbp.ap, in_=lbp.ap, func=AF.Sigmoid), lbp.k(), lbp.k())
            S.dve(lambda e: e.tensor_scalar(out=omp.ap, in0=lbp.ap, scalar1=-1.0, scalar2=1.0, op0=ALU.mult, op1=ALU.add), lbp.k(), omp.k())
    St = [mem.alloc([64], F32) for _ in range(nt)]
    Sb = [mem.alloc([64], BF16) for _ in range(nt)]
    qTb = [[mem.alloc([BLK], F32) for _ in range(nt)] for _ in range(2)]
    kTb = [[mem.alloc([BLK], F32) for _ in range(nt)] for _ in range(2)]
    nchb = BLK // C
    lgb = [mem.alloc([nchb, HD], F32, parts=C) for _ in range(2)]
    vb = [mem.alloc([nchb, 256], BF16, parts=C) for _ in range(2)]
    if kind == "gla":
        af1 = [mem.alloc([BLK], F32, parts=17) for _ in range(2)]
        lgt = [mem.alloc([128], F32) for _ in range(2)]
    E1 = [mem.alloc([C], F32) for _ in range(2)]
    E2 = [mem.alloc([C], F32) for _ in range(2)]
    qe = [mem.alloc([C], BF16) for _ in range(2)]
    ke = [mem.alloc([C], BF16) for _ in range(2)]
    qeb = [mem.alloc([hp, C], BF16) for _ in range(2)]
    keT = [mem.alloc([128], BF16, parts=C) for _ in range(2)]
    Am = [mem.alloc([hp * C], BF16, parts=C) for _ in range(2)]
    tmpS = [mem.alloc([64], F32) for _ in range(2)]
    tmp2 = [mem.alloc([hp, 64], F32) for _ in range(2)]
    osb = [mem.alloc([256], F32, parts=C) for _ in range(2)]
    cc = [0]
    bcnt = [0]
    ocnt = [0]
    for dr in range(2):
        for t in range(nt):
            S.dve(lambda e, t=t: e.memset(St[t].ap, 0.0), [], St[t].k())
            S.pool(lambda e, t=t: e.memset(Sb[t].ap, 0.0), [], Sb[t].k())
        blocks = [(0, TC)] + [(TC + i * BLK, BLK) for i in range(TL // BLK)]
        if dr == 1:
            blocks = [(0, TC)] + [(TC + i * BLK, BLK) for i in reversed(range(TL // BLK))]
        for (t0, bw) in blocks:
            bi = bcnt[0] % 2
            bcnt[0] += 1
            nch = bw // C
            qT, kT, lg, vv = qTb[bi], kTb[bi], lgb[bi], vb[bi]
            if kind == "gla":
                S.dma(lambda e, qT=qT, t0=t0, bw=bw: e.dma_start(out=qT[0].ap[:, 0:bw], in_=g.QK_GLA[0:128, t0:t0 + bw]),
                      dkeys_fm("QK_GLA", 0, t0, bw), qT[0].k())
                S.dma(lambda e, kT=kT, t0=t0, bw=bw: e.dma_start(out=kT[0].ap[:, 0:bw], in_=g.QK_GLA[128:256, t0:t0 + bw]),
                      dkeys_fm("QK_GLA", 1, t0, bw), kT[0].k())
                a1 = af1[bi]
                S.dma(lambda e, a1=a1, t0=t0, bw=bw, dr=dr: e.dma_start(out=a1.ap[0:16, 0:bw], in_=g.AFB[dr * 16:(dr + 1) * 16, t0:t0 + bw]),
                      dkeys_fm("AFB", 0, t0, bw), a1.k())
                S.dma(lambda e, a1=a1, bw=bw: e.dma_start(out=a1.ap[16:17, 0:bw], in_=I["ones_row"][0:1, 0:bw]), [], a1.k())
                for c in range(nch):
                    pb = cc[0] % 2
                    lt = lgt[cc[0] % 2]
                    S.pe(lambda e, a1=a1, c=c, pb=pb, dr=dr: e.matmul(g.PS[pb][:, 0:128], lhsT=a1.ap[:, c * 128:(c + 1) * 128],
                                                                       rhs=wa2b.ap[:, dr, :], start=True, stop=True),
                         a1.k() + wa2b.k(), g.psk[pb])
                    S.act(lambda e, lt=lt, pb=pb: e.activation(out=lt.ap, in_=g.PS[pb][:, 0:128], func=AF.Exp, scale=-1.0),
                          g.psk[pb], lt.k())
                    S.act(lambda e, lt=lt, lg=lg, c=c: e.activation(out=lg.ap[:, c, :], in_=lt.ap, func=AF.Ln, bias=1.0, scale=1.0),
                          lt.k(), lg.k(c * HD, (c + 1) * HD))
                    cc[0] += 1
                S.dma(lambda e, vv=vv, t0=t0, bw=bw, nch=nch: e.dma_start(out=vv.ap[:, 0:nch, :],
                                                                           in_=g.V_GLA[t0:t0 + bw, :].rearrange("(c s) e -> s c e", s=C)),
                      dkeys_tm("V_GLA", 0, t0, bw), vv.k())
            else:
                for t in range(nt):
                    S.dma(lambda e, qT=qT, t=t, t0=t0, bw=bw: e.dma_start(out=qT[t].ap[:, 0:bw], in_=g.Q_HG[t * 128:(t + 1) * 128, t0:t0 + bw]),
                          dkeys_fm("Q_HG", t, t0, bw), qT[t].k())
                    r0 = dr * 256 + t * 128
                    S.dma(lambda e, kT=kT, t=t, t0=t0, bw=bw, r0=r0: e.dma_start(out=kT[t].ap[:, 0:bw], in_=g.ZT_HG[r0:r0 + 128, t0:t0 + bw]),
                          dkeys_fm("ZT_HG", r0 // 128, t0, bw), kT[t].k())
                    S.act(lambda e, kT=kT, t=t, bw=bw: e.activation(out=kT[t].ap[:, 0:bw], in_=kT[t].ap[:, 0:bw], func=AF.Sigmoid, scale=-1.0),
                          kT[t].k(), kT[t].k())
                    S.dve(lambda e, kT=kT, t=t, bw=bw, dr=dr: e.tensor_scalar(out=kT[t].ap[:, 0:bw], in0=kT[t].ap[:, 0:bw],
                                                                               scalar1=omp.ap[:, dr, t:t + 1], scalar2=None, op0=ALU.mult),
                          kT[t].k() + omp.k(), kT[t].k())
                S.dma(lambda e, lg=lg, t0=t0, bw=bw, nch=nch, dr=dr: e.dma_start(out=lg.ap[:, 0:nch, :],
                      in_=g.Z_HG[t0:t0 + bw, dr * 256:(dr + 1) * 256].rearrange("(c s) e -> s c e", s=C)),
                      dkeys_tm("Z_HG", 0, t0, bw), lg.k())
                lgv = lg.ap[:, 0:nch, :]
                S.act(lambda e, lgv=lgv: e.activation(out=lgv, in_=lgv, func=AF.Sigmoid), lg.k(), lg.k())
                S.dve(lambda e, lgv=lgv, nch=nch, dr=dr: e.tensor_tensor(out=lgv, in0=lgv, in1=omb.ap[:, dr:dr + 1, :].broadcast_to([C, nch, 256]),
                                                                          op=ALU.mult), lg.k() + omb.k(), lg.k())
                S.dve(lambda e, lgv=lgv, nch=nch, dr=dr: e.tensor_tensor(out=lgv, in0=lgv, in1=lbb.ap[:, dr:dr + 1, :].broadcast_to([C, nch, 256]),
                                                                          op=ALU.add), lg.k() + lbb.k(), lg.k())
                S.dve(lambda e, lgv=lgv: e.tensor_scalar(out=lgv, in0=lgv, scalar1=1e-20, scalar2=None, op0=ALU.max), lg.k(), lg.k())
                S.act(lambda e, lgv=lgv: e.activation(out=lgv, in_=lgv, func=AF.Ln), lg.k(), lg.k())
                S.dma(lambda e, vv=vv, t0=t0, bw=bw, nch=nch: e.dma_start(out=vv.ap[:, 0:nch, :],
                                                                           in_=g.VG_HG[t0:t0 + bw, 0:256].rearrange("(c s) e -> s c e", s=C)),
                      dkeys_tm("VG_HG", 0, t0, bw), vv.k())
            chs = list(range(nch)) if dr == 0 else list(reversed(range(nch)))
            for c in chs:
                o_bank = 5 + ocnt[0] % 2
                ob = osb[ocnt[0] % 2]
                ocnt[0] += 1
                for t in range(nt):
                    i2 = cc[0] % 2
                    cc[0] += 1
                    e1, e2, qe_, ke_, qb_, kt_, am_, ts_, t2_ = E1[i2], E2[i2], qe[i2], ke[i2], qeb[i2], keT[i2], Am[i2], tmpS[i2], tmp2[i2]
                    bb = i2
                    ab = 3 + i2
                    S.pe(lambda e, lg=lg, c=c, t=t, bb=bb, dr=dr: e.matmul(g.PS[bb][:, 0:C], lhsT=lg.ap[:, c, t * 128:(t + 1) * 128],
                                                                            rhs=tri.ap[:, dr, :], start=True, stop=True),
                         lg.k(c * HD, (c + 1) * HD) + tri.k(), g.psk[bb])
                    S.act(lambda e, e1=e1, bb=bb: e.activation(out=e1.ap, in_=g.PS[bb][:, 0:C], func=AF.Exp), g.psk[bb], e1.k())
                    S.act(lambda e, e2=e2, bb=bb: e.activation(out=e2.ap, in_=g.PS[bb][:, 0:C], func=AF.Exp, scale=-1.0), g.psk[bb], e2.k())
                    S.dve(lambda e, qe_=qe_, e1=e1, qT=qT, t=t, c=c: e.tensor_tensor(out=qe_.ap, in0=qT[t].ap[:, c * C:(c + 1) * C], in1=e1.ap, op=ALU.mult),
                          qT[t].k() + e1.k(), qe_.k())
                    S.dve(lambda e, ke_=ke_, e2=e2, kT=kT, t=t, c=c: e.tensor_tensor(out=ke_.ap, in0=kT[t].ap[:, c * C:(c + 1) * C], in1=e2.ap, op=ALU.mult),
                          kT[t].k() + e2.k(), ke_.k())
                    S.pool(lambda e, qb_=qb_, qe_=qe_: e.tensor_tensor(out=qb_.ap, in0=qe_.ap[:, None, :].broadcast_to([128, hp, C]), in1=hm.ap, op=ALU.mult),
                           qe_.k() + hm.k(), qb_.k())
                    S.pe(lambda e, ke_=ke_: e.transpose(out=g.PSB[2][0:C, 0:128], in_=ke_.ap, identity=g.ident_bf.ap),
                         ke_.k() + g.ident_bf.k(), g.psk[2])
                    S.act(lambda e, kt_=kt_: e.activation(out=kt_.ap, in_=g.PSB[2][0:C, 0:128], func=AF.Copy), g.psk[2], kt_.k())
                    S.pe(lambda e, ke_=ke_, qb_=qb_, ab=ab: e.matmul(g.PS[ab][0:C, 0:hp * C], lhsT=ke_.ap, rhs=qb_.ap.rearrange("p h c -> p (h c)"),
                                                                       start=True, stop=True),
                         ke_.k() + qb_.k(), g.psk[ab])
                    S.dve(lambda e, am_=am_, ab=ab, dr=dr: e.tensor_tensor(out=am_.ap, in0=g.PS[ab][0:C, 0:hp * C], in1=msk.ap[:, dr, :], op=ALU.mult),
                          g.psk[ab] + msk.k(), am_.k())
                    for hh in range(hp):
                        hcol = (t * hp + hh) * 64
                        S.pe(lambda e, am_=am_, hh=hh, hcol=hcol, vv=vv, c=c, o_bank=o_bank:
                             e.matmul(g.PS[o_bank][0:C, hcol:hcol + 64], lhsT=am_.ap[:, hh * C:(hh + 1) * C], rhs=vv.ap[:, c, hcol:hcol + 64],
                                      start=True, stop=False),
                             am_.k() + vv.k(), g.psk[o_bank])
                        S.pe(lambda e, qb_=qb_, hh=hh, hcol=hcol, t=t, o_bank=o_bank:
                             e.matmul(g.PS[o_bank][0:C, hcol:hcol + 64], lhsT=qb_.ap[:, hh, :], rhs=Sb[t].ap,
                                      start=False, stop=True),
                             qb_.k() + Sb[t].k(), g.psk[o_bank])
                    S.pe(lambda e, kt_=kt_, vv=vv, c=c, t=t: e.matmul(g.PS[7][:, 0:hp * 64], lhsT=kt_.ap, rhs=vv.ap[:, c, t * hp * 64:(t + 1) * hp * 64],
                                                                       start=True, stop=True),
                         kt_.k() + vv.k(), g.psk[7])
                    ecol = (C - 1) if dr == 0 else 0
                    S.dve(lambda e, ts_=ts_, t=t, e1=e1, ecol=ecol: e.tensor_scalar(out=ts_.ap, in0=St[t].ap, scalar1=e1.ap[:, ecol:ecol + 1], scalar2=None, op0=ALU.mult),
                          St[t].k() + e1.k(), ts_.k())
                    S.dve(lambda e, t2_=t2_: e.tensor_tensor(out=t2_.ap.rearrange("p h e -> p (h e)"), in0=g.PS[7][:, 0:hp * 64], in1=bd.ap.rearrange("p h e -> p (h e)"), op=ALU.mult),
                          g.psk[7] + bd.k(), t2_.k())
                    for hh in range(hp):
                        src1 = ts_ if hh == 0 else St[t]
                        S.dve(lambda e, t2_=t2_, hh=hh, t=t, e1=e1, ecol=ecol, src1=src1:
                              e.scalar_tensor_tensor(out=St[t].ap, in0=t2_.ap[:, hh, :], scalar=e1.ap[:, ecol:ecol + 1], in1=src1.ap, op0=ALU.mult, op1=ALU.add),
                              t2_.k() + e1.k() + src1.k() + St[t].k(), St[t].k())
                    S.act(lambda e, t=t: e.activation(out=Sb[t].ap, in_=St[t].ap, func=AF.Copy), St[t].k(), Sb[t].k())
                S.dve(lambda e, ob=ob, o_bank=o_bank: e.tensor_copy(out=ob.ap, in_=g.PS[o_bank][0:C, 0:256]), g.psk[o_bank], ob.k())
                tt = t0 + c * C
                S.dma(lambda e, ob=ob, tt=tt, dr=dr: e.dma_start(out=O[dr, tt:tt + C, :], in_=ob.ap), ob.k(), [(okn, dr, u) for u in range(tt // 32, (tt + C) // 32)])
    mem.release(m0)


def dkeys_fm(name, rowtile, t0, bw):
    return [(name, rowtile, s) for s in range(t0 // 512, (t0 + bw - 1) // 512 + 1)]


def dkeys_tm(name, dc0, t0, bw):
    return [(name, i, dc0) for i in range(t0 // 128, (t0 + bw - 1) // 128 + 1)]


def stage_branch_norm(g, l, b, Osrc, okn, ndir, gate, gate_c0, gkn, gname, cscale, br, tok_lo=0):
    nc, S, mem, I = g.nc, g.S, g.mem, g.I
    if isinstance(Osrc, str):
        Osrc = getattr(g, Osrc)
    if isinstance(gate, str):
        gate = getattr(g, gate)
    m0 = mem.mark()
    gbc = mem.alloc([64], F32)
    S.dma(lambda e: e.dma_start(out=gbc.ap, in_=I[gname][l:l + 1, :].partition_broadcast(128)), [], gbc.k())
    if cscale != 1.0:
        S.dve(lambda e: e.tensor_scalar(out=gbc.ap, in0=gbc.ap, scalar1=float(cscale), scalar2=None, op0=ALU.mult), gbc.k(), gbc.k())
    of = [mem.alloc([256], F32) for _ in range(2)]
    ob = [mem.alloc([256], F32) for _ in range(2)]
    gt = [mem.alloc([256], BF16) for _ in range(2)]
    sq = [mem.alloc([256], F32) for _ in range(2)]
    ss = [mem.alloc([8], F32) for _ in range(2)]
    yb = [mem.alloc([256], BF16) for _ in range(2)]
    stg = [mem.alloc([2, 128], BF16) for _ in range(2)]
    for i in range(tok_lo // 128, TT // 128):
        t0 = i * 128
        u = i % 2
        o1, o2, g_, sq_, ss_, y_, st_ = of[u], ob[u], gt[u], sq[u], ss[u], yb[u], stg[u]
        okeys = lambda dr: [(okn, dr, v) for v in range(t0 // 32, t0 // 32 + 4)]
        S.dma(lambda e, o1=o1, t0=t0: e.dma_start(out=o1.ap, in_=Osrc[0, t0:t0 + 128, :]), okeys(0), o1.k())
        if ndir == 2:
            S.dma(lambda e, o2=o2, t0=t0: e.dma_start(out=o2.ap, in_=Osrc[1, t0:t0 + 128, :]), okeys(1), o2.k())
            S.pool(lambda e, o1=o1, o2=o2: e.tensor_tensor(out=o1.ap, in0=o1.ap, in1=o2.ap, op=ALU.add), o1.k() + o2.k(), o1.k())
        if gate is not None:
            S.dma(lambda e, g_=g_, t0=t0: e.dma_start(out=g_.ap, in_=gate[t0:t0 + 128, gate_c0:gate_c0 + 256]), [(gkn, i, gate_c0)], g_.k())
        S.dve(lambda e, o1=o1, sq_=sq_: e.tensor_tensor(out=sq_.ap, in0=o1.ap, in1=o1.ap, op=ALU.mult), o1.k(), sq_.k())
        S.dve(lambda e, sq_=sq_, ss_=ss_: e.tensor_reduce(out=ss_.ap[:, 0:4], in_=sq_.ap.rearrange("p (h e) -> p h e", h=4), axis=AX.X, op=ALU.add),
              sq_.k(), ss_.k())
        S.act(lambda e, ss_=ss_: e.activation(out=ss_.ap[:, 4:8], in_=ss_.ap[:, 0:4], func=AF.Sqrt, bias=EPS, scale=1.0 / 64), ss_.k(), ss_.k())
        S.dve(lambda e, ss_=ss_: e.reciprocal(out=ss_.ap[:, 4:8], in_=ss_.ap[:, 4:8]), ss_.k(), ss_.k())
        S.dve(lambda e, o1=o1, ss_=ss_, sq_=sq_: e.tensor_tensor(out=sq_.ap.rearrange("p (h e) -> p h e", h=4), in0=o1.ap.rearrange("p (h e) -> p h e", h=4),
                                                                in1=ss_.ap[:, 4:8, None].broadcast_to([128, 4, 64]), op=ALU.mult),
              o1.k() + ss_.k(), sq_.k())
        if gate is not None:
            S.pool(lambda e, sq_=sq_: e.tensor_tensor(out=sq_.ap.rearrange("p (h e) -> p h e", h=4), in0=sq_.ap.rearrange("p (h e) -> p h e", h=4),
                                                     in1=gbc.ap[:, None, :].broadcast_to([128, 4, 64]), op=ALU.mult), sq_.k() + gbc.k(), sq_.k())
            S.dve(lambda e, sq_=sq_, g_=g_, y_=y_: e.tensor_tensor(out=y_.ap, in0=sq_.ap, in1=g_.ap, op=ALU.mult), sq_.k() + g_.k(), y_.k())
        else:
            S.pool(lambda e, sq_=sq_, y_=y_: e.tensor_tensor(out=y_.ap.rearrange("p (h e) -> p h e", h=4), in0=sq_.ap.rearrange("p (h e) -> p h e", h=4),
                                                            in1=gbc.ap[:, None, :].broadcast_to([128, 4, 64]), op=ALU.mult), sq_.k() + gbc.k(), y_.k())
        bank = 2 + u
        for c in range(2):
            S.pe(lambda e, y_=y_, c=c, bank=bank: e.transpose(out=g.PSB[bank][:, c * 128:(c + 1) * 128], in_=y_.ap[:, c * 128:(c + 1) * 128],
                                                              identity=g.ident_bf.ap), y_.k() + g.ident_bf.k(), g.psk[bank])
        S.act(lambda e, st_=st_, bank=bank: e.activation(out=st_.ap, in_=g.PSB[bank][:, 0:256].rearrange("p (c t) -> p c t", c=2), func=AF.Copy),
              g.psk[bank], st_.k())
        S.dma(lambda e, st_=st_, t0=t0: e.dma_start(out=g.BRT[br].rearrange("(c p) t -> p c t", p=128)[:, :, t0:t0 + 128], in_=st_.ap),
              st_.k(), [("BRT", br, i)])
    mem.release(m0)


def stage_merge(g, l, b, tok_lo=0):
    nc, S, mem, I = g.nc, g.S, g.mem, g.I
    m0 = mem.mark()
    wb = mem.alloc([4, 2, D], BF16)
    for i in range(4):
        S.dma(lambda e, i=i: e.dma_start(out=wb.ap[:, i], in_=I["w_branch"][l, i].rearrange("(k p) n -> p k n", p=128)), [], wb.k(), q="pq")
    wo = mem.alloc([8, D], BF16)
    S.dma(lambda e: e.dma_start(out=wo.ap, in_=I["w_out"][l].rearrange("(k p) n -> p k n", p=128)), [], wo.k(), q="pq")
    mT = g.hT
    brt = [mem.alloc([4, 2, 512], BF16) for _ in range(2)]
    gts = [mem.alloc([512], BF16) for _ in range(4)]
    tm = [mem.alloc([512], F32) for _ in range(4)]
    slabs = [(s * 512, min(512, TT - s * 512)) for s in range((TT + 511) // 512) if s * 512 + 512 > tok_lo]
    gc = [0]
    for si, (t0, tw) in enumerate(slabs):
        bt = brt[si % 2]
        for i in range(4):
            S.dma(lambda e, bt=bt, i=i, t0=t0, tw=tw: e.dma_start(out=bt.ap[:, i, :, 0:tw], in_=g.BRT[i].rearrange("(c p) t -> p c t", p=128)[:, :, t0:t0 + tw]),
                  [("BRT", i, v) for v in range(t0 // 128, (t0 + tw) // 128)], bt.k())
        for c in range(8):
            for i in range(4):
                bank = 4 + i
                for kc in range(2):
                    S.pe(lambda e, bt=bt, i=i, kc=kc, c=c, bank=bank, tw=tw: e.matmul(g.PS[bank][:, 0:tw], lhsT=wb.ap[:, i, kc, c * 128:(c + 1) * 128],
                                                                                     rhs=bt.ap[:, i, kc, 0:tw], start=(kc == 0), stop=(kc == 1)),
                         wb.k() + bt.k(), g.psk[bank])
                gt = gts[gc[0] % 4]
                gc[0] += 1
                r0 = i * 1024 + c * 128
                S.dma(lambda e, gt=gt, r0=r0, t0=t0, tw=tw: e.dma_start(out=gt.ap[:, 0:tw], in_=g.MG[r0:r0 + 128, t0:t0 + tw]),
                      [("MG", r0 // 128, t0 // 512)], gt.k())
                S.dve(lambda e, i=i, gt=gt, bank=bank, tw=tw: e.tensor_tensor(out=tm[i].ap[:, 0:tw], in0=g.PS[bank][:, 0:tw], in1=gt.ap[:, 0:tw], op=ALU.mult),
                      g.psk[bank] + gt.k(), tm[i].k())
            S.pool(lambda e, tw=tw: e.tensor_tensor(out=tm[0].ap[:, 0:tw], in0=tm[0].ap[:, 0:tw], in1=tm[1].ap[:, 0:tw], op=ALU.add), tm[0].k() + tm[1].k(), tm[0].k())
            S.pool(lambda e, tw=tw: e.tensor_tensor(out=tm[2].ap[:, 0:tw], in0=tm[2].ap[:, 0:tw], in1=tm[3].ap[:, 0:tw], op=ALU.add), tm[2].k() + tm[3].k(), tm[2].k())
            S.pool(lambda e, c=c, t0=t0, tw=tw: e.tensor_tensor(out=mT.ap[:, c, t0:t0 + tw], in0=tm[0].ap[:, 0:tw], in1=tm[2].ap[:, 0:tw], op=ALU.add),
                   tm[0].k() + tm[2].k(), mT.k(c * TT + t0, c * TT + t0 + tw))
    gbc = [mem.alloc([D], F32) for _ in range(2)]
    load_mod_bc(g, l, 2, 2, gbc[0])
    load_mod_bc(g, l, b, 2, gbc[1])
    xr = [mem.alloc([D], F32) for _ in range(2)]
    tp = [mem.alloc([D], F32) for _ in range(2)]
    for i in range(tok_lo // 128, TT // 128):
        t0 = i * 128
        u = i % 2
        isc = t0 < TC
        gb = gbc[0] if isc else gbc[1]
        if isc:
            dst, xk = g.XC[b, t0:t0 + 128, :], [(("XC", b), i)]
        else:
            dst, xk = g.OUT[b, t0 - TC:t0 - TC + 128, :], [(("XX", b), (t0 - TC) // 128)]
        x_, t_ = xr[u], tp[u]
        S.dma(lambda e, x_=x_, dst=dst: e.dma_start(out=x_.ap, in_=dst), xk, x_.k())
        mk = []
        for k in range(8):
            mk += mT.k(k * TT + t0, k * TT + t0 + 128)
        for hf in range(2):
            bank = 2 * u + hf
            for k in range(8):
                S.pe(lambda e, k=k, hf=hf, bank=bank, t0=t0: e.matmul(g.PS[bank], lhsT=mT.ap[:, k, t0:t0 + 128], rhs=wo.ap[:, k, hf * 512:(hf + 1) * 512],
                                                                      start=(k == 0), stop=(k == 7)), mk + wo.k(), g.psk[bank])
            S.dve(lambda e, t_=t_, hf=hf, bank=bank, gb=gb: e.tensor_tensor(out=t_.ap[:, hf * 512:(hf + 1) * 512], in0=g.PS[bank], in1=gb.ap[:, hf * 512:(hf + 1) * 512], op=ALU.mult),
                  g.psk[bank] + gb.k(), t_.k(hf * 512, (hf + 1) * 512))
        S.pool(lambda e, x_=x_, t_=t_: e.tensor_tensor(out=x_.ap, in0=x_.ap, in1=t_.ap, op=ALU.add), x_.k() + t_.k(), x_.k())
        S.dma(lambda e, x_=x_, dst=dst: e.dma_start(out=dst, in_=x_.ap), x_.k(), xk)
    mem.release(m0)


def bound_reg(g, e, bound):
    if not hasattr(g, "_bregs"):
        g._bregs = {}
    if bound not in g._bregs:
        g._bregs[bound] = e.to_reg(bound)
    return g._bregs[bound]


def stage_moe(g, l, b, do_ctx):
    nc, S, mem, I = g.nc, g.S, g.mem, g.I
    m0 = mem.mark()
    segs = [("x", TC, TL, 512, b, b * TL)]
    if do_ctx:
        segs.append(("c", 0, TC, 32, 2, b * TC))
    rw = mem.alloc([8, 16], BF16)
    S.dma(lambda e: e.dma_start(out=rw.ap, in_=I["moe_router"][l].rearrange("(k p) n -> p k n", p=128)), [], rw.k(), q="pq")
    NS = 5
    idxg = mem.alloc([NS, 16], I32)
    idxs = mem.alloc([NS, 16], I32)
    gT = mem.alloc([NS, 16], F32)
    S.dve(lambda e: e.memset(idxg.ap, 0), [], idxg.k())
    S.dve(lambda e: e.memset(idxs.ap, 0), [], idxs.k())
    m1 = mem.mark()
    affT = mem.alloc([TT], F32, parts=16)
    vals = mem.alloc([512], F32, parts=16)
    idx = mem.alloc([512], U32, parts=16)
    idxf = mem.alloc([512], F32, parts=16)
    sm = [mem.alloc([4], F32) for _ in range(2)]
    ex = [mem.alloc([16], F32) for _ in range(2)]
    tlo = 0 if do_ctx else TC
    for i in range(tlo // 128, TT // 128):
        t0 = i * 128
        u = i % 2
        s_, e_ = sm[u], ex[u]
        hk = []
        for k in range(8):
            hk += g.hT.k(k * TT + t0, k * TT + t0 + 128)
        for k in range(8):
            S.pe(lambda e, k=k, u=u, t0=t0: e.matmul(g.PS[u][:, 0:16], lhsT=g.hT.ap[:, k, t0:t0 + 128], rhs=rw.ap[:, k, :], start=(k == 0), stop=(k == 7)),
                 hk + rw.k(), g.psk[u])
        S.dve(lambda e, s_=s_, u=u: e.tensor_reduce(out=s_.ap[:, 0:1], in_=g.PS[u][:, 0:16], axis=AX.X, op=ALU.max), g.psk[u], s_.k())
        S.dve(lambda e, s_=s_: e.tensor_scalar(out=s_.ap[:, 1:2], in0=s_.ap[:, 0:1], scalar1=-1.0, scalar2=None, op0=ALU.mult), s_.k(), s_.k())
        S.act(lambda e, s_=s_, e_=e_, u=u: e.activation(out=e_.ap, in_=g.PS[u][:, 0:16], func=AF.Exp, bias=s_.ap[:, 1:2], scale=1.0, accum_out=s_.ap[:, 2:3]),
              g.psk[u] + s_.k(), e_.k() + s_.k())
        S.dve(lambda e, s_=s_: e.reciprocal(out=s_.ap[:, 3:4], in_=s_.ap[:, 2:3]), s_.k(), s_.k())
        S.dve(lambda e, s_=s_, e_=e_: e.tensor_scalar(out=e_.ap, in0=e_.ap, scalar1=s_.ap[:, 3:4], scalar2=None, op0=ALU.mult), e_.k() + s_.k(), e_.k())
        S.pe(lambda e, e_=e_: e.transpose(out=g.PS[2][0:16, 0:128], in_=e_.ap, identity=g.ident_f.ap), e_.k() + g.ident_f.k(), g.psk[2])
        S.act(lambda e, t0=t0: e.activation(out=affT.ap[:, t0:t0 + 128], in_=g.PS[2][0:16, 0:128], func=AF.Copy), g.psk[2], affT.k(t0, t0 + 128))
    for (sg, s0, slen, cap, jm, soff) in segs:
        work = affT.ap[:, s0:s0 + slen]
        wk = affT.k(s0, s0 + slen)
        for r in range(cap // 8):
            S.dve(lambda e, r=r, work=work: e.max(out=vals.ap[:, r * 8:(r + 1) * 8], in_=work), wk, vals.k(r * 8, (r + 1) * 8))
            S.dve(lambda e, r=r, work=work: e.max_index(out=idx.ap[:, r * 8:(r + 1) * 8], in_max=vals.ap[:, r * 8:(r + 1) * 8], in_values=work),
                  wk + vals.k(r * 8, (r + 1) * 8), idx.k(r * 8, (r + 1) * 8))
            S.dve(lambda e, r=r, work=work: e.match_replace(out=work, in_to_replace=vals.ap[:, r * 8:(r + 1) * 8], in_values=work, imm_value=-1.0),
                  wk + vals.k(r * 8, (r + 1) * 8), wk)
        S.dve(lambda e, cap=cap: e.tensor_copy(out=idxf.ap[:, 0:cap], in_=idx.ap[:, 0:cap]), idx.k(), idxf.k())
        nblk = (cap + 127) // 128
        for jb in range(nblk):
            ns = min(128, cap - jb * 128)
            j = jb if sg == "x" else 4
            S.pe(lambda e, jb=jb, ns=ns: e.transpose(out=g.PS[2][0:ns, 0:16], in_=idxf.ap[:, jb * 128:jb * 128 + ns], identity=g.ident_f.ap[0:16, 0:16]),
                 idxf.k() + g.ident_f.k(), g.psk[2])
            S.dve(lambda e, j=j, ns=ns, s0=s0: e.tensor_scalar(out=idxg.ap[0:ns, j, :], in0=g.PS[2][0:ns, 0:16], scalar1=float(s0), scalar2=None, op0=ALU.add),
                  g.psk[2], idxg.k())
            S.dve(lambda e, j=j, ns=ns, soff=soff: e.tensor_scalar(out=idxs.ap[0:ns, j, :], in0=g.PS[2][0:ns, 0:16], scalar1=float(soff), scalar2=None, op0=ALU.add),
                  g.psk[2], idxs.k())
            S.pe(lambda e, jb=jb, ns=ns: e.transpose(out=g.PS[3][0:ns, 0:16], in_=vals.ap[:, jb * 128:jb * 128 + ns], identity=g.ident_f.ap[0:16, 0:16]),
                 vals.k() + g.ident_f.k(), g.psk[3])
            S.act(lambda e, j=j, ns=ns: e.activation(out=gT.ap[0:ns, j, :], in_=g.PS[3][0:ns, 0:16], func=AF.Copy), g.psk[3], gT.k())
    mem.release(m1)
    wbuf = [mem.alloc([8, D], BF16) for _ in range(4)]
    wcnt = [0]
    NSL = 544
    xg = [mem.alloc([D], BF16) for _ in range(2)]
    xgT = mem.alloc([8, NSL], BF16)
    uT = mem.alloc([8, NSL], BF16)
    tmpa = [mem.alloc([512], F32) for _ in range(2)]
    ysb = [mem.alloc([D], F32) for _ in range(2)]
    mbc = {}
    for (sg, s0, slen, cap, jm, soff) in segs:
        mbc[sg] = mem.alloc([D], F32)
        load_mod_bc(g, l, jm, 5, mbc[sg])
    outflat = g.OUT.rearrange("a t d -> (a t) d")
    xcflat = g.XC.rearrange("a t d -> (a t) d")
    xxkeys = dkeys(("XX", b), 0, TL, 128)
    xckeys = dkeys(("XC", b), 0, TC, 128)
    htk = [("HTOK", i) for i in range(TT // 128)]

    def next_w(src):
        w = wbuf[wcnt[0] % 4]
        wcnt[0] += 1
        S.dma(lambda e: e.dma_start(out=w.ap, in_=src.rearrange("(k p) n -> p k n", p=128)), [], w.k(), q="pq")
        return w

    blocks = [("x", j, 128, j * 128) for j in range(4)]
    slabs = [(0, 512)]
    if do_ctx:
        blocks.append(("c", 4, 32, 512))
        slabs.append((512, 32))
    gcnt = [0]
    for ex_ in range(16):
        w1 = next_w(I["moe_w1"][l, ex_])
        w3 = next_w(I["moe_w3"][l, ex_])
        for (sg, j, ns, col0) in blocks:
            xg_ = xg[gcnt[0] % 2]
            gcnt[0] += 1
            S.dma(lambda e, xg_=xg_, j=j, ns=ns, ex_=ex_: e.indirect_dma_start(out=xg_.ap[0:ns, :], out_offset=None, in_=g.HTOK,
                                                                               in_offset=bass.IndirectOffsetOnAxis(ap=idxg.ap[0:ns, j, ex_:ex_ + 1], axis=0)),
                  idxg.k() + htk, xg_.k(), q="pq")
            for c in range(8):
                S.pe(lambda e, xg_=xg_, c=c, ns=ns: e.transpose(out=g.PSB[3][:, c * 128:c * 128 + ns], in_=xg_.ap[0:ns, c * 128:(c + 1) * 128],
                                                                identity=g.ident_bf.ap[0:ns, 0:ns]), xg_.k() + g.ident_bf.k(), g.psk[3])
            S.act(lambda e, ns=ns, col0=col0: e.activation(out=xgT.ap[:, :, col0:col0 + ns], in_=g.PSB[3].rearrange("p (c t) -> p c t", c=8)[:, :, 0:ns], func=AF.Copy),
                  g.psk[3], xgT.k())
        for fc in range(8):
            for (c0, cw) in slabs:
                ba, bb_ = (4, 5) if c0 == 0 else (2, 2)
                oa, ob_ = (0, 0) if c0 == 0 else (0, 64)
                for k in range(8):
                    S.pe(lambda e, k=k, fc=fc, c0=c0, cw=cw, ba=ba, oa=oa, w1=w1: e.matmul(g.PS[ba][:, oa:oa + cw], lhsT=w1.ap[:, k, fc * 128:(fc + 1) * 128],
                                                                                       rhs=xgT.ap[:, k, c0:c0 + cw], start=(k == 0), stop=(k == 7)),
                         w1.k() + xgT.k(), g.psk[ba])
                ta = tmpa[fc % 2]
                S.act(lambda e, ta=ta, ba=ba, oa=oa, cw=cw: e.activation(out=ta.ap[:, 0:cw], in_=g.PS[ba][:, oa:oa + cw], func=AF.Silu), g.psk[ba], ta.k())
                for k in range(8):
                    S.pe(lambda e, k=k, fc=fc, c0=c0, cw=cw, bb_=bb_, ob_=ob_, w3=w3: e.matmul(g.PS[bb_][:, ob_:ob_ + cw], lhsT=w3.ap[:, k, fc * 128:(fc + 1) * 128],
                                                                                            rhs=xgT.ap[:, k, c0:c0 + cw], start=(k == 0), stop=(k == 7)),
                         w3.k() + xgT.k(), g.psk[bb_])
                S.dve(lambda e, ta=ta, bb_=bb_, ob_=ob_, cw=cw, fc=fc, c0=c0: e.tensor_tensor(out=uT.ap[:, fc, c0:c0 + cw], in0=ta.ap[:, 0:cw], in1=g.PS[bb_][:, ob_:ob_ + cw], op=ALU.mult),
                      ta.k() + g.psk[bb_], uT.k(fc * NSL + c0, fc * NSL + c0 + cw))
        w2 = next_w(I["moe_w2"][l, ex_])
        for (sg, j, ns, col0) in blocks:
            y_ = ysb[j % 2]
            for hf in range(2):
                bank = 6 + hf
                for fc in range(8):
                    S.pe(lambda e, fc=fc, hf=hf, bank=bank, ns=ns, col0=col0, w2=w2: e.matmul(g.PS[bank][0:ns, :], lhsT=uT.ap[:, fc, col0:col0 + ns],
                                                                                         rhs=w2.ap[:, fc, hf * 512:(hf + 1) * 512], start=(fc == 0), stop=(fc == 7)),
                         uT.k() + w2.k(), g.psk[bank])
                S.dve(lambda e, y_=y_, hf=hf, bank=bank, ns=ns, j=j, ex_=ex_, sg=sg: e.scalar_tensor_tensor(out=y_.ap[0:ns, hf * 512:(hf + 1) * 512], in0=g.PS[bank][0:ns, :],
                      scalar=gT.ap[0:ns, j, ex_:ex_ + 1], in1=mbc[sg].ap[0:ns, hf * 512:(hf + 1) * 512], op0=ALU.mult, op1=ALU.mult),
                      g.psk[bank] + gT.k() + mbc[sg].k(), y_.k(hf * 512, (hf + 1) * 512))
            dstf, dkk, bound = (outflat, xxkeys, NB * TL - 1) if sg == "x" else (xcflat, xckeys, NB * TC - 1)
            S.dma(lambda e, y_=y_, ns=ns, j=j, ex_=ex_, dstf=dstf, bound=bound: e.indirect_dma_start(
                out=dstf[:, :], out_offset=bass.IndirectOffsetOnAxis(ap=idxs.ap[0:ns, j, ex_:ex_ + 1], axis=0), in_=y_.ap[0:ns, :], in_offset=None,
                bounds_check=bound_reg(g, e, bound), oob_is_err=True, compute_op=ALU.add), y_.k() + idxs.k() + dkk, dkk, q="pq")
    mem.release(m0)


def stage_da(g, l, b, do_ctx):
    nc, S, mem, I = g.nc, g.S, g.mem, g.I
    lam_init = 0.8 - 0.6 * math.exp(-0.3 * l)
    m0 = mem.mark()
    qT = mem.alloc([2, TT], BF16)
    kT = mem.alloc([2, TT], BF16)
    v1 = mem.alloc([TT // 128, 4, 65], BF16)
    gm4 = mem.alloc([4], F32)
    S.dma(lambda e: e.dma_start(out=gm4.ap, in_=I["gm4"]), [], gm4.k())
    S.dve(lambda e: e.memset(v1.ap, 1.0), [], v1.k())
    lamt = mem.alloc([4, 32], F32)
    lams = mem.alloc([8], F32)
    S.dma(lambda e: e.dma_start(out=lamt.ap, in_=I["da_lam"][l:l + 1].rearrange("o a d -> o (a d)").partition_broadcast(128)
                                .rearrange("p o (a d) -> p (o a) d", a=4)), [], lamt.k())
    S.dve(lambda e: e.tensor_tensor(out=lamt.ap[:, 0, :], in0=lamt.ap[:, 0, :], in1=lamt.ap[:, 1, :], op=ALU.mult), lamt.k(), lamt.k())
    S.dve(lambda e: e.tensor_tensor(out=lamt.ap[:, 2, :], in0=lamt.ap[:, 2, :], in1=lamt.ap[:, 3, :], op=ALU.mult), lamt.k(), lamt.k())
    S.dve(lambda e: e.tensor_reduce(out=lams.ap[:, 0:1], in_=lamt.ap[:, 0, :], axis=AX.X, op=ALU.add), lamt.k(), lams.k())
    S.dve(lambda e: e.tensor_reduce(out=lams.ap[:, 1:2], in_=lamt.ap[:, 2, :], axis=AX.X, op=ALU.add), lamt.k(), lams.k())
    S.act(lambda e: e.activation(out=lams.ap[:, 2:4], in_=lams.ap[:, 0:2], func=AF.Exp), lams.k(), lams.k())
    S.dve(lambda e: e.tensor_tensor(out=lams.ap[:, 4:5], in0=lams.ap[:, 3:4], in1=lams.ap[:, 2:3], op=ALU.subtract), lams.k(), lams.k())
    S.dve(lambda e: e.tensor_scalar(out=lams.ap[:, 4:5], in0=lams.ap[:, 4:5], scalar1=-float(lam_init), scalar2=None, op0=ALU.add), lams.k(), lams.k())
    m1 = mem.mark()
    gqk = mem.alloc([2, 32], F32)
    S.dma(lambda e: e.dma_start(out=gqk.ap[:, 0, :], in_=I["da_qnorm_g"][l:l + 1, :].partition_broadcast(128)), [], gqk.k())
    S.dma(lambda e: e.dma_start(out=gqk.ap[:, 1, :], in_=I["da_knorm_g"][l:l + 1, :].partition_broadcast(128)), [], gqk.k())
    qk = [mem.alloc([512], F32) for _ in range(2)]
    sq = [mem.alloc([512], F32) for _ in range(2)]
    ss = [mem.alloc([32], F32) for _ in range(2)]
    rp = [mem.alloc([2, 2, 8], F32) for _ in range(2)]
    ra = [mem.alloc([16, 2, 8], F32) for _ in range(2)]
    rb = [mem.alloc([16, 2, 8], F32) for _ in range(2)]
    qb = [mem.alloc([512], BF16) for _ in range(2)]
    for i in range(TT // 128):
        t0 = i * 128
        u = i % 2
        q_, s_, ss_, rp_, ra_, rb_, qb_ = qk[u], sq[u], ss[u], rp[u], ra[u], rb[u], qb[u]
        S.dma(lambda e, q_=q_, t0=t0: e.dma_start(out=q_.ap, in_=g.QK_DA[t0:t0 + 128, :]), [("QK_DA", i, 0)], q_.k())
        S.dma(lambda e, t0=t0, i=i: e.dma_start(out=v1.ap[:, i, :, 0:64], in_=g.V_DA[t0:t0 + 128, :].rearrange("p (h e) -> p h e", h=4)),
              [("V_DA", i, 0)], v1.k())
        S.dve(lambda e, q_=q_, s_=s_: e.tensor_tensor(out=s_.ap, in0=q_.ap, in1=q_.ap, op=ALU.mult), q_.k(), s_.k())
        S.dve(lambda e, s_=s_, ss_=ss_: e.tensor_reduce(out=ss_.ap[:, 0:16], in_=s_.ap.rearrange("p (g d) -> p g d", g=16), axis=AX.X, op=ALU.add), s_.k(), ss_.k())
        S.act(lambda e, ss_=ss_: e.activation(out=ss_.ap[:, 16:32], in_=ss_.ap[:, 0:16], func=AF.Sqrt, bias=EPS, scale=1.0 / 32), ss_.k(), ss_.k())
        S.dve(lambda e, ss_=ss_: e.reciprocal(out=ss_.ap[:, 16:32], in_=ss_.ap[:, 16:32]), ss_.k(), ss_.k())
        S.dve(lambda e, q_=q_, ss_=ss_, s_=s_: e.tensor_tensor(out=s_.ap.rearrange("p (g d) -> p g d", g=16), in0=q_.ap.rearrange("p (g d) -> p g d", g=16),
                                                               in1=ss_.ap[:, 16:32, None].broadcast_to([128, 16, 32]), op=ALU.mult), q_.k() + ss_.k(), s_.k())
        for hf in range(2):
            S.pool(lambda e, s_=s_, hf=hf: e.tensor_tensor(out=s_.ap[:, hf * 256:(hf + 1) * 256].rearrange("p (g d) -> p g d", g=8),
                                                          in0=s_.ap[:, hf * 256:(hf + 1) * 256].rearrange("p (g d) -> p g d", g=8),
                                                          in1=gqk.ap[:, hf:hf + 1, :].broadcast_to([128, 8, 32]), op=ALU.mult), s_.k() + gqk.k(), s_.k())
        if t0 >= TC:
            S.dma(lambda e, rp_=rp_, t0=t0: e.dma_start(out=rp_.ap.rearrange("p a b c -> p (a b c)"), in_=I["rope"][t0 - TC:t0 - TC + 128, :]), [], rp_.k())
            xv = s_.ap.rearrange("p (g a h e) -> p g a h e", g=16, a=2, h=2)
            x1, x2 = xv[:, :, :, 0, :], xv[:, :, :, 1, :]
            cosb = rp_.ap[:, 0:1, :, :].broadcast_to([128, 16, 2, 8])
            sinb = rp_.ap[:, 1:2, :, :].broadcast_to([128, 16, 2, 8])
            qv = qb_.ap.rearrange("p (g a h e) -> p g a h e", g=16, a=2, h=2)
            S.dve(lambda e, ra_=ra_, x1=x1, cosb=cosb: e.tensor_tensor(out=ra_.ap, in0=x1, in1=cosb, op=ALU.mult), s_.k() + rp_.k(), ra_.k())
            S.pool(lambda e, rb_=rb_, x2=x2, sinb=sinb: e.tensor_tensor(out=rb_.ap, in0=x2, in1=sinb, op=ALU.mult), s_.k() + rp_.k(), rb_.k())
            S.dve(lambda e, ra_=ra_, rb_=rb_, qv=qv: e.tensor_tensor(out=qv[:, :, :, 0, :], in0=ra_.ap, in1=rb_.ap, op=ALU.subtract), ra_.k() + rb_.k(), qb_.k())
            S.dve(lambda e, ra_=ra_, x1=x1, sinb=sinb: e.tensor_tensor(out=ra_.ap, in0=x1, in1=sinb, op=ALU.mult), s_.k() + rp_.k() + qb_.k(), ra_.k())
            S.pool(lambda e, rb_=rb_, x2=x2, cosb=cosb: e.tensor_tensor(out=rb_.ap, in0=x2, in1=cosb, op=ALU.mult), s_.k() + rp_.k() + qb_.k(), rb_.k())
            S.dve(lambda e, ra_=ra_, rb_=rb_, qv=qv: e.tensor_tensor(out=qv[:, :, :, 1, :], in0=ra_.ap, in1=rb_.ap, op=ALU.add), ra_.k() + rb_.k(), qb_.k())
        else:
            S.dve(lambda e, s_=s_, qb_=qb_: e.tensor_copy(out=qb_.ap, in_=s_.ap), s_.k(), qb_.k())
        bank = 2 + u
        for c in range(4):
            S.pe(lambda e, qb_=qb_, c=c, bank=bank: e.transpose(out=g.PSB[bank][:, c * 128:(c + 1) * 128], in_=qb_.ap[:, c * 128:(c + 1) * 128],
                                                                identity=g.ident_bf.ap), qb_.k() + g.ident_bf.k(), g.psk[bank])
        S.act(lambda e, bank=bank, t0=t0: e.activation(out=qT.ap[:, :, t0:t0 + 128], in_=g.PSB[bank][:, 0:256].rearrange("p (c t) -> p c t", c=2), func=AF.Copy),
              g.psk[bank], qT.k())
        S.dve(lambda e, bank=bank, t0=t0: e.tensor_copy(out=kT.ap[:, :, t0:t0 + 128], in_=g.PSB[bank][:, 256:512].rearrange("p (c t) -> p c t", c=2)),
              g.psk[bank], kT.k())
    mem.release(m1)
    qm = [mem.alloc([512], BF16) for _ in range(2)]
    PT = [mem.alloc([512], BF16) for _ in range(3)]
    o1 = mem.alloc([4, 64], F32)
    rs = mem.alloc([8], F32)
    osb = [mem.alloc([4, 256], F32) for _ in range(2)]
    slabs = []
    if do_ctx:
        slabs.append((0, TC, [0, 1]))
    for s_i in range(TL // 512):
        slabs.append((TC + s_i * 512, 512, list(range(TT // 128))))
    cnt = [0]
    SCALE = 32 ** -0.5
    for si, (q0, qw, ktiles) in enumerate(slabs):
        nqt = qw // 128
        ob = osb[si % 2]
        for h in range(4):
            for j in range(2):
                gi = h * 2 + j
                c, r = gi // 4, gi % 4
                qm_ = qm[gi % 2]
                S.dve(lambda e, qm_=qm_, c=c, r=r, q0=q0, qw=qw: e.tensor_scalar(out=qm_.ap[:, 0:qw], in0=qT.ap[:, c, q0:q0 + qw], scalar1=gm4.ap[:, r:r + 1], scalar2=None, op0=ALU.mult),
                      qT.k() + gm4.k(), qm_.k())
                for ki, kt in enumerate(ktiles):
                    sb = cnt[0] % 2
                    pt = PT[cnt[0] % 3]
                    cnt[0] += 1
                    S.pe(lambda e, qm_=qm_, c=c, kt=kt, sb=sb, qw=qw: e.matmul(g.PS[sb][:, 0:qw], lhsT=kT.ap[:, c, kt * 128:(kt + 1) * 128], rhs=qm_.ap[:, 0:qw], start=True, stop=True),
                         kT.k() + qm_.k(), g.psk[sb])
                    S.act(lambda e, pt=pt, sb=sb, qw=qw: e.activation(out=pt.ap[:, 0:qw], in_=g.PS[sb][:, 0:qw], func=AF.Exp, scale=SCALE), g.psk[sb], pt.k())
                    for qt in range(nqt):
                        S.pe(lambda e, pt=pt, qt=qt, kt=kt, h=h, ki=ki, nk=len(ktiles): e.matmul(g.PS[4 + qt][:, 0:65], lhsT=pt.ap[:, qt * 128:(qt + 1) * 128], rhs=v1.ap[:, kt, h, :],
                                                                                                start=(ki == 0), stop=(ki == nk - 1)),
                             pt.k() + v1.k(), g.psk[4 + qt])
                for qt in range(nqt):
                    S.dve(lambda e, qt=qt, j=j: e.reciprocal(out=rs.ap[:, j * 4 + qt:j * 4 + qt + 1], in_=g.PS[4 + qt][:, 64:65]), g.psk[4 + qt], rs.k())
                    if j == 0:
                        S.dve(lambda e, qt=qt: e.tensor_scalar(out=o1.ap[:, qt, :], in0=g.PS[4 + qt][:, 0:64], scalar1=rs.ap[:, qt:qt + 1], scalar2=None, op0=ALU.mult),
                              g.psk[4 + qt] + rs.k(), o1.k())
                    else:
                        S.dve(lambda e, qt=qt: e.tensor_tensor(out=rs.ap[:, 4 + qt:5 + qt], in0=rs.ap[:, 4 + qt:5 + qt], in1=lams.ap[:, 4:5], op=ALU.mult), rs.k() + lams.k(), rs.k())
                        S.dve(lambda e, qt=qt, h=h, ob=ob: e.scalar_tensor_tensor(out=ob.ap[:, qt, h * 64:(h + 1) * 64], in0=g.PS[4 + qt][:, 0:64], scalar=rs.ap[:, 4 + qt:5 + qt],
                                                                                  in1=o1.ap[:, qt, :], op0=ALU.mult, op1=ALU.add),
                              g.psk[4 + qt] + rs.k() + o1.k(), ob.k())
        for qt in range(nqt):
            tt = q0 + qt * 128
            S.dma(lambda e, ob=ob, qt=qt, tt=tt: e.dma_start(out=g.O_DA[0, tt:tt + 128, :], in_=ob.ap[:, qt, :]), ob.k(), [("O_DA", 0, v) for v in range(tt // 32, tt // 32 + 4)])
    mem.release(m0)


def hy_params(L):
    n = 2 * L
    N1 = n // 128
    return n, N1, N1 // 2


def hy_stage1(g, L, X, N, in_keys, W1):
    S, mem = g.S, g.mem
    n, N1, K1 = hy_params(L)
    m0 = mem.mark()
    G = 16
    pcs = [mem.alloc([G, N], BF16, parts=K1) for _ in range(2)]
    stg = [mem.alloc([G, N], BF16) for _ in range(2)]
    Xv = X.rearrange("(a b) n -> a b n", b=128)
    nmm = G * N // 512
    for gi in range(128 // G):
        pc, st = pcs[gi % 2], stg[gi % 2]
        S.dma(lambda e, pc=pc, gi=gi: e.dma_start(out=pc.ap, in_=Xv[:, gi * G:(gi + 1) * G, :]), in_keys, pc.k())
        pcf = pc.ap.rearrange("p a n -> p (a n)")
        stf = st.ap.rearrange("p a n -> p (a n)")
        for m in range(nmm):
            bank = m % 2
            S.pe(lambda e, pcf=pcf, m=m, bank=bank: e.matmul(g.PS[bank][0:2 * N1, :], lhsT=W1.ap, rhs=pcf[:, m * 512:(m + 1) * 512], start=True, stop=True),
                 pc.k() + W1.k(), g.psk[bank])
            if m % 2 == 0:
                S.act(lambda e, stf=stf, m=m, bank=bank: e.activation(out=stf[0:2 * N1, m * 512:(m + 1) * 512], in_=g.PS[bank][0:2 * N1, :], func=AF.Copy), g.psk[bank], st.k())
            else:
                S.dve(lambda e, stf=stf, m=m, bank=bank: e.tensor_copy(out=stf[0:2 * N1, m * 512:(m + 1) * 512], in_=g.PS[bank][0:2 * N1, :]), g.psk[bank], st.k())
        S.dma(lambda e, st=st, gi=gi: e.dma_start(out=g.AD[0:2 * N1, gi * G:(gi + 1) * G, 0:N], in_=st.ap[0:2 * N1]), st.k(), [("AD", gi)])
    mem.release(m0)


def hy_load_consts(g, L):
    S, mem, I = g.S, g.mem, g.I
    n, N1, K1 = hy_params(L)
    tg = "%d" % L
    W1 = mem.alloc([2 * N1], BF16, parts=K1)
    E = mem.alloc([K1], BF16, parts=2 * N1)
    S.dma(lambda e: e.dma_start(out=W1.ap, in_=I["hy_W1_" + tg]), [], W1.k())
    S.dma(lambda e: e.dma_start(out=E.ap, in_=I["hy_E_" + tg]), [], E.k())
    return W1, E


def stage_hy_filter(g, l, L, upto=99):
    nc, S, mem, I = g.nc, g.S, g.mem, g.I
    n, N1, K1 = hy_params(L)
    tg = "%d" % L
    m0 = mem.mark()
    W1, E = hy_load_consts(g, L)
    m1 = mem.mark()
    NTL = L // 128
    w3 = mem.alloc([512], F32, parts=64)
    S.dma(lambda e: e.dma_start(out=w3.ap, in_=I["hy_w3"][l]), [], w3.k())
    hB = mem.alloc([L], F32, parts=64)
    hpi = mem.alloc([1], F32)
    S.dve(lambda e: e.memset(hpi.ap, math.pi / 2), [], hpi.k())
    m_s = mem.mark()
    fe = mem.alloc([L], F32, parts=33)
    S.dma(lambda e: e.dma_start(out=fe.ap, in_=I["hy_featsT" + tg]), [], fe.k())
    w1 = mem.alloc([64], F32, parts=33)
    w2 = mem.alloc([64], F32, parts=64)
    S.dma(lambda e: e.dma_start(out=w1.ap, in_=I["hy_w1"][l]), [], w1.k())
    S.dma(lambda e: e.dma_start(out=w2.ap, in_=I["hy_w2"][l]), [], w2.k())
    pc = mem.alloc([12], F32, parts=64)
    S.dma(lambda e: e.dma_start(out=pc.ap[:, 0:2], in_=I["hy_freq"][l].rearrange("a p -> p a"), allow_slow_non_contiguous=True), [], pc.k())
    S.dma(lambda e: e.dma_start(out=pc.ap[:, 2:3], in_=I["hy_b1"][l:l + 1, :].rearrange("a p -> p a"), allow_slow_non_contiguous=True), [], pc.k())
    S.dma(lambda e: e.dma_start(out=pc.ap[:, 3:4], in_=I["hy_b2"][l:l + 1, :].rearrange("a p -> p a"), allow_slow_non_contiguous=True), [], pc.k())
    S.dve(lambda e: e.tensor_scalar(out=pc.ap[:, 4:6], in0=pc.ap[:, 0:2], scalar1=1.0 / (2 * math.pi), scalar2=None, op0=ALU.mult), pc.k(), pc.k())
    S.dve(lambda e: e.tensor_tensor(out=pc.ap[:, 8:10], in0=pc.ap[:, 0:2], in1=pc.ap[:, 2:4], op=ALU.mult), pc.k(), pc.k())
    S.dve(lambda e: e.tensor_scalar(out=pc.ap[:, 6:8], in0=pc.ap[:, 8:10], scalar1=1.0 / (2 * math.pi), scalar2=64.5, op0=ALU.mult, op1=ALU.add), pc.k(), pc.k())
    hA = mem.alloc([L], F32, parts=64)
    ki = [mem.alloc([512], I32, parts=64) for _ in range(2)]
    tf = [mem.alloc([512], F32, parts=64) for _ in range(2)]
    xf = [mem.alloc([512], F32, parts=64) for _ in range(2)]
    sn = [mem.alloc([512], F32, parts=64) for _ in range(2)]
    cs = [mem.alloc([512], F32, parts=64) for _ in range(2)]

    def sine_layer(wt, kparts, src, dst, li):
        for si in range((L + 511) // 512):
            c0 = si * 512
            cw = min(512, L - c0)
            u = si % 2
            bank = u
            S.pe(lambda e, c0=c0, cw=cw, bank=bank: e.matmul(g.PS[bank][0:64, 0:cw], lhsT=wt.ap, rhs=src.ap[0:kparts, c0:c0 + cw], start=True, stop=True),
                 wt.k() + src.k(), g.psk[bank])
            k_, t_, x_, s_, c_ = ki[u], tf[u], xf[u], sn[u], cs[u]
            S.dve(lambda e, k_=k_, bank=bank, cw=cw: e.tensor_scalar(out=k_.ap[:, 0:cw], in0=g.PS[bank][0:64, 0:cw], scalar1=pc.ap[:, 4 + li:5 + li], scalar2=pc.ap[:, 6 + li:7 + li],
                                                                     op0=ALU.mult, op1=ALU.add), g.psk[bank] + pc.k(), k_.k())
            S.dve(lambda e, k_=k_, t_=t_, cw=cw: e.tensor_scalar(out=t_.ap[:, 0:cw], in0=k_.ap[:, 0:cw], scalar1=-64.0, scalar2=-2 * math.pi, op0=ALU.add, op1=ALU.mult), k_.k(), t_.k())
            S.dve(lambda e, x_=x_, bank=bank, cw=cw: e.tensor_scalar(out=x_.ap[:, 0:cw], in0=g.PS[bank][0:64, 0:cw], scalar1=pc.ap[:, li:li + 1], scalar2=pc.ap[:, 8 + li:9 + li],
                                                                     op0=ALU.mult, op1=ALU.add), g.psk[bank] + pc.k(), x_.k())
            S.pool(lambda e, x_=x_, t_=t_, cw=cw: e.tensor_tensor(out=x_.ap[:, 0:cw], in0=x_.ap[:, 0:cw], in1=t_.ap[:, 0:cw], op=ALU.add), x_.k() + t_.k(), x_.k())
            S.act(lambda e, x_=x_, s_=s_, cw=cw: e.activation(out=s_.ap[:, 0:cw], in_=x_.ap[:, 0:cw], func=AF.Sin, scale=0.5), x_.k(), s_.k())
            S.act(lambda e, x_=x_, c_=c_, cw=cw: e.activation(out=c_.ap[:, 0:cw], in_=x_.ap[:, 0:cw], func=AF.Sin, scale=0.5, bias=hpi.ap[0:64, 0:1]), x_.k() + hpi.k(), c_.k())
            S.dve(lambda e, s_=s_, c_=c_, c0=c0, cw=cw: e.scalar_tensor_tensor(out=dst.ap[:, c0:c0 + cw], in0=s_.ap[:, 0:cw], scalar=2.0, in1=c_.ap[:, 0:cw], op0=ALU.mult, op1=ALU.mult),
                  s_.k() + c_.k(), dst.k(c0, c0 + cw))

    sine_layer(w1, 33, fe, hA, 0)
    if upto == 1:
        S.dma(lambda e: e.dma_start(out=g.ZF[0:64, 0:256], in_=hA.ap[:, 0:256]), hA.k(), [("ZF", 0)])
        mem.release(m0)
        return
    sine_layer(w2, 64, hA, hB, 1)
    mem.release(m_s)
    if upto == 2:
        S.dma(lambda e: e.dma_start(out=g.ZF[0:64, 0:256], in_=hB.ap[:, 0:256]), hB.k(), [("ZF", 0)])
        mem.release(m0)
        return
    decb = mem.alloc([512], F32)
    S.dma(lambda e: e.dma_start(out=decb.ap, in_=I["hy_decay"][l:l + 1, :].partition_broadcast(128)), [], decb.k())
    S.act(lambda e: e.activation(out=decb.ap, in_=decb.ap, func=AF.Abs), decb.k(), decb.k())
    nd = mem.alloc([NTL], F32)
    S.dma(lambda e: e.dma_start(out=nd.ap, in_=I["hy_negdist" + tg]), [], nd.k())
    hw = mem.alloc([NTL, 512], F32)
    win = [mem.alloc([512], F32) for _ in range(2)]
    ab = [mem.alloc([512], F32) for _ in range(2)]
    for i in range(NTL):
        u = i % 2
        bank = 2 + u
        S.pe(lambda e, i=i, bank=bank: e.matmul(g.PS[bank], lhsT=hB.ap[:, i * 128:(i + 1) * 128], rhs=w3.ap, start=True, stop=True), hB.k() + w3.k(), g.psk[bank])
        S.act(lambda e, i=i, u=u: e.activation(out=win[u].ap, in_=decb.ap, func=AF.Exp, scale=nd.ap[:, i:i + 1]), decb.k() + nd.k(), win[u].k())
        S.dve(lambda e, i=i, u=u, bank=bank: e.scalar_tensor_tensor(out=hw.ap[:, i, :], in0=win[u].ap, scalar=0.05, in1=g.PS[bank], op0=ALU.add, op1=ALU.mult),
              win[u].k() + g.psk[bank], hw.k(i * 512, (i + 1) * 512))
        S.act(lambda e, i=i, u=u: e.activation(out=ab[u].ap, in_=hw.ap[:, i, :], func=AF.Abs), hw.k(i * 512, (i + 1) * 512), ab[u].k())
        S.pe(lambda e, i=i, u=u: e.matmul(g.PS[7], lhsT=g.ones_f.ap, rhs=ab[u].ap, start=(i == 0), stop=(i == NTL - 1)), g.ones_f.k() + ab[u].k(), g.psk[7])
    rcs = mem.alloc([512], F32)
    S.dve(lambda e: e.reciprocal(out=rcs.ap, in_=g.PS[7]), g.psk[7], rcs.k())
    hn = [mem.alloc([512], BF16) for _ in range(2)]
    for i in range(NTL):
        u = i % 2
        S.dve(lambda e, i=i, u=u: e.tensor_tensor(out=hn[u].ap, in0=hw.ap[:, i, :], in1=rcs.ap, op=ALU.mult), hw.k(i * 512, (i + 1) * 512) + rcs.k(), hn[u].k())
        S.dma(lambda e, i=i, u=u: e.dma_start(out=g.HFILT[L][i * 128:(i + 1) * 128, :], in_=hn[u].ap), hn[u].k(), [("HFILT", L, i)])
    mem.release(m1)
    if upto == 3:
        mem.release(m0)
        return
    hy_stage1(g, L, g.HFILT[L], 512, [("HFILT", L, i) for i in range(NTL)], W1)
    if upto == 4:
        g.S.dma(lambda e: e.dma_start(out=g.ZF[0:128, 0:256], in_=g.AD[5, :, 0:256].bitcast(BF16)) if False else e.dma_start(out=g.ZD[0], in_=g.AD[5, :, 0:256]), [("AD", gi) for gi in range(8)], [("ZD", 0)])
        mem.release(m0)
        return
    adk = [("AD", gi) for gi in range(8)]
    are = [mem.alloc([512], BF16) for _ in range(2)]
    aim = [mem.alloc([512], BF16) for _ in range(2)]
    pk = [mem.alloc([4, 128], BF16) for _ in range(2)]
    hs = [mem.alloc([2, 512], F32) for _ in range(2)]
    for f1 in range(N1 if upto > 10 else min(N1, 2 ** upto)):
        u = f1 % 2
        S.dma(lambda e, f1=f1, u=u: e.dma_start(out=are[u].ap, in_=g.AD[f1, :, 0:512]), adk, are[u].k())
        S.dma(lambda e, f1=f1, u=u: e.dma_start(out=aim[u].ap, in_=g.AD[N1 + f1, :, 0:512]), adk, aim[u].k())
        S.dma(lambda e, f1=f1, u=u: e.dma_start(out=pk[u].ap, in_=I["hy_PKH_" + tg][f1]), [], pk[u].k())
        for hb in range(2):
            bank = 4 + hb
            S.pe(lambda e, u=u, hb=hb, bank=bank: e.matmul(g.PS[bank], lhsT=pk[u].ap[:, 2 * hb, :], rhs=are[u].ap, start=True, stop=False), pk[u].k() + are[u].k(), g.psk[bank])
            S.pe(lambda e, u=u, hb=hb, bank=bank: e.matmul(g.PS[bank], lhsT=pk[u].ap[:, 2 * hb + 1, :], rhs=aim[u].ap, start=False, stop=True), pk[u].k() + aim[u].k(), g.psk[bank])
            if hb == 0:
                S.act(lambda e, u=u, hb=hb, bank=bank: e.activation(out=hs[u].ap[:, hb, :], in_=g.PS[bank], func=AF.Copy), g.psk[bank], hs[u].k())
            else:
                S.dve(lambda e, u=u, hb=hb, bank=bank: e.tensor_copy(out=hs[u].ap[:, hb, :], in_=g.PS[bank]), g.psk[bank], hs[u].k())
        S.dma(lambda e, f1=f1, u=u: e.dma_start(out=g.HAB[L][:, f1].rearrange("a p n -> p a n"), in_=hs[u].ap), hs[u].k(), [("HAB", L, f1)])
    mem.release(m0)


def stage_hy(g, l, b, do_ctx):
    nc, S, mem, I = g.nc, g.S, g.mem, g.I
    m0 = mem.mark()
    segs = [(TC, TL)]
    if do_ctx:
        segs.append((0, TC))
    m1 = mem.mark()
    wc = mem.alloc([3, 768], F32)
    S.dma(lambda e: e.dma_start(out=wc.ap, in_=I["hy_conv_w"][l:l + 1].rearrange("o a c -> o (a c)").partition_broadcast(128).rearrange("p o (a c) -> p (o a) c", a=3)), [], wc.k())
    cur = [mem.alloc([768], F32) for _ in range(2)]
    prv = [mem.alloc([768], F32) for _ in range(2)]
    nxt = [mem.alloc([768], F32) for _ in range(2)]
    zb = [mem.alloc([256], BF16) for _ in range(2)]
    cnt = 0
    for (s0, L) in segs:
        for i in range(L // 128):
            t0 = s0 + i * 128
            u = cnt % 2
            cnt += 1
            c_, p_, n_, z_ = cur[u], prv[u], nxt[u], zb[u]
            ti = t0 // 128
            S.dma(lambda e, c_=c_, t0=t0: e.dma_start(out=c_.ap, in_=g.HY[t0:t0 + 128, :]), [("HY", ti, 0), ("HY", ti, 512)], c_.k())
            if i == 0:
                S.pool(lambda e, p_=p_: e.memset(p_.ap, 0.0), [], p_.k())
                S.dma(lambda e, p_=p_, t0=t0: e.dma_start(out=p_.ap[1:128, :], in_=g.HY[t0:t0 + 127, :]), [("HY", ti, 0), ("HY", ti, 512)], p_.k())
            else:
                S.dma(lambda e, p_=p_, t0=t0: e.dma_start(out=p_.ap, in_=g.HY[t0 - 1:t0 + 127, :]),
                      [("HY", ti, 0), ("HY", ti, 512), ("HY", ti - 1, 0), ("HY", ti - 1, 512)], p_.k())
            if i == L // 128 - 1:
                S.pool(lambda e, n_=n_: e.memset(n_.ap, 0.0), [], n_.k())
                S.dma(lambda e, n_=n_, t0=t0: e.dma_start(out=n_.ap[0:127, :], in_=g.HY[t0 + 1:t0 + 128, :]), [("HY", ti, 0), ("HY", ti, 512)], n_.k())
            else:
                S.dma(lambda e, n_=n_, t0=t0: e.dma_start(out=n_.ap, in_=g.HY[t0 + 1:t0 + 129, :]),
                      [("HY", ti, 0), ("HY", ti, 512), ("HY", ti + 1, 0), ("HY", ti + 1, 512)], n_.k())
            S.dve(lambda e, c_=c_: e.tensor_tensor(out=c_.ap, in0=c_.ap, in1=wc.ap[:, 1, :], op=ALU.mult), c_.k() + wc.k(), c_.k())
            S.pool(lambda e, p_=p_: e.tensor_tensor(out=p_.ap, in0=p_.ap, in1=wc.ap[:, 0, :], op=ALU.mult), p_.k() + wc.k(), p_.k())
            S.pool(lambda e, n_=n_: e.tensor_tensor(out=n_.ap, in0=n_.ap, in1=wc.ap[:, 2, :], op=ALU.mult), n_.k() + wc.k(), n_.k())
            S.dve(lambda e, c_=c_, p_=p_: e.tensor_tensor(out=c_.ap, in0=c_.ap, in1=p_.ap, op=ALU.add), c_.k() + p_.k(), c_.k())
            S.dve(lambda e, c_=c_, n_=n_: e.tensor_tensor(out=c_.ap, in0=c_.ap, in1=n_.ap, op=ALU.add), c_.k() + n_.k(), c_.k())
            S.act(lambda e, c_=c_, z_=z_: e.activation(out=z_.ap, in_=c_.ap[:, 0:256], func=AF.Copy), c_.k(), z_.k())
            S.dma(lambda e, c_=c_, t0=t0: e.dma_start(out=g.UCONV[t0:t0 + 128, :], in_=c_.ap), c_.k(), [("UCONV", ti)])
            S.dma(lambda e, c_=c_, t0=t0: e.dma_start(out=g.ZF[t0:t0 + 128, :], in_=c_.ap[:, 0:256]), c_.k(), [("ZF", ti)])
            S.dma(lambda e, z_=z_, t0=t0: e.dma_start(out=g.ZB[0, t0:t0 + 128, :], in_=z_.ap), z_.k(), [("ZB", 0, ti)])
    mem.release(m1)
    def do_seg(s0, L):
        n, N1, K1 = hy_params(L)
        tg = "%d" % L
        m2 = mem.mark()
        W1, E = hy_load_consts(g, L)
        tiles = list(range(s0 // 128, (s0 + L) // 128))
        def do_order(o):
            zin = g.ZB[o % 2, s0:s0 + L, :]
            hy_stage1(g, L, zin, 256, [("ZB", o % 2, ti) for ti in tiles], W1)
            m3 = mem.mark()
            adk = [("AD", gi) for gi in range(8)]
            are = [mem.alloc([256], BF16) for _ in range(2)]
            aim = [mem.alloc([256], BF16) for _ in range(2)]
            pk = [mem.alloc([6, 128], BF16) for _ in range(2)]
            hab = [mem.alloc([2, 256], F32) for _ in range(2)]
            ta = [mem.alloc([256], F32) for _ in range(2)]
            tb = [mem.alloc([256], F32) for _ in range(2)]
            yb = [mem.alloc([256], BF16) for _ in range(2)]
            zs = [mem.alloc([2, 256], BF16) for _ in range(2)]
            for f1 in range(N1):
                u = f1 % 2
                S.dma(lambda e, f1=f1, u=u: e.dma_start(out=are[u].ap, in_=g.AD[f1, :, 0:256]), adk, are[u].k())
                S.dma(lambda e, f1=f1, u=u: e.dma_start(out=aim[u].ap, in_=g.AD[N1 + f1, :, 0:256]), adk, aim[u].k())
                S.dma(lambda e, f1=f1, u=u: e.dma_start(out=pk[u].ap, in_=I["hy_PK_" + tg][f1]), [], pk[u].k())
                S.dma(lambda e, f1=f1, u=u, o=o: e.dma_start(out=hab[u].ap, in_=g.HAB[L][:, f1, :, o * 256:(o + 1) * 256].rearrange("a p n -> p a n")),
                      [("HAB", L, f1)], hab[u].k())
                for hb in range(2):
                    bank = 2 + hb
                    S.pe(lambda e, u=u, hb=hb, bank=bank: e.matmul(g.PS[bank][:, 0:256], lhsT=pk[u].ap[:, 2 * hb, :], rhs=are[u].ap, start=True, stop=False), pk[u].k() + are[u].k(), g.psk[bank])
                    S.pe(lambda e, u=u, hb=hb, bank=bank: e.matmul(g.PS[bank][:, 0:256], lhsT=pk[u].ap[:, 2 * hb + 1, :], rhs=aim[u].ap, start=False, stop=True), pk[u].k() + aim[u].k(), g.psk[bank])
                S.dve(lambda e, u=u: e.tensor_tensor(out=ta[u].ap, in0=g.PS[2][:, 0:256], in1=hab[u].ap[:, 0, :], op=ALU.mult), g.psk[2] + hab[u].k(), ta[u].k())
                S.dve(lambda e, u=u: e.tensor_tensor(out=tb[u].ap, in0=g.PS[3][:, 0:256], in1=hab[u].ap[:, 1, :], op=ALU.mult), g.psk[3] + hab[u].k(), tb[u].k())
                S.pool(lambda e, u=u: e.tensor_tensor(out=yb[u].ap, in0=ta[u].ap, in1=tb[u].ap, op=ALU.add), ta[u].k() + tb[u].k(), yb[u].k())
                for hb in range(2):
                    bank = 4 + hb
                    S.pe(lambda e, u=u, hb=hb, bank=bank: e.matmul(g.PS[bank][:, 0:256], lhsT=pk[u].ap[:, 4 + hb, :], rhs=yb[u].ap, start=True, stop=True), pk[u].k() + yb[u].k(), g.psk[bank])
                    if hb == 0:
                        S.act(lambda e, u=u, hb=hb, bank=bank: e.activation(out=zs[u].ap[:, hb, :], in_=g.PS[bank][:, 0:256], func=AF.Copy), g.psk[bank], zs[u].k())
                    else:
                        S.dve(lambda e, u=u, hb=hb, bank=bank: e.tensor_copy(out=zs[u].ap[:, hb, :], in_=g.PS[bank][:, 0:256]), g.psk[bank], zs[u].k())
                S.dma(lambda e, f1=f1, u=u: e.dma_start(out=g.ZD[f1, :, :], in_=zs[u].ap[:, 0, :]), zs[u].k(), [("ZD", f1)])
                S.dma(lambda e, f1=f1, u=u: e.dma_start(out=g.ZD[N1 + f1, :, :], in_=zs[u].ap[:, 1, :]), zs[u].k(), [("ZD", N1 + f1)])
            mem.release(m3)
            m4 = mem.mark()
            G = 16
            zin_ = [mem.alloc([G, 256], BF16, parts=2 * N1) for _ in range(2)]
            gt = [mem.alloc([G, 256], F32, parts=K1) for _ in range(2)]
            zp = [mem.alloc([G, 256], F32, parts=K1) for _ in range(2)]
            zn = [mem.alloc([G, 256], F32, parts=K1) for _ in range(2)]
            znb = [mem.alloc([G, 256], BF16, parts=K1) for _ in range(2)]
            tq = [mem.alloc([512], F32, parts=K1) for _ in range(2)]
            bb = mem.alloc([256], F32, parts=K1)
            S.dma(lambda e, o=o: e.dma_start(out=bb.ap, in_=I["hy_bias"][l, o:o + 1, :].partition_broadcast(K1)), [], bb.k())
            zdk = [("ZD", r_) for r_ in range(2 * N1)]
            Uv = g.UCONV[s0:s0 + L, :].rearrange("(a b) n -> a b n", b=128)
            ZFv = g.ZF[s0:s0 + L, :].rearrange("(a b) n -> a b n", b=128)
            ZBv = g.ZB[(o + 1) % 2, s0:s0 + L, :].rearrange("(a b) n -> a b n", b=128)
            ukeys = [("UCONV", ti) for ti in tiles]
            zfk = [("ZF", ti) for ti in tiles]
            zbk = [("ZB", (o + 1) % 2, ti) for ti in tiles]
            for gi in range(128 // G):
                u = gi % 2
                S.dma(lambda e, u=u, gi=gi: e.dma_start(out=zin_[u].ap, in_=g.ZD[0:2 * N1, gi * G:(gi + 1) * G, :]), zdk, zin_[u].k())
                S.dma(lambda e, u=u, gi=gi, o=o: e.dma_start(out=gt[u].ap, in_=Uv[:, gi * G:(gi + 1) * G, 256 * (o + 1):256 * (o + 2)]), ukeys, gt[u].k())
                S.dma(lambda e, u=u, gi=gi: e.dma_start(out=zp[u].ap, in_=ZFv[:, gi * G:(gi + 1) * G, :]), zfk, zp[u].k())
                zf_ = zin_[u].ap.rearrange("p a n -> p (a n)")
                for pr in range(G // 2):
                    bank = 6 + pr % 2
                    tq_ = tq[pr % 2]
                    S.pe(lambda e, zf_=zf_, pr=pr, bank=bank: e.matmul(g.PS[bank][0:K1, :], lhsT=E.ap, rhs=zf_[:, pr * 512:(pr + 1) * 512], start=True, stop=True),
                         E.k() + zin_[u].k(), g.psk[bank])
                    zpv = zp[u].ap[:, 2 * pr:2 * pr + 2, :]
                    S.pool(lambda e, tq_=tq_, zpv=zpv: e.tensor_tensor(out=tq_.ap.rearrange("p (a n) -> p a n", a=2), in0=zpv, in1=bb.ap[:, None, :].broadcast_to([K1, 2, 256]), op=ALU.mult),
                           zp[u].k() + bb.k(), tq_.k())
                    S.dve(lambda e, tq_=tq_, bank=bank: e.tensor_tensor(out=tq_.ap, in0=tq_.ap, in1=g.PS[bank][0:K1, :], op=ALU.add), tq_.k() + g.psk[bank], tq_.k())
                    S.dve(lambda e, tq_=tq_, u=u, pr=pr: e.tensor_tensor(out=zn[u].ap[:, 2 * pr:2 * pr + 2, :], in0=tq_.ap.rearrange("p (a n) -> p a n", a=2), in1=gt[u].ap[:, 2 * pr:2 * pr + 2, :], op=ALU.mult),
                          tq_.k() + gt[u].k(), zn[u].k())
                S.act(lambda e, u=u: e.activation(out=znb[u].ap, in_=zn[u].ap, func=AF.Copy), zn[u].k(), znb[u].k())
                S.dma(lambda e, u=u, gi=gi: e.dma_start(out=ZFv[:, gi * G:(gi + 1) * G, :], in_=zn[u].ap), zn[u].k(), zfk)
                S.dma(lambda e, u=u, gi=gi: e.dma_start(out=ZBv[:, gi * G:(gi + 1) * G, :], in_=znb[u].ap), znb[u].k(), zbk)
            mem.release(m4)

        for o in range(2):
            do_order(o)
        mem.release(m2)

    for (s0, L) in segs:
        do_seg(s0, L)
    zt = [mem.alloc([256], BF16) for _ in range(2)]
    stg = [mem.alloc([2, 128], BF16) for _ in range(2)]
    cnt = 0
    for (s0, L) in segs:
        for i in range(L // 128):
            t0 = s0 + i * 128
            ti = t0 // 128
            u = cnt % 2
            cnt += 1
            S.dma(lambda e, u=u, t0=t0: e.dma_start(out=zt[u].ap, in_=g.ZB[0, t0:t0 + 128, :]), [("ZB", 0, ti)], zt[u].k())
            bank = 2 + u
            for c in range(2):
                S.pe(lambda e, u=u, c=c, bank=bank: e.transpose(out=g.PSB[bank][:, c * 128:(c + 1) * 128], in_=zt[u].ap[:, c * 128:(c + 1) * 128], identity=g.ident_bf.ap),
                     zt[u].k() + g.ident_bf.k(), g.psk[bank])
            S.act(lambda e, u=u, bank=bank: e.activation(out=stg[u].ap, in_=g.PSB[bank][:, 0:256].rearrange("p (c t) -> p c t", c=2), func=AF.Copy), g.psk[bank], stg[u].k())
            S.dma(lambda e, u=u, t0=t0: e.dma_start(out=g.BRT[1].rearrange("(c p) t -> p c t", p=128)[:, :, t0:t0 + 128], in_=stg[u].ap), stg[u].k(), [("BRT", 1, ti)])
    mem.release(m0)
```

```python
import math
import numpy as np
import ml_dtypes
import concourse.bass as bass
import concourse.mybir as mybir
from concourse.bass_utils import run_bass_kernel_spmd

F32 = mybir.dt.float32
BF16 = mybir.dt.bfloat16
I32 = mybir.dt.int32
U32 = mybir.dt.uint32
U8 = mybir.dt.uint8
AF = mybir.ActivationFunctionType
ALU = mybir.AluOpType
AX = mybir.AxisListType
NPBF = ml_dtypes.bfloat16

D = 1024
TC = 256
TL = 4096
TT = TC + TL
NB = 2
NCORES = 8
DEPTH = 2
NIN = 7712
EPS = 1e-6
PAGE = 2048

OFF = {}
_o = 0
for _n, _w in (("gla_q", 128), ("gla_k", 128), ("gla_v", 256), ("gla_af", 16), ("gla_ab", 16), ("gla_g", 256),
               ("hy", 768), ("hg_q", 256), ("hg_ff", 256), ("hg_fb", 256), ("hg_i", 256), ("hg_g", 256),
               ("da_q", 256), ("da_k", 256), ("da_v", 256), ("merge", 4096)):
    OFF[_n] = (_o, _w)
    _o += _w
assert _o == NIN

COMPUTE = ("pe", "dve", "act", "pool")
DMAQ = ("sp", "pq")
NSEM_DMA = {"sp": 36, "pq": 20}
EPOCH = 16000
NEPOCH = 9


class Ins:
    __slots__ = ("eng", "fn", "deps", "flag", "dma", "idx", "stage", "n0", "n1", "hoist")

    def __init__(self, eng, fn, dma):
        self.eng = eng
        self.fn = fn
        self.deps = set()
        self.flag = False
        self.dma = dma


class Sched:
    def __init__(self, nc):
        self.nc = nc
        self.ins = []
        self.lastw = {}
        self.readers = {}

    def _add(self, eng, fn, reads, writes, dma=False):
        phys = "pool" if eng == "pq" else eng
        I = Ins(eng, fn, dma)
        I.idx = len(self.ins)
        I.stage = getattr(self, "stage", "")
        lastw = self.lastw
        readers = self.readers
        raw = set()
        for k in reads:
            w = lastw.get(k)
            if w is not None:
                raw.add(w)
        other = set()
        for k in writes:
            w = lastw.get(k)
            if w is not None:
                other.add(w)
            rs = readers.get(k)
            if rs:
                other.update(rs)
        if dma:
            I.deps = raw | other
        else:
            deps = set()
            for d in raw:
                dphys = "pool" if d.eng == "pq" else d.eng
                if d.dma or dphys != phys or phys != "pe":
                    deps.add(d)
            for d in other:
                dphys = "pool" if d.eng == "pq" else d.eng
                if d.dma or dphys != phys:
                    deps.add(d)
            I.deps = deps
        I.deps.discard(I)
        for k in writes:
            lastw[k] = I
            readers[k] = []
        wset = set(writes)
        for k in reads:
            if k not in wset:
                readers.setdefault(k, []).append(I)
        self.ins.append(I)
        return I

    def pe(self, fn, reads, writes):
        return self._add("pe", fn, reads, writes)

    def dve(self, fn, reads, writes):
        return self._add("dve", fn, reads, writes)

    def act(self, fn, reads, writes):
        return self._add("act", fn, reads, writes)

    def pool(self, fn, reads, writes):
        return self._add("pool", fn, reads, writes)

    def dma(self, fn, reads, writes, q="sp"):
        I = self._add(q, fn, reads, writes, dma=True)
        I.hoist = (q == "sp" and len(writes) > 0 and all(isinstance(k, tuple) and k[0] == "sb" for k in writes)
                   and not any(isinstance(k, tuple) and k[0] == "sb" for k in reads))
        return I

    def emit(self, final_keys=()):
        nc = self.nc
        for I in self.ins:
            for d in I.deps:
                d.flag = True
        finals = []
        for k in final_keys:
            if k in self.lastw and self.lastw[k] not in finals:
                finals.append(self.lastw[k])
        for f in finals:
            f.flag = True
        sems = {e: [nc.alloc_semaphore("s_%s%d" % (e, i)) for i in range(NEPOCH)] for e in COMPUTE}
        dsem = {q: [nc.alloc_semaphore("d_%s%d" % (q, i)) for i in range(NSEM_DMA[q])] for q in DMAQ}
        sp_list = [I for I in self.ins if I.eng == "sp"]
        pos = {I: i for i, I in enumerate(sp_list)}
        f = {}
        lastf = {"pe": -1, "dve": -1, "act": -1, "pool": -1}
        for I in self.ins:
            phys = "pool" if I.eng == "pq" else I.eng
            v = lastf[phys] if phys != "sp" else -1
            for d in I.deps:
                dv = pos[d] if d.eng == "sp" else f[d]
                if dv > v:
                    v = dv
            f[I] = v
            if phys != "sp":
                lastf[phys] = v
        sp_out = []
        for I in sp_list:
            if getattr(I, "hoist", False):
                k = len(sp_out)
                while k > 0 and (not getattr(sp_out[k - 1], "hoist", False)) and pos[sp_out[k - 1]] > f[I]:
                    k -= 1
                sp_out.insert(k, I)
            else:
                sp_out.append(I)
        streams = {"pe": [], "dve": [], "act": [], "pool": [], "sp": sp_out}
        for I in self.ins:
            if I.eng != "sp":
                streams["pool" if I.eng == "pq" else I.eng].append(I)
        dcnt = {q: [0] * NSEM_DMA[q] for q in DMAQ}
        dnext = {q: 0 for q in DMAQ}
        ccnt = {e: 0 for e in COMPUTE}
        ev = {}
        guard = {}
        for I in self.ins:
            if (not I.dma) and I.flag:
                ccnt[I.eng] += 1
                ep = (ccnt[I.eng] - 1) // EPOCH
                assert ep < NEPOCH, "too many semaphore epochs"
                ev[I] = (sems[I.eng][ep], (ccnt[I.eng] - 1) % EPOCH + 1)
        for qn, st in (("sp", streams["sp"]), ("pq", streams["pool"])):
            for I in st:
                if not I.dma:
                    continue
                q = I.eng
                i = dnext[q]
                dnext[q] = (i + 1) % NSEM_DMA[q]
                prev = dcnt[q][i]
                dcnt[q][i] += 16
                ev[I] = (dsem[q][i], dcnt[q][i])
                guard[I] = (dsem[q][i], prev) if prev > 0 else None
        self.stats = {k: len(v) for k, v in streams.items()}
        self.stats["semmax"] = dict(ccnt)

        def run_stream(phys, eng):
            waited = {}
            for I in streams[phys]:
                need = {}
                for d in I.deps:
                    s, v = ev[d]
                    key = id(s)
                    if key not in need or need[key][1] < v:
                        need[key] = (s, v)
                if I.dma and guard[I] is not None:
                    s, v = guard[I]
                    key = id(s)
                    if key not in need or need[key][1] < v:
                        need[key] = (s, v)
                for key, (s, v) in need.items():
                    if waited.get(key, 0) < v:
                        eng.wait_ge(s, v)
                        waited[key] = v
                I.n0 = nc.get_next_instruction_name()
                bi = I.fn(eng)
                if I.dma:
                    bi.then_inc(ev[I][0], 16)
                elif I.flag:
                    bi.then_inc(ev[I][0], 1)
            if phys == "sp":
                for f in finals:
                    s, v = ev[f]
                    eng.wait_ge(s, v)

        with nc.Block() as block:
            @block.tensor
            def _(e):
                run_stream("pe", e)

            @block.vector
            def _(e):
                run_stream("dve", e)

            @block.scalar
            def _(e):
                run_stream("act", e)

            @block.gpsimd
            def _(e):
                run_stream("pool", e)

            @block.sync
            def _(e):
                run_stream("sp", e)


class Tn:
    def __init__(self, ap, off, nbytes, esize):
        self.ap = ap
        self.off = off
        self.nbytes = nbytes
        self.esize = esize

    def k(self, lo=0, hi=None):
        b0 = self.off + lo * self.esize
        b1 = self.off + (self.nbytes if hi is None else hi * self.esize)
        return [("sb", p) for p in range(b0 // PAGE, (b1 - 1) // PAGE + 1)]


ESZ = {F32: 4, BF16: 2, I32: 4, U32: 4, U8: 1}


class Mem:
    def __init__(self, nc, nbytes):
        self.ar = nc.alloc_sbuf_tensor("arena", [128, nbytes], U8).ap()
        self.nbytes = nbytes
        self.top = 0
        self.peak = 0

    def alloc(self, shape, dtype, parts=128):
        es = ESZ[dtype]
        n = 1
        for s in shape:
            n *= s
        nb = n * es
        off = (self.top + 63) // 64 * 64
        assert off + nb <= self.nbytes, "SBUF arena overflow: need %d have %d" % (off + nb, self.nbytes)
        self.top = off + nb
        self.peak = max(self.peak, self.top)
        ap = self.ar[0:parts, off:off + nb].bitcast(dtype)
        if len(shape) == 2:
            ap = ap.rearrange("p (a b) -> p a b", a=shape[0])
        elif len(shape) == 3:
            ap = ap.rearrange("p (a b c) -> p a b c", a=shape[0], b=shape[1])
        return Tn(ap, off, nb, es)

    def mark(self):
        return self.top

    def release(self, m):
        self.top = m


def dkeys(name, lo, hi, gran):
    return [(name, i) for i in range(lo // gran, (hi - 1) // gran + 1)]


def host_consts():
    c = {}
    c["ident_bf"] = np.eye(128, dtype=np.float32).astype(NPBF)
    c["ident_f"] = np.eye(128, dtype=np.float32)
    c["ones_f"] = np.ones((128, 128), dtype=np.float32)
    c["ones_row"] = np.ones((1, 512), dtype=np.float32)
    tpos = np.arange(TL)
    pos = np.stack([tpos // 64, tpos % 64], 1).astype(np.float32)
    fr = (10000.0 ** (-np.arange(8, dtype=np.float32) / 8.0)).astype(np.float32)
    ang = pos[:, :, None] * fr[None, None, :]
    c["rope"] = np.stack([np.cos(ang), np.sin(ang)], 1).astype(np.float32).reshape(TL, 32)
    gm = np.zeros((128, 4), np.float32)
    for r_ in range(4):
        gm[r_ * 32:(r_ + 1) * 32, r_] = 1.0
    c["gm4"] = gm
    for L in (TL, TC):
        n = 2 * L
        N1 = n // 128
        K1 = N1 // 2
        tg = "%d" % L
        j = np.arange(L, dtype=np.float64)
        tt_ = j / max(L - 1, 1)
        w_ = 2 * np.pi * j / L
        f_ = np.linspace(1e-4, 15, 16)
        feats = np.concatenate([tt_[:, None], np.cos(w_[:, None] * f_), -np.sin(w_[:, None] * f_)], axis=-1)
        c["hy_featsT" + tg] = np.ascontiguousarray(feats.T).astype(np.float32)
        dist = np.abs(j - L // 2) / (L // 2)
        c["hy_negdist" + tg] = np.ascontiguousarray((-dist).reshape(L // 128, 128).T).astype(np.float32)
        t1 = np.arange(K1)[:, None]
        f1 = np.arange(N1)[None, :]
        W1 = np.exp(-2j * np.pi * f1 * t1 / N1 - 1j * np.pi * t1 / N1)
        c["hy_W1_" + tg] = np.concatenate([W1.real, W1.imag], 1).astype(NPBF)
        E = np.exp(2j * np.pi * f1.T * t1.T / N1 + 1j * np.pi * t1.T / N1)
        c["hy_E_" + tg] = np.concatenate([(2.0 / n) * E.real, -(2.0 / n) * E.imag], 0).astype(NPBF)
        t2 = np.arange(128)[:, None]
        f2 = np.arange(64)[None, :]
        PK = np.zeros((N1, 128, 6, 128), np.float64)
        PKH = np.zeros((N1, 128, 4, 128), np.float64)
        for a in range(N1):
            f = a + N1 * f2
            ang = np.pi * (2 * f + 1) * t2 / n
            Gc, Gs = np.cos(ang), -np.sin(ang)
            PK[a, :, 0, :] = np.concatenate([Gc, Gs], 1)
            PK[a, :, 1, :] = np.concatenate([-Gs, Gc], 1)
            PK[a, :, 2, :] = np.concatenate([Gs, Gc], 1)
            PK[a, :, 3, :] = np.concatenate([Gc, -Gs], 1)
            PK[a, :, 4, :] = np.concatenate([Gc.T, Gs.T], 0)
            PK[a, :, 5, :] = np.concatenate([-Gs.T, Gc.T], 0)
            rot = np.exp(1j * np.pi * (2 * f + 1) / 4)
            Gp = (Gc + 1j * Gs) * rot
            Gpc, Gps = Gp.real, Gp.imag
            PKH[a, :, 0, :] = np.concatenate([Gpc, Gpc], 1)
            PKH[a, :, 1, :] = np.concatenate([-Gps, -Gps], 1)
            PKH[a, :, 2, :] = np.concatenate([-Gps, Gps], 1)
            PKH[a, :, 3, :] = np.concatenate([-Gpc, Gpc], 1)
        c["hy_PK_" + tg] = PK.astype(NPBF)
        c["hy_PKH_" + tg] = PKH.astype(NPBF)
    for C, hp in ((128, 4), (32, 2)):
        s_ = np.arange(C)[:, None]
        t_ = np.arange(C)[None, :]
        trif = (s_ <= t_).astype(np.float32)
        trib = (s_ >= t_).astype(np.float32)
        c["tri%d" % C] = np.stack([trif, trib], 1).copy()
        c["msk%d" % C] = np.stack([np.tile(trif, (1, hp)), np.tile(trib, (1, hp))], 1).astype(np.float32)
        dk = 128 // hp
        hm = np.zeros((128, hp, C), np.float32)
        bd = np.zeros((128, hp, 64), np.float32)
        for hh in range(hp):
            hm[hh * dk:(hh + 1) * dk, hh, :] = 1.0
            bd[hh * dk:(hh + 1) * dk, hh, :] = 1.0
        c["hm%d" % C] = hm
        c["bd%d" % C] = bd
    return c


class K:
    pass


def build_program(dbg=(), stop=None, feed=(), plan=None, need=None):
    nc = bass.Bass("TRN2", target_bir_lowering=False)
    S = Sched(nc)
    mem = Mem(nc, 206 * 1024)
    g = K()
    g.nc, g.S, g.mem = nc, S, mem
    g.dbg = {}

    def din(name, shape, dt=F32):
        if need is not None and name not in need:
            return nc.dram_tensor(name, list(shape), dt, kind="Internal").ap()
        return nc.dram_tensor(name, list(shape), dt, kind="ExternalInput").ap()

    def dscr(name, shape, dt=F32):
        kind = "ExternalOutput" if name in dbg else ("ExternalInput" if name in feed else "Internal")
        t = nc.dram_tensor(name, list(shape), dt, kind=kind).ap()
        if name in dbg:
            g.dbg[name] = t
        return t
    g.dscr = dscr

    I = {}
    I["x"] = din("x", [NB, TL, D])
    I["ctx"] = din("ctx", [NB, TC, D])
    I["ccT"] = din("ccT", [128, 8, 3])
    I["ada_w"] = din("ada_w", [DEPTH, D, 6 * D])
    I["ada_b"] = din("ada_b", [DEPTH, 6 * D])
    I["norm1_g"] = din("norm1_g", [DEPTH, D])
    I["norm2_g"] = din("norm2_g", [DEPTH, D])
    I["w_in"] = din("w_in", [DEPTH, D, NIN])
    for nm, shp in (("gla_wa2", [DEPTH, 2, 16, 128]), ("gla_ba", [DEPTH, 2, 128]), ("gla_norm_g", [DEPTH, 64]),
                    ("hy_conv_w", [DEPTH, 3, 768]), ("hy_w1", [DEPTH, 33, 64]), ("hy_b1", [DEPTH, 64]),
                    ("hy_w2", [DEPTH, 64, 64]), ("hy_b2", [DEPTH, 64]), ("hy_w3", [DEPTH, 64, 512]),
                    ("hy_freq", [DEPTH, 2, 64]), ("hy_decay", [DEPTH, 512]), ("hy_bias", [DEPTH, 2, 256]),
                    ("hg_lower", [DEPTH, 2, 256]), ("hg_norm_g", [DEPTH, 64]), ("da_qnorm_g", [DEPTH, 32]),
                    ("da_knorm_g", [DEPTH, 32]), ("da_lam", [DEPTH, 4, 32]), ("da_norm_g", [DEPTH, 64]),
                    ("w_branch", [DEPTH, 4, 256, D]), ("w_out", [DEPTH, D, D]), ("moe_router", [DEPTH, D, 16]),
                    ("moe_w1", [DEPTH, 16, D, D]), ("moe_w3", [DEPTH, 16, D, D]), ("moe_w2", [DEPTH, 16, D, D])):
        I[nm] = din(nm, shp)
    cst = host_consts()
    for k, v in cst.items():
        I[k] = din(k, v.shape, BF16 if v.dtype == NPBF else F32)
    g.I = I
    OUT = nc.dram_tensor("out", [NB, TL, D], F32, kind="ExternalOutput").ap()
    g.OUT = OUT
    g.XC = dscr("XC", [NB, TC, D])
    g.MODS = dscr("MODS", [DEPTH, 3, 6 * D])
    g.HTOK = dscr("HTOK", [TT, D], BF16)
    g.QK_GLA = dscr("QK_GLA", [256, TT])
    g.AFB = dscr("AFB", [32, TT])
    g.V_GLA = dscr("V_GLA", [TT, 256], BF16)
    g.G_GLA = dscr("G_GLA", [TT, 256], BF16)
    g.HY = dscr("HY", [TT, 768])
    g.Q_HG = dscr("Q_HG", [256, TT])
    g.ZT_HG = dscr("ZT_HG", [512, TT])
    g.Z_HG = dscr("Z_HG", [TT, 512])
    g.VG_HG = dscr("VG_HG", [TT, 512], BF16)
    g.QK_DA = dscr("QK_DA", [TT, 512])
    g.V_DA = dscr("V_DA", [TT, 256], BF16)
    g.MG = dscr("MG", [4096, TT], BF16)
    g.O_GLA = dscr("O_GLA", [2, TT, 256])
    g.O_HG = dscr("O_HG", [2, TT, 256])
    g.O_DA = dscr("O_DA", [1, TT, 256])
    g.BRT = dscr("BRT", [4, 256, TT], BF16)
    g.HFILT = {TL: dscr("HFILT_X", [TL, 512], BF16), TC: dscr("HFILT_C", [TC, 512], BF16)}
    g.HAB = {TL: dscr("HAB_X", [2, 64, 128, 512]), TC: dscr("HAB_C", [2, 4, 128, 512])}
    g.UCONV = dscr("UCONV", [TT, 768])
    g.ZB = dscr("ZB", [2, TT, 256], BF16)
    g.ZF = dscr("ZF", [TT, 256])
    g.AD = dscr("AD", [128, 128, 512], BF16)
    g.ZD = dscr("ZD", [128, 128, 256], BF16)

    psall = nc.alloc_psum_tensor("psall", [128, 8 * 512], F32).ap()
    g.PS = [psall[:, b * 512:(b + 1) * 512] for b in range(8)]
    g.PSB = [psall[:, b * 512:(b + 1) * 512].bitcast(BF16) for b in range(8)]
    g.psk = [[("ps", b)] for b in range(8)]

    g.ident_bf = mem.alloc([128], BF16)
    g.ident_f = mem.alloc([128], F32)
    g.ones_f = mem.alloc([128], F32)
    S.dma(lambda e: e.dma_start(out=g.ident_bf.ap, in_=I["ident_bf"]), [], g.ident_bf.k())
    S.dma(lambda e: e.dma_start(out=g.ident_f.ap, in_=I["ident_f"]), [], g.ident_f.k())
    S.dma(lambda e: e.dma_start(out=g.ones_f.ap, in_=I["ones_f"]), [], g.ones_f.k())
    g.hT = mem.alloc([8, TT], BF16)

    for b in range(NB):
        for q in range(4):
            S.dma(lambda e, b=b, q=q: e.dma_start(out=OUT[b, q * 1024:(q + 1) * 1024, :], in_=I["x"][b, q * 1024:(q + 1) * 1024, :]),
                  [], dkeys(("XX", b), q * 1024, (q + 1) * 1024, 128))
        S.dma(lambda e, b=b: e.dma_start(out=g.XC[b], in_=I["ctx"][b]), [], dkeys(("XC", b), 0, TC, 128))

    if plan is not None:
        for (fn, args) in plan:
            globals()[fn](g, *args)
        return finish(g)
    stage_mods_all(g)
    for l in range(DEPTH):
        last = (l == DEPTH - 1)
        lam_init = 0.8 - 0.6 * math.exp(-0.3 * l)
        S.stage = "hy_filter"
        stage_hy_filter(g, l, TL)
        if not last:
            stage_hy_filter(g, l, TC)
        tok_lo = TC if last else 0
        for b in range(NB):
            S.stage = "norm1"
            stage_norm(g, l, b, 1)
            S.stage = "stage_proj"
            stage_proj(g, l, b)
            S.stage = "recur_gla"
            stage_recur(g, l, b, "gla")
            S.stage = "recur_hg"
            stage_recur(g, l, b, "hg")
            S.stage = "stage_da"
            stage_da(g, l, b, not last)
            S.stage = "stage_hy"
            stage_hy(g, l, b, not last)
            S.stage = "brnorm"
            stage_branch_norm(g, l, b, "O_GLA", "O_GLA", 2, "G_GLA", 0, "G_GLA", "gla_norm_g", 1.0, 0, tok_lo)
            S.stage = "brnorm"
            stage_branch_norm(g, l, b, "O_HG", "O_HG", 2, "VG_HG", 256, "VG_HG", "hg_norm_g", 1.0, 2, tok_lo)
            S.stage = "brnorm"
            stage_branch_norm(g, l, b, "O_DA", "O_DA", 1, None, 0, None, "da_norm_g", 1.0 - lam_init, 3, tok_lo)
            S.stage = "stage_merge"
            stage_merge(g, l, b, tok_lo)
            S.stage = "norm2"
            stage_norm(g, l, b, 2, segs=("x",) if last else ("c", "x"))
            S.stage = "stage_moe"
            stage_moe(g, l, b, not last)
    return finish(g)


def finish(g):
    fk = []
    for b in range(NB):
        fk += dkeys(("XX", b), 0, TL, 128)
    for name in g.dbg:
        fk.append(("DBG", name))
    for k in list(g.S.lastw.keys()):
        if isinstance(k, tuple) and len(k) >= 1 and isinstance(k[0], str) and k[0] in g.dbg:
            fk.append(k)
    g.S.emit(final_keys=fk)
    return g


def stage_mods_all(g):
    nc, S, mem, I = g.nc, g.S, g.mem, g.I
    m0 = mem.mark()
    scT = mem.alloc([8, 3], F32)
    S.dma(lambda e: e.dma_start(out=scT.ap, in_=I["ccT"]), [], scT.k())
    S.act(lambda e: e.activation(out=scT.ap, in_=scT.ap, func=AF.Silu), scT.k(), scT.k())
    modrow = mem.alloc([6 * D], F32, parts=3)
    bias = mem.alloc([6 * D], F32, parts=3)
    gbc = mem.alloc([2, D], F32, parts=3)
    wsl = [mem.alloc([8, 512], F32) for _ in range(2)]
    for l in range(DEPTH):
        S.dma(lambda e, l=l: e.dma_start(out=bias.ap, in_=I["ada_b"][l:l + 1, :].partition_broadcast(3)), [], bias.k())
        S.dma(lambda e, l=l: e.dma_start(out=gbc.ap[:, 0, :], in_=I["norm1_g"][l:l + 1, :].partition_broadcast(3)), [], gbc.k())
        S.dma(lambda e, l=l: e.dma_start(out=gbc.ap[:, 1, :], in_=I["norm2_g"][l:l + 1, :].partition_broadcast(3)), [], gbc.k())
        for n in range(12):
            w = wsl[n % 2]
            src = I["ada_w"][l].rearrange("(k p) n -> p k n", p=128)[:, :, n * 512:(n + 1) * 512]
            S.dma(lambda e, w=w, src=src: e.dma_start(out=w.ap, in_=src), [], w.k())
            bank = n % 2
            for k in range(8):
                S.pe(lambda e, w=w, k=k, bank=bank: e.matmul(g.PS[bank][0:3, :], lhsT=scT.ap[:, k, :], rhs=w.ap[:, k, :],
                                                            start=(k == 0), stop=(k == 7)),
                     scT.k() + w.k(), g.psk[bank])
            S.dve(lambda e, n=n, bank=bank: e.tensor_tensor(out=modrow.ap[:, n * 512:(n + 1) * 512], in0=g.PS[bank][0:3, :],
                                                              in1=bias.ap[:, n * 512:(n + 1) * 512], op=ALU.add),
                  g.psk[bank] + bias.k(), modrow.k(n * 512, (n + 1) * 512))
        for (ch, gi) in ((1, 0), (4, 1)):
            S.dve(lambda e, ch=ch, gi=gi: e.scalar_tensor_tensor(out=modrow.ap[:, ch * D:(ch + 1) * D], in0=modrow.ap[:, ch * D:(ch + 1) * D],
                                                                  scalar=1.0, in1=gbc.ap[:, gi, :], op0=ALU.add, op1=ALU.mult),
                  modrow.k(ch * D, (ch + 1) * D) + gbc.k(), modrow.k(ch * D, (ch + 1) * D))
        S.dma(lambda e, l=l: e.dma_start(out=g.MODS[l], in_=modrow.ap), modrow.k(), [("MODS", l)])
    mem.release(m0)


def load_mod_bc(g, l, j, ch, dst):
    src = g.MODS[l, j:j + 1, ch * D:(ch + 1) * D].partition_broadcast(128)
    g.S.dma(lambda e: e.dma_start(out=dst.ap, in_=src), [("MODS", l)], dst.k())


def stage_norm(g, l, b, which, segs=("c", "x")):
    nc, S, mem = g.nc, g.S, g.mem
    m0 = mem.mark()
    sbc = mem.alloc([D], F32)
    shbc = mem.alloc([D], F32)
    xts = [mem.alloc([D], F32) for _ in range(2)]
    t1s = [mem.alloc([D], F32) for _ in range(2)]
    hbs = [mem.alloc([D], BF16) for _ in range(2)]
    junk = mem.alloc([D], BF16)
    st = [mem.alloc([2], F32) for _ in range(2)]
    cnt = 0
    for seg in segs:
        j = 2 if seg == "c" else b
        load_mod_bc(g, l, j, 1 if which == 1 else 4, sbc)
        load_mod_bc(g, l, j, 0 if which == 1 else 3, shbc)
        ntile = TC // 128 if seg == "c" else TL // 128
        for i in range(ntile):
            tok0 = i * 128 if seg == "c" else TC + i * 128
            xt, t1, hb, s_ = xts[cnt % 2], t1s[cnt % 2], hbs[cnt % 2], st[cnt % 2]
            bank = 2 + cnt % 2
            cnt += 1
            if seg == "c":
                src, sk = g.XC[b, i * 128:(i + 1) * 128, :], [(("XC", b), i)]
            else:
                src, sk = g.OUT[b, i * 128:(i + 1) * 128, :], [(("XX", b), i)]
            S.dma(lambda e, xt=xt, src=src: e.dma_start(out=xt.ap, in_=src), sk, xt.k())
            S.act(lambda e, xt=xt, s_=s_: e.activation(out=junk.ap, in_=xt.ap, func=AF.Square, accum_out=s_.ap[:, 0:1]),
                  xt.k(), junk.k() + s_.k())
            S.act(lambda e, s_=s_: e.activation(out=s_.ap[:, 1:2], in_=s_.ap[:, 0:1], func=AF.Sqrt, bias=EPS, scale=1.0 / D),
                  s_.k(), s_.k())
            S.dve(lambda e, s_=s_: e.reciprocal(out=s_.ap[:, 1:2], in_=s_.ap[:, 1:2]), s_.k(), s_.k())
            S.dve(lambda e, xt=xt, t1=t1, s_=s_: e.scalar_tensor_tensor(out=t1.ap, in0=xt.ap, scalar=s_.ap[:, 1:2], in1=sbc.ap,
                                                                         op0=ALU.mult, op1=ALU.mult),
                  xt.k() + s_.k() + sbc.k(), t1.k())
            S.pool(lambda e, t1=t1, hb=hb: e.tensor_tensor(out=hb.ap, in0=t1.ap, in1=shbc.ap, op=ALU.add),
                   t1.k() + shbc.k(), hb.k())
            if which == 2:
                S.dma(lambda e, hb=hb, tok0=tok0: e.dma_start(out=g.HTOK[tok0:tok0 + 128, :], in_=hb.ap),
                      hb.k(), [("HTOK", tok0 // 128)])
            for c in range(8):
                S.pe(lambda e, hb=hb, c=c, bank=bank: e.transpose(out=g.PSB[bank][:, c * 128:(c + 1) * 128],
                                                                  in_=hb.ap[:, c * 128:(c + 1) * 128], identity=g.ident_bf.ap),
                     hb.k() + g.ident_bf.k(), g.psk[bank])
            dst = g.hT.ap[:, :, tok0:tok0 + 128]
            hk = []
            for c in range(8):
                hk += g.hT.k(c * TT + tok0, c * TT + tok0 + 128)
            srcp = g.PSB[bank].rearrange("p (c t) -> p c t", c=8)
            if cnt % 2 == 0:
                S.act(lambda e, dst=dst, srcp=srcp: e.activation(out=dst, in_=srcp, func=AF.Copy), g.psk[bank], hk)
            else:
                S.dve(lambda e, dst=dst, srcp=srcp: e.tensor_copy(out=dst, in_=srcp), g.psk[bank], hk)
    mem.release(m0)


def load_w_bf16(g, dst, src_ap, nk=8):
    src = src_ap.rearrange("(k p) n -> p k n", p=128)
    g.S.dma(lambda e: e.dma_start(out=dst.ap, in_=src), [], dst.k(), q="pq")


def stage_proj(g, l, b):
    nc, S, mem, I = g.nc, g.S, g.mem, g.I
    m0 = mem.mark()
    W = I["w_in"][l]
    wsl = [mem.alloc([8, 512], BF16) for _ in range(2)]
    stg = [mem.alloc([512], F32) for _ in range(4)]
    wcnt = [0]
    scnt = [0]
    pcnt = [0]
    slabs = [(s * 512, min(512, TT - s * 512)) for s in range((TT + 511) // 512)]

    def next_w(c0, width):
        w = wsl[wcnt[0] % 2]
        wcnt[0] += 1
        wv = Tn(w.ap[:, :, 0:width], w.off, w.nbytes, w.esize)
        src = W[:, c0:c0 + width].rearrange("(k p) n -> p k n", p=128)
        S.dma(lambda e: e.dma_start(out=wv.ap, in_=src), [], w.k(), q="pq")
        return w

    def evac(kind, out_ap, ps_ap, rk, wk, scale=1.0):
        if kind == "copy":
            if scnt[0] % 2 == 0:
                S.dve(lambda e: e.tensor_copy(out=out_ap, in_=ps_ap), rk, wk)
            else:
                S.act(lambda e: e.activation(out=out_ap, in_=ps_ap, func=AF.Copy), rk, wk)
        elif kind == "scale":
            S.act(lambda e: e.activation(out=out_ap, in_=ps_ap, func=AF.Copy, scale=scale), rk, wk)
        elif kind == "silu":
            S.act(lambda e: e.activation(out=out_ap, in_=ps_ap, func=AF.Silu), rk, wk)
        elif kind == "sigmoid":
            S.act(lambda e: e.activation(out=out_ap, in_=ps_ap, func=AF.Sigmoid), rk, wk)

    fm = [
        (OFF["gla_q"][0], 128, g.QK_GLA, 0, "scale", F32, "QK_GLA"),
        (OFF["gla_k"][0], 128, g.QK_GLA, 128, "copy", F32, "QK_GLA"),
        (OFF["gla_af"][0], 32, g.AFB, 0, "copy", F32, "AFB"),
        (OFF["hg_q"][0], 256, g.Q_HG, 0, "silu", F32, "Q_HG"),
        (OFF["hg_ff"][0], 512, g.ZT_HG, 0, "copy", F32, "ZT_HG"),
        (OFF["merge"][0], 4096, g.MG, 0, "sigmoid", BF16, "MG"),
    ]
    for (c0, ncols, dst, r0, kind, dt, kn) in fm:
        for cs in range(0, ncols, 512):
            cw = min(512, ncols - cs)
            w = next_w(c0 + cs, cw)
            for ch in range(0, cw, 128):
                m = min(128, cw - ch)
                for (t0, tw) in slabs:
                    bank = 4 + pcnt[0] % 4
                    pcnt[0] += 1
                    hk = []
                    for k in range(8):
                        hk += g.hT.k(k * TT + t0, k * TT + t0 + tw)
                    for k in range(8):
                        S.pe(lambda e, w=w, k=k, ch=ch, m=m, bank=bank, t0=t0, tw=tw:
                             e.matmul(g.PS[bank][0:m, 0:tw], lhsT=w.ap[:, k, ch:ch + m], rhs=g.hT.ap[:, k, t0:t0 + tw],
                                      start=(k == 0), stop=(k == 7)),
                             w.k() + hk, g.psk[bank])
                    st = stg[scnt[0] % 4]
                    scnt[0] += 1
                    if dt == BF16:
                        sv = st.ap.bitcast(BF16)[0:m, 0:tw]
                    else:
                        sv = st.ap[0:m, 0:tw]
                    evac(kind, sv, g.PS[bank][0:m, 0:tw], g.psk[bank], st.k(), scale=32 ** -0.5)
                    rr = r0 + cs + ch
                    S.dma(lambda e, dst=dst, rr=rr, m=m, t0=t0, tw=tw, sv=sv: e.dma_start(out=dst[rr:rr + m, t0:t0 + tw], in_=sv),
                          st.k(), [(kn, rr // 128, t0 // 512)])
    tmg = [
        (OFF["gla_v"][0], 256, g.V_GLA, 0, "copy", BF16, "V_GLA"),
        (OFF["gla_g"][0], 256, g.G_GLA, 0, "silu", BF16, "G_GLA"),
        (OFF["hy"][0], 512, g.HY, 0, "copy", F32, "HY"),
        (OFF["hy"][0] + 512, 256, g.HY, 512, "copy", F32, "HY"),
        (OFF["hg_ff"][0], 512, g.Z_HG, 0, "copy", F32, "Z_HG"),
        (OFF["hg_i"][0], 256, g.VG_HG, 0, "copy", BF16, "VG_HG"),
        (OFF["hg_g"][0], 256, g.VG_HG, 256, "silu", BF16, "VG_HG"),
        (OFF["da_q"][0], 512, g.QK_DA, 0, "copy", F32, "QK_DA"),
        (OFF["da_v"][0], 256, g.V_DA, 0, "copy", BF16, "V_DA"),
    ]
    for (c0, ncols, dst, dc0, kind, dt, kn) in tmg:
        w = next_w(c0, ncols)
        for i in range(TT // 128):
            t0 = i * 128
            bank = 4 + pcnt[0] % 4
            pcnt[0] += 1
            hk = []
            for k in range(8):
                hk += g.hT.k(k * TT + t0, k * TT + t0 + 128)
            for k in range(8):
                S.pe(lambda e, w=w, k=k, bank=bank, t0=t0, ncols=ncols:
                     e.matmul(g.PS[bank][:, 0:ncols], lhsT=g.hT.ap[:, k, t0:t0 + 128], rhs=w.ap[:, k, 0:ncols],
                              start=(k == 0), stop=(k == 7)),
                     w.k() + hk, g.psk[bank])
            st = stg[scnt[0] % 4]
            scnt[0] += 1
            if dt == BF16:
                sv = st.ap.bitcast(BF16)[:, 0:ncols]
            else:
                sv = st.ap[:, 0:ncols]
            evac(kind, sv, g.PS[bank][:, 0:ncols], g.psk[bank], st.k())
            S.dma(lambda e, dst=dst, t0=t0, dc0=dc0, ncols=ncols, sv=sv: e.dma_start(out=dst[t0:t0 + 128, dc0:dc0 + ncols], in_=sv),
                  st.k(), [(kn, i, dc0)])
    mem.release(m0)


_CACHE = {}


def make_in_maps(inputs):
    cst = host_consts()
    maps = []
    x = np.ascontiguousarray(inputs["x"], dtype=np.float32)
    ctx = np.ascontiguousarray(inputs["ctx"], dtype=np.float32)
    c = np.asarray(inputs["c"], dtype=np.float32)
    c_ctx = np.asarray(inputs["c_ctx"], dtype=np.float32)
    shared = {}
    for k in inputs:
        if k not in ("x", "c", "ctx", "c_ctx"):
            shared[k] = np.ascontiguousarray(inputs[k], dtype=np.float32)
    for core in range(NCORES):
        b0 = core * NB
        cc = np.stack([c[b0], c[b0 + 1], c_ctx], axis=1)
        ccT = np.ascontiguousarray(cc.reshape(8, 128, 3).transpose(1, 0, 2))
        m = {"x": x[b0:b0 + NB], "ctx": ctx[b0:b0 + NB], "ccT": ccT}
        m.update(shared)
        m.update(cst)
        maps.append(m)
    return maps


def kernel(**inputs):
    if "prog" not in _CACHE:
        _CACHE["prog"] = build_program()
    g = _CACHE["prog"]
    maps = make_in_maps(inputs)
    res = run_bass_kernel_spmd(g.nc, maps, core_ids=list(range(NCORES)))
    out = np.concatenate([np.asarray(r["out"]) for r in res.results], axis=0)
    return out.astype(np.float32)


def stage_recur(g, l, b, kind):
    nc, S, mem, I = g.nc, g.S, g.mem, g.I
    m0 = mem.mark()
    if kind == "gla":
        dk, C, hp, nt = 32, 128, 4, 1
        O = g.O_GLA
        okn = "O_GLA"
    else:
        dk, C, hp, nt = 64, 32, 2, 2
        O = g.O_HG
        okn = "O_HG"
    HD = 4 * dk
    BLK = 512
    tri = mem.alloc([2, C], F32, parts=C)
    msk = mem.alloc([2, hp * C], F32, parts=C)
    hm = mem.alloc([hp, C], F32)
    bd = mem.alloc([hp, 64], F32)
    S.dma(lambda e: e.dma_start(out=tri.ap, in_=I["tri%d" % C]), [], tri.k())
    S.dma(lambda e: e.dma_start(out=msk.ap, in_=I["msk%d" % C]), [], msk.k())
    S.dma(lambda e: e.dma_start(out=hm.ap, in_=I["hm%d" % C]), [], hm.k())
    S.dma(lambda e: e.dma_start(out=bd.ap, in_=I["bd%d" % C]), [], bd.k())
    if kind == "gla":
        S.dve(lambda e: e.tensor_scalar(out=tri.ap, in0=tri.ap, scalar1=-1.0 / 16.0, scalar2=None, op0=ALU.mult), tri.k(), tri.k())
        wa2b = mem.alloc([2, 128], F32, parts=17)
        for d_ in range(2):
            S.dma(lambda e, d_=d_: e.dma_start(out=wa2b.ap[0:16, d_, :], in_=I["gla_wa2"][l, d_]), [], wa2b.k())
            S.dma(lambda e, d_=d_: e.dma_start(out=wa2b.ap[16:17, d_, :], in_=I["gla_ba"][l, d_:d_ + 1, :]), [], wa2b.k())
    else:
        lbb = mem.alloc([2, 256], F32, parts=C)
        omb = mem.alloc([2, 256], F32, parts=C)
        lbp = mem.alloc([2, 2], F32)
        omp = mem.alloc([2, 2], F32)
        if l == 0:
            S.dve(lambda e: e.memset(lbb.ap, 0.0), [], lbb.k())
            S.dve(lambda e: e.memset(omb.ap, 1.0), [], omb.k())
            S.dve(lambda e: e.memset(lbp.ap, 0.0), [], lbp.k())
            S.dve(lambda e: e.memset(omp.ap, 1.0), [], omp.k())
        else:
            t0_ = mem.alloc([2, 256], F32, parts=C)
            S.dma(lambda e: e.dma_start(out=lbb.ap, in_=I["hg_lower"][1:2].rearrange("o d e -> o (d e)").partition_broadcast(C)
                                        .rearrange("p o (d e) -> p (o d) e", d=2)), [], lbb.k())
            S.dma(lambda e: e.dma_start(out=t0_.ap, in_=I["hg_lower"][0:1].rearrange("o d e -> o (d e)").partition_broadcast(C)
                                        .rearrange("p o (d e) -> p (o d) e", d=2)), [], t0_.k())
            S.dve(lambda e: e.tensor_tensor(out=lbb.ap, in0=lbb.ap, in1=t0_.ap, op=ALU.subtract), lbb.k() + t0_.k(), lbb.k())
            S.act(lambda e: e.activation(out=lbb.ap, in_=lbb.ap, func=AF.Sigmoid), lbb.k(), lbb.k())
            S.dve(lambda e: e.tensor_scalar(out=omb.ap, in0=lbb.ap, scalar1=-1.0, scalar2=1.0, op0=ALU.mult, op1=ALU.add), lbb.k(), omb.k())
            t1_ = mem.alloc([2, 2], F32)
            S.dma(lambda e: e.dma_start(out=lbp.ap, in_=I["hg_lower"][1].rearrange("d (t p) -> p d t", p=128), allow_slow_non_contiguous=True), [], lbp.k())
            S.dma(lambda e: e.dma_start(out=t1_.ap, in_=I["hg_lower"][0].rearrange("d (t p) -> p d t", p=128), allow_slow_non_contiguous=True), [], t1_.k())
            S.dve(lambda e: e.tensor_tensor(out=lbp.ap, in0=lbp.ap, in1=t1_.ap, op=ALU.subtract), lbp.k() + t1_.k(), lbp.k())
            S.act(lambda e: e.activation(out=lbp.ap, in_=lbp.ap, func=AF.Sigmoid), lbp.k(), lbp.k())
            S.dve(lambda e: e.tensor_scalar(out=omp.ap, in0=lbp.ap, scalar1=-1.0, scalar2=1.0, op0=ALU.mult, op1=ALU.add), lbp.k(), omp.k())
    St = [mem.alloc([64], F32) for _ in range(nt)]
    Sb = [mem.alloc([64], BF16) for _ in range(nt)]
    qTb = [[mem.alloc([BLK], F32) for _ in range(nt)] for _ in range(2)]
    kTb = [[mem.alloc([BLK], F32) for _ in range(nt)] for _ in range(2)]
    nchb = BLK // C
    lgb = [mem.alloc([nchb, HD], F32, parts=C) for _ in range(2)]
    vb = [mem.alloc([nchb, 256], BF16, parts=C) for _ in range(2)]
    if kind == "gla":
        af1 = [mem.alloc([BLK], F32, parts=17) for _ in range(2)]
        lgt = [mem.alloc([128], F32) for _ in range(2)]
    E1 = [mem.alloc([C], F32) for _ in range(2)]
    E2 = [mem.alloc([C], F32) for _ in range(2)]
    qe = [mem.alloc([C], BF16) for _ in range(2)]
    ke = [mem.alloc([C], BF16) for _ in range(2)]
    qeb = [mem.alloc([hp, C], BF16) for _ in range(2)]
    keT = [mem.alloc([128], BF16, parts=C) for _ in range(2)]
    Am = [mem.alloc([hp * C], BF16, parts=C) for _ in range(2)]
    tmpS = [mem.alloc([64], F32) for _ in range(2)]
    tmp2 = [mem.alloc([hp, 64], F32) for _ in range(2)]
    osb = [mem.alloc([256], F32, parts=C) for _ in range(2)]
    cc = [0]
    bcnt = [0]
    ocnt = [0]
    for dr in range(2):
        for t in range(nt):
            S.dve(lambda e, t=t: e.memset(St[t].ap, 0.0), [], St[t].k())
            S.pool(lambda e, t=t: e.memset(Sb[t].ap, 0.0), [], Sb[t].k())
        blocks = [(0, TC)] + [(TC + i * BLK, BLK) for i in range(TL // BLK)]
        if dr == 1:
            blocks = [(0, TC)] + [(TC + i * BLK, BLK) for i in reversed(range(TL // BLK))]
        for (t0, bw) in blocks:
            bi = bcnt[0] % 2
            bcnt[0] += 1
            nch = bw // C
            qT, kT, lg, vv = qTb[bi], kTb[bi], lgb[bi], vb[bi]
            if kind == "gla":
                S.dma(lambda e, qT=qT, t0=t0, bw=bw: e.dma_start(out=qT[0].ap[:, 0:bw], in_=g.QK_GLA[0:128, t0:t0 + bw]),
                      dkeys_fm("QK_GLA", 0, t0, bw), qT[0].k())
                S.dma(lambda e, kT=kT, t0=t0, bw=bw: e.dma_start(out=kT[0].ap[:, 0:bw], in_=g.QK_GLA[128:256, t0:t0 + bw]),
                      dkeys_fm("QK_GLA", 1, t0, bw), kT[0].k())
                a1 = af1[bi]
                S.dma(lambda e, a1=a1, t0=t0, bw=bw, dr=dr: e.dma_start(out=a1.ap[0:16, 0:bw], in_=g.AFB[dr * 16:(dr + 1) * 16, t0:t0 + bw]),
                      dkeys_fm("AFB", 0, t0, bw), a1.k())
                S.dma(lambda e, a1=a1, bw=bw: e.dma_start(out=a1.ap[16:17, 0:bw], in_=I["ones_row"][0:1, 0:bw]), [], a1.k())
                for c in range(nch):
                    pb = cc[0] % 2
                    lt = lgt[cc[0] % 2]
                    S.pe(lambda e, a1=a1, c=c, pb=pb, dr=dr: e.matmul(g.PS[pb][:, 0:128], lhsT=a1.ap[:, c * 128:(c + 1) * 128],
                                                                       rhs=wa2b.ap[:, dr, :], start=True, stop=True),
                         a1.k() + wa2b.k(), g.psk[pb])
                    S.act(lambda e, lt=lt, pb=pb: e.activation(out=lt.ap, in_=g.PS[pb][:, 0:128], func=AF.Exp, scale=-1.0),
                          g.psk[pb], lt.k())
                    S.act(lambda e, lt=lt, lg=lg, c=c: e.activation(out=lg.ap[:, c, :], in_=lt.ap, func=AF.Ln, bias=1.0, scale=1.0),
                          lt.k(), lg.k(c * HD, (c + 1) * HD))
                    cc[0] += 1
                S.dma(lambda e, vv=vv, t0=t0, bw=bw, nch=nch: e.dma_start(out=vv.ap[:, 0:nch, :],
                                                                           in_=g.V_GLA[t0:t0 + bw, :].rearrange("(c s) e -> s c e", s=C)),
                      dkeys_tm("V_GLA", 0, t0, bw), vv.k())
            else:
                for t in range(nt):
                    S.dma(lambda e, qT=qT, t=t, t0=t0, bw=bw: e.dma_start(out=qT[t].ap[:, 0:bw], in_=g.Q_HG[t * 128:(t + 1) * 128, t0:t0 + bw]),
                          dkeys_fm("Q_HG", t, t0, bw), qT[t].k())
                    r0 = dr * 256 + t * 128
                    S.dma(lambda e, kT=kT, t=t, t0=t0, bw=bw, r0=r0: e.dma_start(out=kT[t].ap[:, 0:bw], in_=g.ZT_HG[r0:r0 + 128, t0:t0 + bw]),
                          dkeys_fm("ZT_HG", r0 // 128, t0, bw), kT[t].k())
                    S.act(lambda e, kT=kT, t=t, bw=bw: e.activation(out=kT[t].ap[:, 0:bw], in_=kT[t].ap[:, 0:bw], func=AF.Sigmoid, scale=-1.0),
                          kT[t].k(), kT[t].k())
                    S.dve(lambda e, kT=kT, t=t, bw=bw, dr=dr: e.tensor_scalar(out=kT[t].ap[:, 0:bw], in0=kT[t].ap[:, 0:bw],
                                                                               scalar1=omp.ap[:, dr, t:t + 1], scalar2=None, op0=ALU.mult),
                          kT[t].k() + omp.k(), kT[t].k())
                S.dma(lambda e, lg=lg, t0=t0, bw=bw, nch=nch, dr=dr: e.dma_start(out=lg.ap[:, 0:nch, :],
                      in_=g.Z_HG[t0:t0 + bw, dr * 256:(dr + 1) * 256].rearrange("(c s) e -> s c e", s=C)),
                      dkeys_tm("Z_HG", 0, t0, bw), lg.k())
                lgv = lg.ap[:, 0:nch, :]
                S.act(lambda e, lgv=lgv: e.activation(out=lgv, in_=lgv, func=AF.Sigmoid), lg.k(), lg.k())
                S.dve(lambda e, lgv=lgv, nch=nch, dr=dr: e.tensor_tensor(out=lgv, in0=lgv, in1=omb.ap[:, dr:dr + 1, :].broadcast_to([C, nch, 256]),
                                                                          op=ALU.mult), lg.k() + omb.k(), lg.k())
                S.dve(lambda e, lgv=lgv, nch=nch, dr=dr: e.tensor_tensor(out=lgv, in0=lgv, in1=lbb.ap[:, dr:dr + 1, :].broadcast_to([C, nch, 256]),
                                                                          op=ALU.add), lg.k() + lbb.k(), lg.k())
                S.dve(lambda e, lgv=lgv: e.tensor_scalar(out=lgv, in0=lgv, scalar1=1e-20, scalar2=None, op0=ALU.max), lg.k(), lg.k())
                S.act(lambda e, lgv=lgv: e.activation(out=lgv, in_=lgv, func=AF.Ln), lg.k(), lg.k())
                S.dma(lambda e, vv=vv, t0=t0, bw=bw, nch=nch: e.dma_start(out=vv.ap[:, 0:nch, :],
                                                                           in_=g.VG_HG[t0:t0 + bw, 0:256].rearrange("(c s) e -> s c e", s=C)),
                      dkeys_tm("VG_HG", 0, t0, bw), vv.k())
            chs = list(range(nch)) if dr == 0 else list(reversed(range(nch)))
            for c in chs:
                o_bank = 5 + ocnt[0] % 2
                ob = osb[ocnt[0] % 2]
                ocnt[0] += 1
                for t in range(nt):
                    i2 = cc[0] % 2
                    cc[0] += 1
                    e1, e2, qe_, ke_, qb_, kt_, am_, ts_, t2_ = E1[i2], E2[i2], qe[i2], ke[i2], qeb[i2], keT[i2], Am[i2], tmpS[i2], tmp2[i2]
                    bb = i2
                    ab = 3 + i2
                    S.pe(lambda e, lg=lg, c=c, t=t, bb=bb, dr=dr: e.matmul(g.PS[bb][:, 0:C], lhsT=lg.ap[:, c, t * 128:(t + 1) * 128],
                                                                            rhs=tri.ap[:, dr, :], start=True, stop=True),
                         lg.k(c * HD, (c + 1) * HD) + tri.k(), g.psk[bb])
                    S.act(lambda e, e1=e1, bb=bb: e.activation(out=e1.ap, in_=g.PS[bb][:, 0:C], func=AF.Exp), g.psk[bb], e1.k())
                    S.act(lambda e, e2=e2, bb=bb: e.activation(out=e2.ap, in_=g.PS[bb][:, 0:C], func=AF.Exp, scale=-1.0), g.psk[bb], e2.k())
                    S.dve(lambda e, qe_=qe_, e1=e1, qT=qT, t=t, c=c: e.tensor_tensor(out=qe_.ap, in0=qT[t].ap[:, c * C:(c + 1) * C], in1=e1.ap, op=ALU.mult),
                          qT[t].k() + e1.k(), qe_.k())
                    S.dve(lambda e, ke_=ke_, e2=e2, kT=kT, t=t, c=c: e.tensor_tensor(out=ke_.ap, in0=kT[t].ap[:, c * C:(c + 1) * C], in1=e2.ap, op=ALU.mult),
                          kT[t].k() + e2.k(), ke_.k())
                    S.pool(lambda e, qb_=qb_, qe_=qe_: e.tensor_tensor(out=qb_.ap, in0=qe_.ap[:, None, :].broadcast_to([128, hp, C]), in1=hm.ap, op=ALU.mult),
                           qe_.k() + hm.k(), qb_.k())
                    S.pe(lambda e, ke_=ke_: e.transpose(out=g.PSB[2][0:C, 0:128], in_=ke_.ap, identity=g.ident_bf.ap),
                         ke_.k() + g.ident_bf.k(), g.psk[2])
                    S.act(lambda e, kt_=kt_: e.activation(out=kt_.ap, in_=g.PSB[2][0:C, 0:128], func=AF.Copy), g.psk[2], kt_.k())
                    S.pe(lambda e, ke_=ke_, qb_=qb_, ab=ab: e.matmul(g.PS[ab][0:C, 0:hp * C], lhsT=ke_.ap, rhs=qb_.ap.rearrange("p h c -> p (h c)"),
                                                                       start=True, stop=True),
                         ke_.k() + qb_.k(), g.psk[ab])
                    S.dve(lambda e, am_=am_, ab=ab, dr=dr: e.tensor_tensor(out=am_.ap, in0=g.PS[ab][0:C, 0:hp * C], in1=msk.ap[:, dr, :], op=ALU.mult),
                          g.psk[ab] + msk.k(), am_.k())
                    for hh in range(hp):
                        hcol = (t * hp + hh) * 64
                        S.pe(lambda e, am_=am_, hh=hh, hcol=hcol, vv=vv, c=c, o_bank=o_bank:
                             e.matmul(g.PS[o_bank][0:C, hcol:hcol + 64], lhsT=am_.ap[:, hh * C:(hh + 1) * C], rhs=vv.ap[:, c, hcol:hcol + 64],
                                      start=True, stop=False),
                             am_.k() + vv.k(), g.psk[o_bank])
                        S.pe(lambda e, qb_=qb_, hh=hh, hcol=hcol, t=t, o_bank=o_bank:
                             e.matmul(g.PS[o_bank][0:C, hcol:hcol + 64], lhsT=qb_.ap[:, hh, :], rhs=Sb[t].ap,
                                      start=False, stop=True),
                             qb_.k() + Sb[t].k(), g.psk[o_bank])
                    S.pe(lambda e, kt_=kt_, vv=vv, c=c, t=t: e.matmul(g.PS[7][:, 0:hp * 64], lhsT=kt_.ap, rhs=vv.ap[:, c, t * hp * 64:(t + 1) * hp * 64],
                                                                       start=True, stop=True),
                         kt_.k() + vv.k(), g.psk[7])
                    ecol = (C - 1) if dr == 0 else 0
                    S.dve(lambda e, ts_=ts_, t=t, e1=e1, ecol=ecol: e.tensor_scalar(out=ts_.ap, in0=St[t].ap, scalar1=e1.ap[:, ecol:ecol + 1], scalar2=None, op0=ALU.mult),
                          St[t].k() + e1.k(), ts_.k())
                    S.dve(lambda e, t2_=t2_: e.tensor_tensor(out=t2_.ap.rearrange("p h e -> p (h e)"), in0=g.PS[7][:, 0:hp * 64], in1=bd.ap.rearrange("p h e -> p (h e)"), op=ALU.mult),
                          g.psk[7] + bd.k(), t2_.k())
                    for hh in range(hp):
                        src1 = ts_ if hh == 0 else St[t]
                        S.dve(lambda e, t2_=t2_, hh=hh, t=t, e1=e1, ecol=ecol, src1=src1:
                              e.scalar_tensor_tensor(out=St[t].ap, in0=t2_.ap[:, hh, :], scalar=e1.ap[:, ecol:ecol + 1], in1=src1.ap, op0=ALU.mult, op1=ALU.add),
                              t2_.k() + e1.k() + src1.k() + St[t].k(), St[t].k())
                    S.act(lambda e, t=t: e.activation(out=Sb[t].ap, in_=St[t].ap, func=AF.Copy), St[t].k(), Sb[t].k())
                S.dve(lambda e, ob=ob, o_bank=o_bank: e.tensor_copy(out=ob.ap, in_=g.PS[o_bank][0:C, 0:256]), g.psk[o_bank], ob.k())
                tt = t0 + c * C
                S.dma(lambda e, ob=ob, tt=tt, dr=dr: e.dma_start(out=O[dr, tt:tt + C, :], in_=ob.ap), ob.k(), [(okn, dr, u) for u in range(tt // 32, (tt + C) // 32)])
    mem.release(m0)


def dkeys_fm(name, rowtile, t0, bw):
    return [(name, rowtile, s) for s in range(t0 // 512, (t0 + bw - 1) // 512 + 1)]


def dkeys_tm(name, dc0, t0, bw):
    return [(name, i, dc0) for i in range(t0 // 128, (t0 + bw - 1) // 128 + 1)]


def stage_branch_norm(g, l, b, Osrc, okn, ndir, gate, gate_c0, gkn, gname, cscale, br, tok_lo=0):
    nc, S, mem, I = g.nc, g.S, g.mem, g.I
    if isinstance(Osrc, str):
        Osrc = getattr(g, Osrc)
    if isinstance(gate, str):
        gate = getattr(g, gate)
    m0 = mem.mark()
    gbc = mem.alloc([64], F32)
    S.dma(lambda e: e.dma_start(out=gbc.ap, in_=I[gname][l:l + 1, :].partition_broadcast(128)), [], gbc.k())
    if cscale != 1.0:
        S.dve(lambda e: e.tensor_scalar(out=gbc.ap, in0=gbc.ap, scalar1=float(cscale), scalar2=None, op0=ALU.mult), gbc.k(), gbc.k())
    of = [mem.alloc([256], F32) for _ in range(2)]
    ob = [mem.alloc([256], F32) for _ in range(2)]
    gt = [mem.alloc([256], BF16) for _ in range(2)]
    sq = [mem.alloc([256], F32) for _ in range(2)]
    ss = [mem.alloc([8], F32) for _ in range(2)]
    yb = [mem.alloc([256], BF16) for _ in range(2)]
    stg = [mem.alloc([2, 128], BF16) for _ in range(2)]
    for i in range(tok_lo // 128, TT // 128):
        t0 = i * 128
        u = i % 2
        o1, o2, g_, sq_, ss_, y_, st_ = of[u], ob[u], gt[u], sq[u], ss[u], yb[u], stg[u]
        okeys = lambda dr: [(okn, dr, v) for v in range(t0 // 32, t0 // 32 + 4)]
        S.dma(lambda e, o1=o1, t0=t0: e.dma_start(out=o1.ap, in_=Osrc[0, t0:t0 + 128, :]), okeys(0), o1.k())
        if ndir == 2:
            S.dma(lambda e, o2=o2, t0=t0: e.dma_start(out=o2.ap, in_=Osrc[1, t0:t0 + 128, :]), okeys(1), o2.k())
            S.pool(lambda e, o1=o1, o2=o2: e.tensor_tensor(out=o1.ap, in0=o1.ap, in1=o2.ap, op=ALU.add), o1.k() + o2.k(), o1.k())
        if gate is not None:
            S.dma(lambda e, g_=g_, t0=t0: e.dma_start(out=g_.ap, in_=gate[t0:t0 + 128, gate_c0:gate_c0 + 256]), [(gkn, i, gate_c0)], g_.k())
        S.dve(lambda e, o1=o1, sq_=sq_: e.tensor_tensor(out=sq_.ap, in0=o1.ap, in1=o1.ap, op=ALU.mult), o1.k(), sq_.k())
        S.dve(lambda e, sq_=sq_, ss_=ss_: e.tensor_reduce(out=ss_.ap[:, 0:4], in_=sq_.ap.rearrange("p (h e) -> p h e", h=4), axis=AX.X, op=ALU.add),
              sq_.k(), ss_.k())
        S.act(lambda e, ss_=ss_: e.activation(out=ss_.ap[:, 4:8], in_=ss_.ap[:, 0:4], func=AF.Sqrt, bias=EPS, scale=1.0 / 64), ss_.k(), ss_.k())
        S.dve(lambda e, ss_=ss_: e.reciprocal(out=ss_.ap[:, 4:8], in_=ss_.ap[:, 4:8]), ss_.k(), ss_.k())
        S.dve(lambda e, o1=o1, ss_=ss_, sq_=sq_: e.tensor_tensor(out=sq_.ap.rearrange("p (h e) -> p h e", h=4), in0=o1.ap.rearrange("p (h e) -> p h e", h=4),
                                                                in1=ss_.ap[:, 4:8, None].broadcast_to([128, 4, 64]), op=ALU.mult),
              o1.k() + ss_.k(), sq_.k())
        if gate is not None:
            S.pool(lambda e, sq_=sq_: e.tensor_tensor(out=sq_.ap.rearrange("p (h e) -> p h e", h=4), in0=sq_.ap.rearrange("p (h e) -> p h e", h=4),
                                                     in1=gbc.ap[:, None, :].broadcast_to([128, 4, 64]), op=ALU.mult), sq_.k() + gbc.k(), sq_.k())
            S.dve(lambda e, sq_=sq_, g_=g_, y_=y_: e.tensor_tensor(out=y_.ap, in0=sq_.ap, in1=g_.ap, op=ALU.mult), sq_.k() + g_.k(), y_.k())
        else:
            S.pool(lambda e, sq_=sq_, y_=y_: e.tensor_tensor(out=y_.ap.rearrange("p (h e) -> p h e", h=4), in0=sq_.ap.rearrange("p (h e) -> p h e", h=4),
                                                            in1=gbc.ap[:, None, :].broadcast_to([128, 4, 64]), op=ALU.mult), sq_.k() + gbc.k(), y_.k())
        bank = 2 + u
        for c in range(2):
            S.pe(lambda e, y_=y_, c=c, bank=bank: e.transpose(out=g.PSB[bank][:, c * 128:(c + 1) * 128], in_=y_.ap[:, c * 128:(c + 1) * 128],
                                                              identity=g.ident_bf.ap), y_.k() + g.ident_bf.k(), g.psk[bank])
        S.act(lambda e, st_=st_, bank=bank: e.activation(out=st_.ap, in_=g.PSB[bank][:, 0:256].rearrange("p (c t) -> p c t", c=2), func=AF.Copy),
              g.psk[bank], st_.k())
        S.dma(lambda e, st_=st_, t0=t0: e.dma_start(out=g.BRT[br].rearrange("(c p) t -> p c t", p=128)[:, :, t0:t0 + 128], in_=st_.ap),
              st_.k(), [("BRT", br, i)])
    mem.release(m0)


def stage_merge(g, l, b, tok_lo=0):
    nc, S, mem, I = g.nc, g.S, g.mem, g.I
    m0 = mem.mark()
    wb = mem.alloc([4, 2, D], BF16)
    for i in range(4):
        S.dma(lambda e, i=i: e.dma_start(out=wb.ap[:, i], in_=I["w_branch"][l, i].rearrange("(k p) n -> p k n", p=128)), [], wb.k(), q="pq")
    wo = mem.alloc([8, D], BF16)
    S.dma(lambda e: e.dma_start(out=wo.ap, in_=I["w_out"][l].rearrange("(k p) n -> p k n", p=128)), [], wo.k(), q="pq")
    mT = g.hT
    brt = [mem.alloc([4, 2, 512], BF16) for _ in range(2)]
    gts = [mem.alloc([512], BF16) for _ in range(4)]
    tm = [mem.alloc([512], F32) for _ in range(4)]
    slabs = [(s * 512, min(512, TT - s * 512)) for s in range((TT + 511) // 512) if s * 512 + 512 > tok_lo]
    gc = [0]
    for si, (t0, tw) in enumerate(slabs):
        bt = brt[si % 2]
        for i in range(4):
            S.dma(lambda e, bt=bt, i=i, t0=t0, tw=tw: e.dma_start(out=bt.ap[:, i, :, 0:tw], in_=g.BRT[i].rearrange("(c p) t -> p c t", p=128)[:, :, t0:t0 + tw]),
                  [("BRT", i, v) for v in range(t0 // 128, (t0 + tw) // 128)], bt.k())
        for c in range(8):
            for i in range(4):
                bank = 4 + i
                for kc in range(2):
                    S.pe(lambda e, bt=bt, i=i, kc=kc, c=c, bank=bank, tw=tw: e.matmul(g.PS[bank][:, 0:tw], lhsT=wb.ap[:, i, kc, c * 128:(c + 1) * 128],
                                                                                     rhs=bt.ap[:, i, kc, 0:tw], start=(kc == 0), stop=(kc == 1)),
                         wb.k() + bt.k(), g.psk[bank])
                gt = gts[gc[0] % 4]
                gc[0] += 1
                r0 = i * 1024 + c * 128
                S.dma(lambda e, gt=gt, r0=r0, t0=t0, tw=tw: e.dma_start(out=gt.ap[:, 0:tw], in_=g.MG[r0:r0 + 128, t0:t0 + tw]),
                      [("MG", r0 // 128, t0 // 512)], gt.k())
                S.dve(lambda e, i=i, gt=gt, bank=bank, tw=tw: e.tensor_tensor(out=tm[i].ap[:, 0:tw], in0=g.PS[bank][:, 0:tw], in1=gt.ap[:, 0:tw], op=ALU.mult),
                      g.psk[bank] + gt.k(), tm[i].k())
            S.pool(lambda e, tw=tw: e.tensor_tensor(out=tm[0].ap[:, 0:tw], in0=tm[0].ap[:, 0:tw], in1=tm[1].ap[:, 0:tw], op=ALU.add), tm[0].k() + tm[1].k(), tm[0].k())
            S.pool(lambda e, tw=tw: e.tensor_tensor(out=tm[2].ap[:, 0:tw], in0=tm[2].ap[:, 0:tw], in1=tm[3].ap[:, 0:tw], op=ALU.add), tm[2].k() + tm[3].k(), tm[2].k())
            S.pool(lambda e, c=c, t0=t0, tw=tw: e.tensor_tensor(out=mT.ap[:, c, t0:t0 + tw], in0=tm[0].ap[:, 0:tw], in1=tm[2].ap[:, 0:tw], op=ALU.add),
                   tm[0].k() + tm[2].k(), mT.k(c * TT + t0, c * TT + t0 + tw))
    gbc = [mem.alloc([D], F32) for _ in range(2)]
    load_mod_bc(g, l, 2, 2, gbc[0])
    load_mod_bc(g, l, b, 2, gbc[1])
    xr = [mem.alloc([D], F32) for _ in range(2)]
    tp = [mem.alloc([D], F32) for _ in range(2)]
    for i in range(tok_lo // 128, TT // 128):
        t0 = i * 128
        u = i % 2
        isc = t0 < TC
        gb = gbc[0] if isc else gbc[1]
        if isc:
            dst, xk = g.XC[b, t0:t0 + 128, :], [(("XC", b), i)]
        else:
            dst, xk = g.OUT[b, t0 - TC:t0 - TC + 128, :], [(("XX", b), (t0 - TC) // 128)]
        x_, t_ = xr[u], tp[u]
        S.dma(lambda e, x_=x_, dst=dst: e.dma_start(out=x_.ap, in_=dst), xk, x_.k())
        mk = []
        for k in range(8):
            mk += mT.k(k * TT + t0, k * TT + t0 + 128)
        for hf in range(2):
            bank = 2 * u + hf
            for k in range(8):
                S.pe(lambda e, k=k, hf=hf, bank=bank, t0=t0: e.matmul(g.PS[bank], lhsT=mT.ap[:, k, t0:t0 + 128], rhs=wo.ap[:, k, hf * 512:(hf + 1) * 512],
                                                                      start=(k == 0), stop=(k == 7)), mk + wo.k(), g.psk[bank])
            S.dve(lambda e, t_=t_, hf=hf, bank=bank, gb=gb: e.tensor_tensor(out=t_.ap[:, hf * 512:(hf + 1) * 512], in0=g.PS[bank], in1=gb.ap[:, hf * 512:(hf + 1) * 512], op=ALU.mult),
                  g.psk[bank] + gb.k(), t_.k(hf * 512, (hf + 1) * 512))
        S.pool(lambda e, x_=x_, t_=t_: e.tensor_tensor(out=x_.ap, in0=x_.ap, in1=t_.ap, op=ALU.add), x_.k() + t_.k(), x_.k())
        S.dma(lambda e, x_=x_, dst=dst: e.dma_start(out=dst, in_=x_.ap), x_.k(), xk)
    mem.release(m0)


def bound_reg(g, e, bound):
    if not hasattr(g, "_bregs"):
        g._bregs = {}
    if bound not in g._bregs:
        g._bregs[bound] = e.to_reg(bound)
    return g._bregs[bound]


def stage_moe(g, l, b, do_ctx):
    nc, S, mem, I = g.nc, g.S, g.mem, g.I
    m0 = mem.mark()
    segs = [("x", TC, TL, 512, b, b * TL)]
    if do_ctx:
        segs.append(("c", 0, TC, 32, 2, b * TC))
    rw = mem.alloc([8, 16], BF16)
    S.dma(lambda e: e.dma_start(out=rw.ap, in_=I["moe_router"][l].rearrange("(k p) n -> p k n", p=128)), [], rw.k(), q="pq")
    NS = 5
    idxg = mem.alloc([NS, 16], I32)
    idxs = mem.alloc([NS, 16], I32)
    gT = mem.alloc([NS, 16], F32)
    S.dve(lambda e: e.memset(idxg.ap, 0), [], idxg.k())
    S.dve(lambda e: e.memset(idxs.ap, 0), [], idxs.k())
    m1 = mem.mark()
    affT = mem.alloc([TT], F32, parts=16)
    vals = mem.alloc([512], F32, parts=16)
    idx = mem.alloc([512], U32, parts=16)
    idxf = mem.alloc([512], F32, parts=16)
    sm = [mem.alloc([4], F32) for _ in range(2)]
    ex = [mem.alloc([16], F32) for _ in range(2)]
    tlo = 0 if do_ctx else TC
    for i in range(tlo // 128, TT // 128):
        t0 = i * 128
        u = i % 2
        s_, e_ = sm[u], ex[u]
        hk = []
        for k in range(8):
            hk += g.hT.k(k * TT + t0, k * TT + t0 + 128)
        for k in range(8):
            S.pe(lambda e, k=k, u=u, t0=t0: e.matmul(g.PS[u][:, 0:16], lhsT=g.hT.ap[:, k, t0:t0 + 128], rhs=rw.ap[:, k, :], start=(k == 0), stop=(k == 7)),
                 hk + rw.k(), g.psk[u])
        S.dve(lambda e, s_=s_, u=u: e.tensor_reduce(out=s_.ap[:, 0:1], in_=g.PS[u][:, 0:16], axis=AX.X, op=ALU.max), g.psk[u], s_.k())
        S.dve(lambda e, s_=s_: e.tensor_scalar(out=s_.ap[:, 1:2], in0=s_.ap[:, 0:1], scalar1=-1.0, scalar2=None, op0=ALU.mult), s_.k(), s_.k())
        S.act(lambda e, s_=s_, e_=e_, u=u: e.activation(out=e_.ap, in_=g.PS[u][:, 0:16], func=AF.Exp, bias=s_.ap[:, 1:2], scale=1.0, accum_out=s_.ap[:, 2:3]),
              g.psk[u] + s_.k(), e_.k() + s_.k())
        S.dve(lambda e, s_=s_: e.reciprocal(out=s_.ap[:, 3:4], in_=s_.ap[:, 2:3]), s_.k(), s_.k())
        S.dve(lambda e, s_=s_, e_=e_: e.tensor_scalar(out=e_.ap, in0=e_.ap, scalar1=s_.ap[:, 3:4], scalar2=None, op0=ALU.mult), e_.k() + s_.k(), e_.k())
        S.pe(lambda e, e_=e_: e.transpose(out=g.PS[2][0:16, 0:128], in_=e_.ap, identity=g.ident_f.ap), e_.k() + g.ident_f.k(), g.psk[2])
        S.act(lambda e, t0=t0: e.activation(out=affT.ap[:, t0:t0 + 128], in_=g.PS[2][0:16, 0:128], func=AF.Copy), g.psk[2], affT.k(t0, t0 + 128))
    for (sg, s0, slen, cap, jm, soff) in segs:
        work = affT.ap[:, s0:s0 + slen]
        wk = affT.k(s0, s0 + slen)
        for r in range(cap // 8):
            S.dve(lambda e, r=r, work=work: e.max(out=vals.ap[:, r * 8:(r + 1) * 8], in_=work), wk, vals.k(r * 8, (r + 1) * 8))
            S.dve(lambda e, r=r, work=work: e.max_index(out=idx.ap[:, r * 8:(r + 1) * 8], in_max=vals.ap[:, r * 8:(r + 1) * 8], in_values=work),
                  wk + vals.k(r * 8, (r + 1) * 8), idx.k(r * 8, (r + 1) * 8))
            S.dve(lambda e, r=r, work=work: e.match_replace(out=work, in_to_replace=vals.ap[:, r * 8:(r + 1) * 8], in_values=work, imm_value=-1.0),
                  wk + vals.k(r * 8, (r + 1) * 8), wk)
        S.dve(lambda e, cap=cap: e.tensor_copy(out=idxf.ap[:, 0:cap], in_=idx.ap[:, 0:cap]), idx.k(), idxf.k())
        nblk = (cap + 127) // 128
        for jb in range(nblk):
            ns = min(128, cap - jb * 128)
            j = jb if sg == "x" else 4
            S.pe(lambda e, jb=jb, ns=ns: e.transpose(out=g.PS[2][0:ns, 0:16], in_=idxf.ap[:, jb * 128:jb * 128 + ns], identity=g.ident_f.ap[0:16, 0:16]),
                 idxf.k() + g.ident_f.k(), g.psk[2])
            S.dve(lambda e, j=j, ns=ns, s0=s0: e.tensor_scalar(out=idxg.ap[0:ns, j, :], in0=g.PS[2][0:ns, 0:16], scalar1=float(s0), scalar2=None, op0=ALU.add),
                  g.psk[2], idxg.k())
            S.dve(lambda e, j=j, ns=ns, soff=soff: e.tensor_scalar(out=idxs.ap[0:ns, j, :], in0=g.PS[2][0:ns, 0:16], scalar1=float(soff), scalar2=None, op0=ALU.add),
                  g.psk[2], idxs.k())
            S.pe(lambda e, jb=jb, ns=ns: e.transpose(out=g.PS[3][0:ns, 0:16], in_=vals.ap[:, jb * 128:jb * 128 + ns], identity=g.ident_f.ap[0:16, 0:16]),
                 vals.k() + g.ident_f.k(), g.psk[3])
            S.act(lambda e, j=j, ns=ns: e.activation(out=gT.ap[0:ns, j, :], in_=g.PS[3][0:ns, 0:16], func=AF.Copy), g.psk[3], gT.k())
    mem.release(m1)
    wbuf = [mem.alloc([8, D], BF16) for _ in range(4)]
    wcnt = [0]
    NSL = 544
    xg = [mem.alloc([D], BF16) for _ in range(2)]
    xgT = mem.alloc([8, NSL], BF16)
    uT = mem.alloc([8, NSL], BF16)
    tmpa = [mem.alloc([512], F32) for _ in range(2)]
    ysb = [mem.alloc([D], F32) for _ in range(2)]
    mbc = {}
    for (sg, s0, slen, cap, jm, soff) in segs:
        mbc[sg] = mem.alloc([D], F32)
        load_mod_bc(g, l, jm, 5, mbc[sg])
    outflat = g.OUT.rearrange("a t d -> (a t) d")
    xcflat = g.XC.rearrange("a t d -> (a t) d")
    xxkeys = dkeys(("XX", b), 0, TL, 128)
    xckeys = dkeys(("XC", b), 0, TC, 128)
    htk = [("HTOK", i) for i in range(TT // 128)]

    def next_w(src):
        w = wbuf[wcnt[0] % 4]
        wcnt[0] += 1
        S.dma(lambda e: e.dma_start(out=w.ap, in_=src.rearrange("(k p) n -> p k n", p=128)), [], w.k(), q="pq")
        return w

    blocks = [("x", j, 128, j * 128) for j in range(4)]
    slabs = [(0, 512)]
    if do_ctx:
        blocks.append(("c", 4, 32, 512))
        slabs.append((512, 32))
    gcnt = [0]
    for ex_ in range(16):
        w1 = next_w(I["moe_w1"][l, ex_])
        w3 = next_w(I["moe_w3"][l, ex_])
        for (sg, j, ns, col0) in blocks:
            xg_ = xg[gcnt[0] % 2]
            gcnt[0] += 1
            S.dma(lambda e, xg_=xg_, j=j, ns=ns, ex_=ex_: e.indirect_dma_start(out=xg_.ap[0:ns, :], out_offset=None, in_=g.HTOK,
                                                                               in_offset=bass.IndirectOffsetOnAxis(ap=idxg.ap[0:ns, j, ex_:ex_ + 1], axis=0)),
                  idxg.k() + htk, xg_.k(), q="pq")
            for c in range(8):
                S.pe(lambda e, xg_=xg_, c=c, ns=ns: e.transpose(out=g.PSB[3][:, c * 128:c * 128 + ns], in_=xg_.ap[0:ns, c * 128:(c + 1) * 128],
                                                                identity=g.ident_bf.ap[0:ns, 0:ns]), xg_.k() + g.ident_bf.k(), g.psk[3])
            S.act(lambda e, ns=ns, col0=col0: e.activation(out=xgT.ap[:, :, col0:col0 + ns], in_=g.PSB[3].rearrange("p (c t) -> p c t", c=8)[:, :, 0:ns], func=AF.Copy),
                  g.psk[3], xgT.k())
        for fc in range(8):
            for (c0, cw) in slabs:
                ba, bb_ = (4, 5) if c0 == 0 else (2, 2)
                oa, ob_ = (0, 0) if c0 == 0 else (0, 64)
                for k in range(8):
                    S.pe(lambda e, k=k, fc=fc, c0=c0, cw=cw, ba=ba, oa=oa, w1=w1: e.matmul(g.PS[ba][:, oa:oa + cw], lhsT=w1.ap[:, k, fc * 128:(fc + 1) * 128],
                                                                                       rhs=xgT.ap[:, k, c0:c0 + cw], start=(k == 0), stop=(k == 7)),
                         w1.k() + xgT.k(), g.psk[ba])
                ta = tmpa[fc % 2]
                S.act(lambda e, ta=ta, ba=ba, oa=oa, cw=cw: e.activation(out=ta.ap[:, 0:cw], in_=g.PS[ba][:, oa:oa + cw], func=AF.Silu), g.psk[ba], ta.k())
                for k in range(8):
                    S.pe(lambda e, k=k, fc=fc, c0=c0, cw=cw, bb_=bb_, ob_=ob_, w3=w3: e.matmul(g.PS[bb_][:, ob_:ob_ + cw], lhsT=w3.ap[:, k, fc * 128:(fc + 1) * 128],
                                                                                            rhs=xgT.ap[:, k, c0:c0 + cw], start=(k == 0), stop=(k == 7)),
                         w3.k() + xgT.k(), g.psk[bb_])
                S.dve(lambda e, ta=ta, bb_=bb_, ob_=ob_, cw=cw, fc=fc, c0=c0: e.tensor_tensor(out=uT.ap[:, fc, c0:c0 + cw], in0=ta.ap[:, 0:cw], in1=g.PS[bb_][:, ob_:ob_ + cw], op=ALU.mult),
                      ta.k() + g.psk[bb_], uT.k(fc * NSL + c0, fc * NSL + c0 + cw))
        w2 = next_w(I["moe_w2"][l, ex_])
        for (sg, j, ns, col0) in blocks:
            y_ = ysb[j % 2]
            for hf in range(2):
                bank = 6 + hf
                for fc in range(8):
                    S.pe(lambda e, fc=fc, hf=hf, bank=bank, ns=ns, col0=col0, w2=w2: e.matmul(g.PS[bank][0:ns, :], lhsT=uT.ap[:, fc, col0:col0 + ns],
                                                                                         rhs=w2.ap[:, fc, hf * 512:(hf + 1) * 512], start=(fc == 0), stop=(fc == 7)),
                         uT.k() + w2.k(), g.psk[bank])
                S.dve(lambda e, y_=y_, hf=hf, bank=bank, ns=ns, j=j, ex_=ex_, sg=sg: e.scalar_tensor_tensor(out=y_.ap[0:ns, hf * 512:(hf + 1) * 512], in0=g.PS[bank][0:ns, :],
                      scalar=gT.ap[0:ns, j, ex_:ex_ + 1], in1=mbc[sg].ap[0:ns, hf * 512:(hf + 1) * 512], op0=ALU.mult, op1=ALU.mult),
                      g.psk[bank] + gT.k() + mbc[sg].k(), y_.k(hf * 512, (hf + 1) * 512))
            dstf, dkk, bound = (outflat, xxkeys, NB * TL - 1) if sg == "x" else (xcflat, xckeys, NB * TC - 1)
            S.dma(lambda e, y_=y_, ns=ns, j=j, ex_=ex_, dstf=dstf, bound=bound: e.indirect_dma_start(
                out=dstf[:, :], out_offset=bass.IndirectOffsetOnAxis(ap=idxs.ap[0:ns, j, ex_:ex_ + 1], axis=0), in_=y_.ap[0:ns, :], in_offset=None,
                bounds_check=bound_reg(g, e, bound), oob_is_err=True, compute_op=ALU.add), y_.k() + idxs.k() + dkk, dkk, q="pq")
    mem.release(m0)


def stage_da(g, l, b, do_ctx):
    nc, S, mem, I = g.nc, g.S, g.mem, g.I
    lam_init = 0.8 - 0.6 * math.exp(-0.3 * l)
    m0 = mem.mark()
    qT = mem.alloc([2, TT], BF16)
    kT = mem.alloc([2, TT], BF16)
    v1 = mem.alloc([TT // 128, 4, 65], BF16)
    gm4 = mem.alloc([4], F32)
    S.dma(lambda e: e.dma_start(out=gm4.ap, in_=I["gm4"]), [], gm4.k())
    S.dve(lambda e: e.memset(v1.ap, 1.0), [], v1.k())
    lamt = mem.alloc([4, 32], F32)
    lams = mem.alloc([8], F32)
    S.dma(lambda e: e.dma_start(out=lamt.ap, in_=I["da_lam"][l:l + 1].rearrange("o a d -> o (a d)").partition_broadcast(128)
                                .rearrange("p o (a d) -> p (o a) d", a=4)), [], lamt.k())
    S.dve(lambda e: e.tensor_tensor(out=lamt.ap[:, 0, :], in0=lamt.ap[:, 0, :], in1=lamt.ap[:, 1, :], op=ALU.mult), lamt.k(), lamt.k())
    S.dve(lambda e: e.tensor_tensor(out=lamt.ap[:, 2, :], in0=lamt.ap[:, 2, :], in1=lamt.ap[:, 3, :], op=ALU.mult), lamt.k(), lamt.k())
    S.dve(lambda e: e.tensor_reduce(out=lams.ap[:, 0:1], in_=lamt.ap[:, 0, :], axis=AX.X, op=ALU.add), lamt.k(), lams.k())
    S.dve(lambda e: e.tensor_reduce(out=lams.ap[:, 1:2], in_=lamt.ap[:, 2, :], axis=AX.X, op=ALU.add), lamt.k(), lams.k())
    S.act(lambda e: e.activation(out=lams.ap[:, 2:4], in_=lams.ap[:, 0:2], func=AF.Exp), lams.k(), lams.k())
    S.dve(lambda e: e.tensor_tensor(out=lams.ap[:, 4:5], in0=lams.ap[:, 3:4], in1=lams.ap[:, 2:3], op=ALU.subtract), lams.k(), lams.k())
    S.dve(lambda e: e.tensor_scalar(out=lams.ap[:, 4:5], in0=lams.ap[:, 4:5], scalar1=-float(lam_init), scalar2=None, op0=ALU.add), lams.k(), lams.k())
    m1 = mem.mark()
    gqk = mem.alloc([2, 32], F32)
    S.dma(lambda e: e.dma_start(out=gqk.ap[:, 0, :], in_=I["da_qnorm_g"][l:l + 1, :].partition_broadcast(128)), [], gqk.k())
    S.dma(lambda e: e.dma_start(out=gqk.ap[:, 1, :], in_=I["da_knorm_g"][l:l + 1, :].partition_broadcast(128)), [], gqk.k())
    qk = [mem.alloc([512], F32) for _ in range(2)]
    sq = [mem.alloc([512], F32) for _ in range(2)]
    ss = [mem.alloc([32], F32) for _ in range(2)]
    rp = [mem.alloc([2, 2, 8], F32) for _ in range(2)]
    ra = [mem.alloc([16, 2, 8], F32) for _ in range(2)]
    rb = [mem.alloc([16, 2, 8], F32) for _ in range(2)]
    qb = [mem.alloc([512], BF16) for _ in range(2)]
    for i in range(TT // 128):
        t0 = i * 128
        u = i % 2
        q_, s_, ss_, rp_, ra_, rb_, qb_ = qk[u], sq[u], ss[u], rp[u], ra[u], rb[u], qb[u]
        S.dma(lambda e, q_=q_, t0=t0: e.dma_start(out=q_.ap, in_=g.QK_DA[t0:t0 + 128, :]), [("QK_DA", i, 0)], q_.k())
        S.dma(lambda e, t0=t0, i=i: e.dma_start(out=v1.ap[:, i, :, 0:64], in_=g.V_DA[t0:t0 + 128, :].rearrange("p (h e) -> p h e", h=4)),
              [("V_DA", i, 0)], v1.k())
        S.dve(lambda e, q_=q_, s_=s_: e.tensor_tensor(out=s_.ap, in0=q_.ap, in1=q_.ap, op=ALU.mult), q_.k(), s_.k())
        S.dve(lambda e, s_=s_, ss_=ss_: e.tensor_reduce(out=ss_.ap[:, 0:16], in_=s_.ap.rearrange("p (g d) -> p g d", g=16), axis=AX.X, op=ALU.add), s_.k(), ss_.k())
        S.act(lambda e, ss_=ss_: e.activation(out=ss_.ap[:, 16:32], in_=ss_.ap[:, 0:16], func=AF.Sqrt, bias=EPS, scale=1.0 / 32), ss_.k(), ss_.k())
        S.dve(lambda e, ss_=ss_: e.reciprocal(out=ss_.ap[:, 16:32], in_=ss_.ap[:, 16:32]), ss_.k(), ss_.k())
        S.dve(lambda e, q_=q_, ss_=ss_, s_=s_: e.tensor_tensor(out=s_.ap.rearrange("p (g d) -> p g d", g=16), in0=q_.ap.rearrange("p (g d) -> p g d", g=16),
                                                               in1=ss_.ap[:, 16:32, None].broadcast_to([128, 16, 32]), op=ALU.mult), q_.k() + ss_.k(), s_.k())
        for hf in range(2):
            S.pool(lambda e, s_=s_, hf=hf: e.tensor_tensor(out=s_.ap[:, hf * 256:(hf + 1) * 256].rearrange("p (g d) -> p g d", g=8),
                                                          in0=s_.ap[:, hf * 256:(hf + 1) * 256].rearrange("p (g d) -> p g d", g=8),
                                                          in1=gqk.ap[:, hf:hf + 1, :].broadcast_to([128, 8, 32]), op=ALU.mult), s_.k() + gqk.k(), s_.k())
        if t0 >= TC:
            S.dma(lambda e, rp_=rp_, t0=t0: e.dma_start(out=rp_.ap.rearrange("p a b c -> p (a b c)"), in_=I["rope"][t0 - TC:t0 - TC + 128, :]), [], rp_.k())
            xv = s_.ap.rearrange("p (g a h e) -> p g a h e", g=16, a=2, h=2)
            x1, x2 = xv[:, :, :, 0, :], xv[:, :, :, 1, :]
            cosb = rp_.ap[:, 0:1, :, :].broadcast_to([128, 16, 2, 8])
            sinb = rp_.ap[:, 1:2, :, :].broadcast_to([128, 16, 2, 8])
            qv = qb_.ap.rearrange("p (g a h e) -> p g a h e", g=16, a=2, h=2)
            S.dve(lambda e, ra_=ra_, x1=x1, cosb=cosb: e.tensor_tensor(out=ra_.ap, in0=x1, in1=cosb, op=ALU.mult), s_.k() + rp_.k(), ra_.k())
            S.pool(lambda e, rb_=rb_, x2=x2, sinb=sinb: e.tensor_tensor(out=rb_.ap, in0=x2, in1=sinb, op=ALU.mult), s_.k() + rp_.k(), rb_.k())
            S.dve(lambda e, ra_=ra_, rb_=rb_, qv=qv: e.tensor_tensor(out=qv[:, :, :, 0, :], in0=ra_.ap, in1=rb_.ap, op=ALU.subtract), ra_.k() + rb_.k(), qb_.k())
            S.dve(lambda e, ra_=ra_, x1=x1, sinb=sinb: e.tensor_tensor(out=ra_.ap, in0=x1, in1=sinb, op=ALU.mult), s_.k() + rp_.k() + qb_.k(), ra_.k())
            S.pool(lambda e, rb_=rb_, x2=x2, cosb=cosb: e.tensor_tensor(out=rb_.ap, in0=x2, in1=cosb, op=ALU.mult), s_.k() + rp_.k() + qb_.k(), rb_.k())
            S.dve(lambda e, ra_=ra_, rb_=rb_, qv=qv: e.tensor_tensor(out=qv[:, :, :, 1, :], in0=ra_.ap, in1=rb_.ap, op=ALU.add), ra_.k() + rb_.k(), qb_.k())
        else:
            S.dve(lambda e, s_=s_, qb_=qb_: e.tensor_copy(out=qb_.ap, in_=s_.ap), s_.k(), qb_.k())
        bank = 2 + u
        for c in range(4):
            S.pe(lambda e, qb_=qb_, c=c, bank=bank: e.transpose(out=g.PSB[bank][:, c * 128:(c + 1) * 128], in_=qb_.ap[:, c * 128:(c + 1) * 128],
                                                                identity=g.ident_bf.ap), qb_.k() + g.ident_bf.k(), g.psk[bank])
        S.act(lambda e, bank=bank, t0=t0: e.activation(out=qT.ap[:, :, t0:t0 + 128], in_=g.PSB[bank][:, 0:256].rearrange("p (c t) -> p c t", c=2), func=AF.Copy),
              g.psk[bank], qT.k())
        S.dve(lambda e, bank=bank, t0=t0: e.tensor_copy(out=kT.ap[:, :, t0:t0 + 128], in_=g.PSB[bank][:, 256:512].rearrange("p (c t) -> p c t", c=2)),
              g.psk[bank], kT.k())
    mem.release(m1)
    qm = [mem.alloc([512], BF16) for _ in range(2)]
    PT = [mem.alloc([512], BF16) for _ in range(3)]
    o1 = mem.alloc([4, 64], F32)
    rs = mem.alloc([8], F32)
    osb = [mem.alloc([4, 256], F32) for _ in range(2)]
    slabs = []
    if do_ctx:
        slabs.append((0, TC, [0, 1]))
    for s_i in range(TL // 512):
        slabs.append((TC + s_i * 512, 512, list(range(TT // 128))))
    cnt = [0]
    SCALE = 32 ** -0.5
    for si, (q0, qw, ktiles) in enumerate(slabs):
        nqt = qw // 128
        ob = osb[si % 2]
        for h in range(4):
            for j in range(2):
                gi = h * 2 + j
                c, r = gi // 4, gi % 4
                qm_ = qm[gi % 2]
                S.dve(lambda e, qm_=qm_, c=c, r=r, q0=q0, qw=qw: e.tensor_scalar(out=qm_.ap[:, 0:qw], in0=qT.ap[:, c, q0:q0 + qw], scalar1=gm4.ap[:, r:r + 1], scalar2=None, op0=ALU.mult),
                      qT.k() + gm4.k(), qm_.k())
                nk = len(ktiles)

                def emit_pv(pt, ki, kt, h=h, nk=nk, nqt=nqt):
                    for qt in range(nqt):
                        S.pe(lambda e, pt=pt, qt=qt, kt=kt, h=h, ki=ki, nk=nk: e.matmul(g.PS[4 + qt][:, 0:65], lhsT=pt.ap[:, qt * 128:(qt + 1) * 128], rhs=v1.ap[:, kt, h, :],
                                                                                        start=(ki == 0), stop=(ki == nk - 1)),
                             pt.k() + v1.k(), g.psk[4 + qt])

                pend = None
                for ki, kt in enumerate(ktiles):
                    sb = cnt[0] % 2
                    pt = PT[cnt[0] % 3]
                    cnt[0] += 1
                    S.pe(lambda e, qm_=qm_, c=c, kt=kt, sb=sb, qw=qw: e.matmul(g.PS[sb][:, 0:qw], lhsT=kT.ap[:, c, kt * 128:(kt + 1) * 128], rhs=qm_.ap[:, 0:qw], start=True, stop=True),
                         kT.k() + qm_.k(), g.psk[sb])
                    S.act(lambda e, pt=pt, sb=sb, qw=qw: e.activation(out=pt.ap[:, 0:qw], in_=g.PS[sb][:, 0:qw], func=AF.Exp, scale=SCALE), g.psk[sb], pt.k())
                    if pend is not None:
                        emit_pv(*pend)
                    pend = (pt, ki, kt)
                emit_pv(*pend)
                for qt in range(nqt):
                    S.dve(lambda e, qt=qt, j=j: e.reciprocal(out=rs.ap[:, j * 4 + qt:j * 4 + qt + 1], in_=g.PS[4 + qt][:, 64:65]), g.psk[4 + qt], rs.k())
                    if j == 0:
                        S.dve(lambda e, qt=qt: e.tensor_scalar(out=o1.ap[:, qt, :], in0=g.PS[4 + qt][:, 0:64], scalar1=rs.ap[:, qt:qt + 1], scalar2=None, op0=ALU.mult),
                              g.psk[4 + qt] + rs.k(), o1.k())
                    else:
                        S.dve(lambda e, qt=qt: e.tensor_tensor(out=rs.ap[:, 4 + qt:5 + qt], in0=rs.ap[:, 4 + qt:5 + qt], in1=lams.ap[:, 4:5], op=ALU.mult), rs.k() + lams.k(), rs.k())
                        S.dve(lambda e, qt=qt, h=h, ob=ob: e.scalar_tensor_tensor(out=ob.ap[:, qt, h * 64:(h + 1) * 64], in0=g.PS[4 + qt][:, 0:64], scalar=rs.ap[:, 4 + qt:5 + qt],
                                                                                  in1=o1.ap[:, qt, :], op0=ALU.mult, op1=ALU.add),
                              g.psk[4 + qt] + rs.k() + o1.k(), ob.k())
        for qt in range(nqt):
            tt = q0 + qt * 128
            S.dma(lambda e, ob=ob, qt=qt, tt=tt: e.dma_start(out=g.O_DA[0, tt:tt + 128, :], in_=ob.ap[:, qt, :]), ob.k(), [("O_DA", 0, v) for v in range(tt // 32, tt // 32 + 4)])
    mem.release(m0)


def hy_params(L):
    n = 2 * L
    N1 = n // 128
    return n, N1, N1 // 2


def hy_stage1(g, L, X, N, in_keys, W1):
    S, mem = g.S, g.mem
    n, N1, K1 = hy_params(L)
    m0 = mem.mark()
    G = 16
    pcs = [mem.alloc([G, N], BF16, parts=K1) for _ in range(2)]
    stg = [mem.alloc([G, N], BF16) for _ in range(2)]
    Xv = X.rearrange("(a b) n -> a b n", b=128)
    nmm = G * N // 512
    for gi in range(128 // G):
        pc, st = pcs[gi % 2], stg[gi % 2]
        S.dma(lambda e, pc=pc, gi=gi: e.dma_start(out=pc.ap, in_=Xv[:, gi * G:(gi + 1) * G, :]), in_keys, pc.k())
        pcf = pc.ap.rearrange("p a n -> p (a n)")
        stf = st.ap.rearrange("p a n -> p (a n)")
        for m in range(nmm):
            bank = m % 2
            S.pe(lambda e, pcf=pcf, m=m, bank=bank: e.matmul(g.PS[bank][0:2 * N1, :], lhsT=W1.ap, rhs=pcf[:, m * 512:(m + 1) * 512], start=True, stop=True),
                 pc.k() + W1.k(), g.psk[bank])
            if m % 2 == 0:
                S.act(lambda e, stf=stf, m=m, bank=bank: e.activation(out=stf[0:2 * N1, m * 512:(m + 1) * 512], in_=g.PS[bank][0:2 * N1, :], func=AF.Copy), g.psk[bank], st.k())
            else:
                S.dve(lambda e, stf=stf, m=m, bank=bank: e.tensor_copy(out=stf[0:2 * N1, m * 512:(m + 1) * 512], in_=g.PS[bank][0:2 * N1, :]), g.psk[bank], st.k())
        S.dma(lambda e, st=st, gi=gi: e.dma_start(out=g.AD[0:2 * N1, gi * G:(gi + 1) * G, 0:N], in_=st.ap[0:2 * N1]), st.k(), [("AD", gi)])
    mem.release(m0)


def hy_load_consts(g, L):
    S, mem, I = g.S, g.mem, g.I
    n, N1, K1 = hy_params(L)
    tg = "%d" % L
    W1 = mem.alloc([2 * N1], BF16, parts=K1)
    E = mem.alloc([K1], BF16, parts=2 * N1)
    S.dma(lambda e: e.dma_start(out=W1.ap, in_=I["hy_W1_" + tg]), [], W1.k())
    S.dma(lambda e: e.dma_start(out=E.ap, in_=I["hy_E_" + tg]), [], E.k())
    return W1, E


def stage_hy_filter(g, l, L, upto=99):
    nc, S, mem, I = g.nc, g.S, g.mem, g.I
    n, N1, K1 = hy_params(L)
    tg = "%d" % L
    m0 = mem.mark()
    W1, E = hy_load_consts(g, L)
    m1 = mem.mark()
    NTL = L // 128
    w3 = mem.alloc([512], F32, parts=64)
    S.dma(lambda e: e.dma_start(out=w3.ap, in_=I["hy_w3"][l]), [], w3.k())
    hB = mem.alloc([L], F32, parts=64)
    hpi = mem.alloc([1], F32)
    S.dve(lambda e: e.memset(hpi.ap, math.pi / 2), [], hpi.k())
    m_s = mem.mark()
    fe = mem.alloc([L], F32, parts=33)
    S.dma(lambda e: e.dma_start(out=fe.ap, in_=I["hy_featsT" + tg]), [], fe.k())
    w1 = mem.alloc([64], F32, parts=33)
    w2 = mem.alloc([64], F32, parts=64)
    S.dma(lambda e: e.dma_start(out=w1.ap, in_=I["hy_w1"][l]), [], w1.k())
    S.dma(lambda e: e.dma_start(out=w2.ap, in_=I["hy_w2"][l]), [], w2.k())
    pc = mem.alloc([12], F32, parts=64)
    S.dma(lambda e: e.dma_start(out=pc.ap[:, 0:2], in_=I["hy_freq"][l].rearrange("a p -> p a"), allow_slow_non_contiguous=True), [], pc.k())
    S.dma(lambda e: e.dma_start(out=pc.ap[:, 2:3], in_=I["hy_b1"][l:l + 1, :].rearrange("a p -> p a"), allow_slow_non_contiguous=True), [], pc.k())
    S.dma(lambda e: e.dma_start(out=pc.ap[:, 3:4], in_=I["hy_b2"][l:l + 1, :].rearrange("a p -> p a"), allow_slow_non_contiguous=True), [], pc.k())
    S.dve(lambda e: e.tensor_scalar(out=pc.ap[:, 4:6], in0=pc.ap[:, 0:2], scalar1=1.0 / (2 * math.pi), scalar2=None, op0=ALU.mult), pc.k(), pc.k())
    S.dve(lambda e: e.tensor_tensor(out=pc.ap[:, 8:10], in0=pc.ap[:, 0:2], in1=pc.ap[:, 2:4], op=ALU.mult), pc.k(), pc.k())
    S.dve(lambda e: e.tensor_scalar(out=pc.ap[:, 6:8], in0=pc.ap[:, 8:10], scalar1=1.0 / (2 * math.pi), scalar2=64.5, op0=ALU.mult, op1=ALU.add), pc.k(), pc.k())
    hA = mem.alloc([L], F32, parts=64)
    ki = [mem.alloc([512], I32, parts=64) for _ in range(2)]
    tf = [mem.alloc([512], F32, parts=64) for _ in range(2)]
    xf = [mem.alloc([512], F32, parts=64) for _ in range(2)]
    sn = [mem.alloc([512], F32, parts=64) for _ in range(2)]
    cs = [mem.alloc([512], F32, parts=64) for _ in range(2)]

    def sine_layer(wt, kparts, src, dst, li):
        for si in range((L + 511) // 512):
            c0 = si * 512
            cw = min(512, L - c0)
            u = si % 2
            bank = u
            S.pe(lambda e, c0=c0, cw=cw, bank=bank: e.matmul(g.PS[bank][0:64, 0:cw], lhsT=wt.ap, rhs=src.ap[0:kparts, c0:c0 + cw], start=True, stop=True),
                 wt.k() + src.k(), g.psk[bank])
            k_, t_, x_, s_, c_ = ki[u], tf[u], xf[u], sn[u], cs[u]
            S.dve(lambda e, k_=k_, bank=bank, cw=cw: e.tensor_scalar(out=k_.ap[:, 0:cw], in0=g.PS[bank][0:64, 0:cw], scalar1=pc.ap[:, 4 + li:5 + li], scalar2=pc.ap[:, 6 + li:7 + li],
                                                                     op0=ALU.mult, op1=ALU.add), g.psk[bank] + pc.k(), k_.k())
            S.dve(lambda e, k_=k_, t_=t_, cw=cw: e.tensor_scalar(out=t_.ap[:, 0:cw], in0=k_.ap[:, 0:cw], scalar1=-64.0, scalar2=-2 * math.pi, op0=ALU.add, op1=ALU.mult), k_.k(), t_.k())
            S.dve(lambda e, x_=x_, bank=bank, cw=cw: e.tensor_scalar(out=x_.ap[:, 0:cw], in0=g.PS[bank][0:64, 0:cw], scalar1=pc.ap[:, li:li + 1], scalar2=pc.ap[:, 8 + li:9 + li],
                                                                     op0=ALU.mult, op1=ALU.add), g.psk[bank] + pc.k(), x_.k())
            S.pool(lambda e, x_=x_, t_=t_, cw=cw: e.tensor_tensor(out=x_.ap[:, 0:cw], in0=x_.ap[:, 0:cw], in1=t_.ap[:, 0:cw], op=ALU.add), x_.k() + t_.k(), x_.k())
            S.act(lambda e, x_=x_, s_=s_, cw=cw: e.activation(out=s_.ap[:, 0:cw], in_=x_.ap[:, 0:cw], func=AF.Sin, scale=0.5), x_.k(), s_.k())
            S.act(lambda e, x_=x_, c_=c_, cw=cw: e.activation(out=c_.ap[:, 0:cw], in_=x_.ap[:, 0:cw], func=AF.Sin, scale=0.5, bias=hpi.ap[0:64, 0:1]), x_.k() + hpi.k(), c_.k())
            S.dve(lambda e, s_=s_, c_=c_, c0=c0, cw=cw: e.scalar_tensor_tensor(out=dst.ap[:, c0:c0 + cw], in0=s_.ap[:, 0:cw], scalar=2.0, in1=c_.ap[:, 0:cw], op0=ALU.mult, op1=ALU.mult),
                  s_.k() + c_.k(), dst.k(c0, c0 + cw))

    sine_layer(w1, 33, fe, hA, 0)
    if upto == 1:
        S.dma(lambda e: e.dma_start(out=g.ZF[0:64, 0:256], in_=hA.ap[:, 0:256]), hA.k(), [("ZF", 0)])
        mem.release(m0)
        return
    sine_layer(w2, 64, hA, hB, 1)
    mem.release(m_s)
    if upto == 2:
        S.dma(lambda e: e.dma_start(out=g.ZF[0:64, 0:256], in_=hB.ap[:, 0:256]), hB.k(), [("ZF", 0)])
        mem.release(m0)
        return
    decb = mem.alloc([512], F32)
    S.dma(lambda e: e.dma_start(out=decb.ap, in_=I["hy_decay"][l:l + 1, :].partition_broadcast(128)), [], decb.k())
    S.act(lambda e: e.activation(out=decb.ap, in_=decb.ap, func=AF.Abs), decb.k(), decb.k())
    nd = mem.alloc([NTL], F32)
    S.dma(lambda e: e.dma_start(out=nd.ap, in_=I["hy_negdist" + tg]), [], nd.k())
    hw = mem.alloc([NTL, 512], F32)
    win = [mem.alloc([512], F32) for _ in range(2)]
    ab = [mem.alloc([512], F32) for _ in range(2)]
    for i in range(NTL):
        u = i % 2
        bank = 2 + u
        S.pe(lambda e, i=i, bank=bank: e.matmul(g.PS[bank], lhsT=hB.ap[:, i * 128:(i + 1) * 128], rhs=w3.ap, start=True, stop=True), hB.k() + w3.k(), g.psk[bank])
        S.act(lambda e, i=i, u=u: e.activation(out=win[u].ap, in_=decb.ap, func=AF.Exp, scale=nd.ap[:, i:i + 1]), decb.k() + nd.k(), win[u].k())
        S.dve(lambda e, i=i, u=u, bank=bank: e.scalar_tensor_tensor(out=hw.ap[:, i, :], in0=win[u].ap, scalar=0.05, in1=g.PS[bank], op0=ALU.add, op1=ALU.mult),
              win[u].k() + g.psk[bank], hw.k(i * 512, (i + 1) * 512))
        S.act(lambda e, i=i, u=u: e.activation(out=ab[u].ap, in_=hw.ap[:, i, :], func=AF.Abs), hw.k(i * 512, (i + 1) * 512), ab[u].k())
        S.pe(lambda e, i=i, u=u: e.matmul(g.PS[7], lhsT=g.ones_f.ap, rhs=ab[u].ap, start=(i == 0), stop=(i == NTL - 1)), g.ones_f.k() + ab[u].k(), g.psk[7])
    rcs = mem.alloc([512], F32)
    S.dve(lambda e: e.reciprocal(out=rcs.ap, in_=g.PS[7]), g.psk[7], rcs.k())
    hn = [mem.alloc([512], BF16) for _ in range(2)]
    for i in range(NTL):
        u = i % 2
        S.dve(lambda e, i=i, u=u: e.tensor_tensor(out=hn[u].ap, in0=hw.ap[:, i, :], in1=rcs.ap, op=ALU.mult), hw.k(i * 512, (i + 1) * 512) + rcs.k(), hn[u].k())
        S.dma(lambda e, i=i, u=u: e.dma_start(out=g.HFILT[L][i * 128:(i + 1) * 128, :], in_=hn[u].ap), hn[u].k(), [("HFILT", L, i)])
    mem.release(m1)
    if upto == 3:
        mem.release(m0)
        return
    hy_stage1(g, L, g.HFILT[L], 512, [("HFILT", L, i) for i in range(NTL)], W1)
    if upto == 4:
        g.S.dma(lambda e: e.dma_start(out=g.ZF[0:128, 0:256], in_=g.AD[5, :, 0:256].bitcast(BF16)) if False else e.dma_start(out=g.ZD[0], in_=g.AD[5, :, 0:256]), [("AD", gi) for gi in range(8)], [("ZD", 0)])
        mem.release(m0)
        return
    adk = [("AD", gi) for gi in range(8)]
    are = [mem.alloc([512], BF16) for _ in range(2)]
    aim = [mem.alloc([512], BF16) for _ in range(2)]
    pk = [mem.alloc([4, 128], BF16) for _ in range(2)]
    hs = [mem.alloc([2, 512], F32) for _ in range(2)]
    for f1 in range(N1 if upto > 10 else min(N1, 2 ** upto)):
        u = f1 % 2
        S.dma(lambda e, f1=f1, u=u: e.dma_start(out=are[u].ap, in_=g.AD[f1, :, 0:512]), adk, are[u].k())
        S.dma(lambda e, f1=f1, u=u: e.dma_start(out=aim[u].ap, in_=g.AD[N1 + f1, :, 0:512]), adk, aim[u].k())
        S.dma(lambda e, f1=f1, u=u: e.dma_start(out=pk[u].ap, in_=I["hy_PKH_" + tg][f1]), [], pk[u].k())
        for hb in range(2):
            bank = 4 + hb
            S.pe(lambda e, u=u, hb=hb, bank=bank: e.matmul(g.PS[bank], lhsT=pk[u].ap[:, 2 * hb, :], rhs=are[u].ap, start=True, stop=False), pk[u].k() + are[u].k(), g.psk[bank])
            S.pe(lambda e, u=u, hb=hb, bank=bank: e.matmul(g.PS[bank], lhsT=pk[u].ap[:, 2 * hb + 1, :], rhs=aim[u].ap, start=False, stop=True), pk[u].k() + aim[u].k(), g.psk[bank])
            if hb == 0:
                S.act(lambda e, u=u, hb=hb, bank=bank: e.activation(out=hs[u].ap[:, hb, :], in_=g.PS[bank], func=AF.Copy), g.psk[bank], hs[u].k())
            else:
                S.dve(lambda e, u=u, hb=hb, bank=bank: e.tensor_copy(out=hs[u].ap[:, hb, :], in_=g.PS[bank]), g.psk[bank], hs[u].k())
        S.dma(lambda e, f1=f1, u=u: e.dma_start(out=g.HAB[L][:, f1].rearrange("a p n -> p a n"), in_=hs[u].ap), hs[u].k(), [("HAB", L, f1)])
    mem.release(m0)


def stage_hy(g, l, b, do_ctx):
    nc, S, mem, I = g.nc, g.S, g.mem, g.I
    m0 = mem.mark()
    segs = [(TC, TL)]
    if do_ctx:
        segs.append((0, TC))
    m1 = mem.mark()
    wc = mem.alloc([3, 768], F32)
    S.dma(lambda e: e.dma_start(out=wc.ap, in_=I["hy_conv_w"][l:l + 1].rearrange("o a c -> o (a c)").partition_broadcast(128).rearrange("p o (a c) -> p (o a) c", a=3)), [], wc.k())
    cur = [mem.alloc([768], F32) for _ in range(2)]
    prv = [mem.alloc([768], F32) for _ in range(2)]
    nxt = [mem.alloc([768], F32) for _ in range(2)]
    zb = [mem.alloc([256], BF16) for _ in range(2)]
    cnt = 0
    for (s0, L) in segs:
        for i in range(L // 128):
            t0 = s0 + i * 128
            u = cnt % 2
            cnt += 1
            c_, p_, n_, z_ = cur[u], prv[u], nxt[u], zb[u]
            ti = t0 // 128
            S.dma(lambda e, c_=c_, t0=t0: e.dma_start(out=c_.ap, in_=g.HY[t0:t0 + 128, :]), [("HY", ti, 0), ("HY", ti, 512)], c_.k())
            if i == 0:
                S.pool(lambda e, p_=p_: e.memset(p_.ap, 0.0), [], p_.k())
                S.dma(lambda e, p_=p_, t0=t0: e.dma_start(out=p_.ap[1:128, :], in_=g.HY[t0:t0 + 127, :]), [("HY", ti, 0), ("HY", ti, 512)], p_.k())
            else:
                S.dma(lambda e, p_=p_, t0=t0: e.dma_start(out=p_.ap, in_=g.HY[t0 - 1:t0 + 127, :]),
                      [("HY", ti, 0), ("HY", ti, 512), ("HY", ti - 1, 0), ("HY", ti - 1, 512)], p_.k())
            if i == L // 128 - 1:
                S.pool(lambda e, n_=n_: e.memset(n_.ap, 0.0), [], n_.k())
                S.dma(lambda e, n_=n_, t0=t0: e.dma_start(out=n_.ap[0:127, :], in_=g.HY[t0 + 1:t0 + 128, :]), [("HY", ti, 0), ("HY", ti, 512)], n_.k())
            else:
                S.dma(lambda e, n_=n_, t0=t0: e.dma_start(out=n_.ap, in_=g.HY[t0 + 1:t0 + 129, :]),
                      [("HY", ti, 0), ("HY", ti, 512), ("HY", ti + 1, 0), ("HY", ti + 1, 512)], n_.k())
            S.dve(lambda e, c_=c_: e.tensor_tensor(out=c_.ap, in0=c_.ap, in1=wc.ap[:, 1, :], op=ALU.mult), c_.k() + wc.k(), c_.k())
            S.pool(lambda e, p_=p_: e.tensor_tensor(out=p_.ap, in0=p_.ap, in1=wc.ap[:, 0, :], op=ALU.mult), p_.k() + wc.k(), p_.k())
            S.pool(lambda e, n_=n_: e.tensor_tensor(out=n_.ap, in0=n_.ap, in1=wc.ap[:, 2, :], op=ALU.mult), n_.k() + wc.k(), n_.k())
            S.dve(lambda e, c_=c_, p_=p_: e.tensor_tensor(out=c_.ap, in0=c_.ap, in1=p_.ap, op=ALU.add), c_.k() + p_.k(), c_.k())
            S.dve(lambda e, c_=c_, n_=n_: e.tensor_tensor(out=c_.ap, in0=c_.ap, in1=n_.ap, op=ALU.add), c_.k() + n_.k(), c_.k())
            S.act(lambda e, c_=c_, z_=z_: e.activation(out=z_.ap, in_=c_.ap[:, 0:256], func=AF.Copy), c_.k(), z_.k())
            S.dma(lambda e, c_=c_, t0=t0: e.dma_start(out=g.UCONV[t0:t0 + 128, :], in_=c_.ap), c_.k(), [("UCONV", ti)])
            S.dma(lambda e, c_=c_, t0=t0: e.dma_start(out=g.ZF[t0:t0 + 128, :], in_=c_.ap[:, 0:256]), c_.k(), [("ZF", ti)])
            S.dma(lambda e, z_=z_, t0=t0: e.dma_start(out=g.ZB[0, t0:t0 + 128, :], in_=z_.ap), z_.k(), [("ZB", 0, ti)])
    mem.release(m1)
    def do_seg(s0, L):
        n, N1, K1 = hy_params(L)
        tg = "%d" % L
        m2 = mem.mark()
        W1, E = hy_load_consts(g, L)
        tiles = list(range(s0 // 128, (s0 + L) // 128))
        def do_order(o):
            zin = g.ZB[o % 2, s0:s0 + L, :]
            hy_stage1(g, L, zin, 256, [("ZB", o % 2, ti) for ti in tiles], W1)
            m3 = mem.mark()
            adk = [("AD", gi) for gi in range(8)]
            are = [mem.alloc([256], BF16) for _ in range(2)]
            aim = [mem.alloc([256], BF16) for _ in range(2)]
            pk = [mem.alloc([6, 128], BF16) for _ in range(2)]
            hab = [mem.alloc([2, 256], F32) for _ in range(2)]
            ta = [mem.alloc([256], F32) for _ in range(2)]
            tb = [mem.alloc([256], F32) for _ in range(2)]
            yb = [mem.alloc([256], BF16) for _ in range(2)]
            zs = [mem.alloc([2, 256], BF16) for _ in range(2)]
            for f1 in range(N1):
                u = f1 % 2
                S.dma(lambda e, f1=f1, u=u: e.dma_start(out=are[u].ap, in_=g.AD[f1, :, 0:256]), adk, are[u].k())
                S.dma(lambda e, f1=f1, u=u: e.dma_start(out=aim[u].ap, in_=g.AD[N1 + f1, :, 0:256]), adk, aim[u].k())
                S.dma(lambda e, f1=f1, u=u: e.dma_start(out=pk[u].ap, in_=I["hy_PK_" + tg][f1]), [], pk[u].k())
                S.dma(lambda e, f1=f1, u=u, o=o: e.dma_start(out=hab[u].ap, in_=g.HAB[L][:, f1, :, o * 256:(o + 1) * 256].rearrange("a p n -> p a n")),
                      [("HAB", L, f1)], hab[u].k())
                for hb in range(2):
                    bank = 2 + hb
                    S.pe(lambda e, u=u, hb=hb, bank=bank: e.matmul(g.PS[bank][:, 0:256], lhsT=pk[u].ap[:, 2 * hb, :], rhs=are[u].ap, start=True, stop=False), pk[u].k() + are[u].k(), g.psk[bank])
                    S.pe(lambda e, u=u, hb=hb, bank=bank: e.matmul(g.PS[bank][:, 0:256], lhsT=pk[u].ap[:, 2 * hb + 1, :], rhs=aim[u].ap, start=False, stop=True), pk[u].k() + aim[u].k(), g.psk[bank])
                S.dve(lambda e, u=u: e.tensor_tensor(out=ta[u].ap, in0=g.PS[2][:, 0:256], in1=hab[u].ap[:, 0, :], op=ALU.mult), g.psk[2] + hab[u].k(), ta[u].k())
                S.dve(lambda e, u=u: e.tensor_tensor(out=tb[u].ap, in0=g.PS[3][:, 0:256], in1=hab[u].ap[:, 1, :], op=ALU.mult), g.psk[3] + hab[u].k(), tb[u].k())
                S.pool(lambda e, u=u: e.tensor_tensor(out=yb[u].ap, in0=ta[u].ap, in1=tb[u].ap, op=ALU.add), ta[u].k() + tb[u].k(), yb[u].k())
                for hb in range(2):
                    bank = 4 + hb
                    S.pe(lambda e, u=u, hb=hb, bank=bank: e.matmul(g.PS[bank][:, 0:256], lhsT=pk[u].ap[:, 4 + hb, :], rhs=yb[u].ap, start=True, stop=True), pk[u].k() + yb[u].k(), g.psk[bank])
                    if hb == 0:
                        S.act(lambda e, u=u, hb=hb, bank=bank: e.activation(out=zs[u].ap[:, hb, :], in_=g.PS[bank][:, 0:256], func=AF.Copy), g.psk[bank], zs[u].k())
                    else:
                        S.dve(lambda e, u=u, hb=hb, bank=bank: e.tensor_copy(out=zs[u].ap[:, hb, :], in_=g.PS[bank][:, 0:256]), g.psk[bank], zs[u].k())
                S.dma(lambda e, f1=f1, u=u: e.dma_start(out=g.ZD[f1, :, :], in_=zs[u].ap[:, 0, :]), zs[u].k(), [("ZD", f1)])
                S.dma(lambda e, f1=f1, u=u: e.dma_start(out=g.ZD[N1 + f1, :, :], in_=zs[u].ap[:, 1, :]), zs[u].k(), [("ZD", N1 + f1)])
            mem.release(m3)
            m4 = mem.mark()
            G = 16
            zin_ = [mem.alloc([G, 256], BF16, parts=2 * N1) for _ in range(2)]
            gt = [mem.alloc([G, 256], F32, parts=K1) for _ in range(2)]
            zp = [mem.alloc([G, 256], F32, parts=K1) for _ in range(2)]
            zn = [mem.alloc([G, 256], F32, parts=K1) for _ in range(2)]
            znb = [mem.alloc([G, 256], BF16, parts=K1) for _ in range(2)]
            tq = [mem.alloc([512], F32, parts=K1) for _ in range(2)]
            bb = mem.alloc([256], F32, parts=K1)
            S.dma(lambda e, o=o: e.dma_start(out=bb.ap, in_=I["hy_bias"][l, o:o + 1, :].partition_broadcast(K1)), [], bb.k())
            zdk = [("ZD", r_) for r_ in range(2 * N1)]
            Uv = g.UCONV[s0:s0 + L, :].rearrange("(a b) n -> a b n", b=128)
            ZFv = g.ZF[s0:s0 + L, :].rearrange("(a b) n -> a b n", b=128)
            ZBv = g.ZB[(o + 1) % 2, s0:s0 + L, :].rearrange("(a b) n -> a b n", b=128)
            ukeys = [("UCONV", ti) for ti in tiles]
            zfk = [("ZF", ti) for ti in tiles]
            zbk = [("ZB", (o + 1) % 2, ti) for ti in tiles]
            for gi in range(128 // G):
                u = gi % 2
                S.dma(lambda e, u=u, gi=gi: e.dma_start(out=zin_[u].ap, in_=g.ZD[0:2 * N1, gi * G:(gi + 1) * G, :]), zdk, zin_[u].k())
                S.dma(lambda e, u=u, gi=gi, o=o: e.dma_start(out=gt[u].ap, in_=Uv[:, gi * G:(gi + 1) * G, 256 * (o + 1):256 * (o + 2)]), ukeys, gt[u].k())
                S.dma(lambda e, u=u, gi=gi: e.dma_start(out=zp[u].ap, in_=ZFv[:, gi * G:(gi + 1) * G, :]), zfk, zp[u].k())
                zf_ = zin_[u].ap.rearrange("p a n -> p (a n)")
                for pr in range(G // 2):
                    bank = 6 + pr % 2
                    tq_ = tq[pr % 2]
                    S.pe(lambda e, zf_=zf_, pr=pr, bank=bank: e.matmul(g.PS[bank][0:K1, :], lhsT=E.ap, rhs=zf_[:, pr * 512:(pr + 1) * 512], start=True, stop=True),
                         E.k() + zin_[u].k(), g.psk[bank])
                    zpv = zp[u].ap[:, 2 * pr:2 * pr + 2, :]
                    S.pool(lambda e, tq_=tq_, zpv=zpv: e.tensor_tensor(out=tq_.ap.rearrange("p (a n) -> p a n", a=2), in0=zpv, in1=bb.ap[:, None, :].broadcast_to([K1, 2, 256]), op=ALU.mult),
                           zp[u].k() + bb.k(), tq_.k())
                    S.dve(lambda e, tq_=tq_, bank=bank: e.tensor_tensor(out=tq_.ap, in0=tq_.ap, in1=g.PS[bank][0:K1, :], op=ALU.add), tq_.k() + g.psk[bank], tq_.k())
                    S.dve(lambda e, tq_=tq_, u=u, pr=pr: e.tensor_tensor(out=zn[u].ap[:, 2 * pr:2 * pr + 2, :], in0=tq_.ap.rearrange("p (a n) -> p a n", a=2), in1=gt[u].ap[:, 2 * pr:2 * pr + 2, :], op=ALU.mult),
                          tq_.k() + gt[u].k(), zn[u].k())
                S.act(lambda e, u=u: e.activation(out=znb[u].ap, in_=zn[u].ap, func=AF.Copy), zn[u].k(), znb[u].k())
                S.dma(lambda e, u=u, gi=gi: e.dma_start(out=ZFv[:, gi * G:(gi + 1) * G, :], in_=zn[u].ap), zn[u].k(), zfk)
                S.dma(lambda e, u=u, gi=gi: e.dma_start(out=ZBv[:, gi * G:(gi + 1) * G, :], in_=znb[u].ap), znb[u].k(), zbk)
            mem.release(m4)

        for o in range(2):
            do_order(o)
        mem.release(m2)

    for (s0, L) in segs:
        do_seg(s0, L)
    zt = [mem.alloc([256], BF16) for _ in range(2)]
    stg = [mem.alloc([2, 128], BF16) for _ in range(2)]
    cnt = 0
    for (s0, L) in segs:
        for i in range(L // 128):
            t0 = s0 + i * 128
            ti = t0 // 128
            u = cnt % 2
            cnt += 1
            S.dma(lambda e, u=u, t0=t0: e.dma_start(out=zt[u].ap, in_=g.ZB[0, t0:t0 + 128, :]), [("ZB", 0, ti)], zt[u].k())
            bank = 2 + u
            for c in range(2):
                S.pe(lambda e, u=u, c=c, bank=bank: e.transpose(out=g.PSB[bank][:, c * 128:(c + 1) * 128], in_=zt[u].ap[:, c * 128:(c + 1) * 128], identity=g.ident_bf.ap),
                     zt[u].k() + g.ident_bf.k(), g.psk[bank])
            S.act(lambda e, u=u, bank=bank: e.activation(out=stg[u].ap, in_=g.PSB[bank][:, 0:256].rearrange("p (c t) -> p c t", c=2), func=AF.Copy), g.psk[bank], stg[u].k())
            S.dma(lambda e, u=u, t0=t0: e.dma_start(out=g.BRT[1].rearrange("(c p) t -> p c t", p=128)[:, :, t0:t0 + 128], in_=stg[u].ap), stg[u].k(), [("BRT", 1, ti)])
    mem.release(m0)
```

```python
import math
import numpy as np
import ml_dtypes
import concourse.bass as bass
import concourse.mybir as mybir
from concourse.bass_utils import run_bass_kernel_spmd

F32 = mybir.dt.float32
BF16 = mybir.dt.bfloat16
I32 = mybir.dt.int32
U32 = mybir.dt.uint32
U8 = mybir.dt.uint8
AF = mybir.ActivationFunctionType
ALU = mybir.AluOpType
AX = mybir.AxisListType
NPBF = ml_dtypes.bfloat16

D = 1024
TC = 256
TL = 4096
TT = TC + TL
NB = 2
NCORES = 8
DEPTH = 2
NIN = 7712
EPS = 1e-6
PAGE = 2048

OFF = {}
_o = 0
for _n, _w in (("gla_q", 128), ("gla_k", 128), ("gla_v", 256), ("gla_af", 16), ("gla_ab", 16), ("gla_g", 256),
               ("hy", 768), ("hg_q", 256), ("hg_ff", 256), ("hg_fb", 256), ("hg_i", 256), ("hg_g", 256),
               ("da_q", 256), ("da_k", 256), ("da_v", 256), ("merge", 4096)):
    OFF[_n] = (_o, _w)
    _o += _w
assert _o == NIN

COMPUTE = ("pe", "dve", "act", "pool")
DMAQ = ("sp", "pq")
NSEM_DMA = {"sp": 36, "pq": 20}
EPOCH = 16000
NEPOCH = 9


class Ins:
    __slots__ = ("eng", "fn", "deps", "flag", "dma", "idx", "stage", "n0", "n1", "hoist")

    def __init__(self, eng, fn, dma):
        self.eng = eng
        self.fn = fn
        self.deps = set()
        self.flag = False
        self.dma = dma


class Sched:
    def __init__(self, nc):
        self.nc = nc
        self.ins = []
        self.lastw = {}
        self.readers = {}

    def _add(self, eng, fn, reads, writes, dma=False):
        phys = "pool" if eng == "pq" else eng
        I = Ins(eng, fn, dma)
        I.idx = len(self.ins)
        I.stage = getattr(self, "stage", "")
        lastw = self.lastw
        readers = self.readers
        raw = set()
        for k in reads:
            w = lastw.get(k)
            if w is not None:
                raw.add(w)
        other = set()
        for k in writes:
            w = lastw.get(k)
            if w is not None:
                other.add(w)
            rs = readers.get(k)
            if rs:
                other.update(rs)
        if dma:
            I.deps = raw | other
        else:
            deps = set()
            for d in raw:
                dphys = "pool" if d.eng == "pq" else d.eng
                if d.dma or dphys != phys or phys != "pe":
                    deps.add(d)
            for d in other:
                dphys = "pool" if d.eng == "pq" else d.eng
                if d.dma or dphys != phys:
                    deps.add(d)
            I.deps = deps
        I.deps.discard(I)
        for k in writes:
            lastw[k] = I
            readers[k] = []
        wset = set(writes)
        for k in reads:
            if k not in wset:
                readers.setdefault(k, []).append(I)
        self.ins.append(I)
        return I

    def pe(self, fn, reads, writes):
        return self._add("pe", fn, reads, writes)

    def dve(self, fn, reads, writes):
        return self._add("dve", fn, reads, writes)

    def act(self, fn, reads, writes):
        return self._add("act", fn, reads, writes)

    def pool(self, fn, reads, writes):
        return self._add("pool", fn, reads, writes)

    def dma(self, fn, reads, writes, q="sp"):
        I = self._add(q, fn, reads, writes, dma=True)
        I.hoist = (q == "sp" and len(writes) > 0 and all(isinstance(k, tuple) and k[0] == "sb" for k in writes)
                   and not any(isinstance(k, tuple) and k[0] == "sb" for k in reads))
        return I

    def emit(self, final_keys=()):
        nc = self.nc
        for I in self.ins:
            for d in I.deps:
                d.flag = True
        finals = []
        for k in final_keys:
            if k in self.lastw and self.lastw[k] not in finals:
                finals.append(self.lastw[k])
        for f in finals:
            f.flag = True
        sems = {e: [nc.alloc_semaphore("s_%s%d" % (e, i)) for i in range(NEPOCH)] for e in COMPUTE}
        dsem = {q: [nc.alloc_semaphore("d_%s%d" % (q, i)) for i in range(NSEM_DMA[q])] for q in DMAQ}
        sp_list = [I for I in self.ins if I.eng == "sp"]
        pos = {I: i for i, I in enumerate(sp_list)}
        f = {}
        lastf = {"pe": -1, "dve": -1, "act": -1, "pool": -1}
        for I in self.ins:
            phys = "pool" if I.eng == "pq" else I.eng
            v = lastf[phys] if phys != "sp" else -1
            for d in I.deps:
                dv = pos[d] if d.eng == "sp" else f[d]
                if dv > v:
                    v = dv
            f[I] = v
            if phys != "sp":
                lastf[phys] = v
        sp_out = []
        for I in sp_list:
            if getattr(I, "hoist", False):
                k = len(sp_out)
                while k > 0 and (not getattr(sp_out[k - 1], "hoist", False)) and pos[sp_out[k - 1]] > f[I]:
                    k -= 1
                sp_out.insert(k, I)
            else:
                sp_out.append(I)
        streams = {"pe": [], "dve": [], "act": [], "pool": [], "sp": sp_out}
        for I in self.ins:
            if I.eng != "sp":
                streams["pool" if I.eng == "pq" else I.eng].append(I)
        dcnt = {q: [0] * NSEM_DMA[q] for q in DMAQ}
        dnext = {q: 0 for q in DMAQ}
        ccnt = {e: 0 for e in COMPUTE}
        ev = {}
        guard = {}
        for I in self.ins:
            if (not I.dma) and I.flag:
                ccnt[I.eng] += 1
                ep = (ccnt[I.eng] - 1) // EPOCH
                assert ep < NEPOCH, "too many semaphore epochs"
                ev[I] = (sems[I.eng][ep], (ccnt[I.eng] - 1) % EPOCH + 1)
        for qn, st in (("sp", streams["sp"]), ("pq", streams["pool"])):
            for I in st:
                if not I.dma:
                    continue
                q = I.eng
                i = dnext[q]
                dnext[q] = (i + 1) % NSEM_DMA[q]
                prev = dcnt[q][i]
                dcnt[q][i] += 16
                ev[I] = (dsem[q][i], dcnt[q][i])
                guard[I] = (dsem[q][i], prev) if prev > 0 else None
        self.stats = {k: len(v) for k, v in streams.items()}
        self.stats["semmax"] = dict(ccnt)

        def run_stream(phys, eng):
            waited = {}
            for I in streams[phys]:
                need = {}
                for d in I.deps:
                    s, v = ev[d]
                    key = id(s)
                    if key not in need or need[key][1] < v:
                        need[key] = (s, v)
                if I.dma and guard[I] is not None:
                    s, v = guard[I]
                    key = id(s)
                    if key not in need or need[key][1] < v:
                        need[key] = (s, v)
                for key, (s, v) in need.items():
                    if waited.get(key, 0) < v:
                        eng.wait_ge(s, v)
                        waited[key] = v
                I.n0 = nc.get_next_instruction_name()
                bi = I.fn(eng)
                if I.dma:
                    bi.then_inc(ev[I][0], 16)
                elif I.flag:
                    bi.then_inc(ev[I][0], 1)
            if phys == "sp":
                for f in finals:
                    s, v = ev[f]
                    eng.wait_ge(s, v)

        with nc.Block() as block:
            @block.tensor
            def _(e):
                run_stream("pe", e)

            @block.vector
            def _(e):
                run_stream("dve", e)

            @block.scalar
            def _(e):
                run_stream("act", e)

            @block.gpsimd
            def _(e):
                run_stream("pool", e)

            @block.sync
            def _(e):
                run_stream("sp", e)


class Tn:
    def __init__(self, ap, off, nbytes, esize):
        self.ap = ap
        self.off = off
        self.nbytes = nbytes
        self.esize = esize

    def k(self, lo=0, hi=None):
        b0 = self.off + lo * self.esize
        b1 = self.off + (self.nbytes if hi is None else hi * self.esize)
        return [("sb", p) for p in range(b0 // PAGE, (b1 - 1) // PAGE + 1)]


ESZ = {F32: 4, BF16: 2, I32: 4, U32: 4, U8: 1}


class Mem:
    def __init__(self, nc, nbytes):
        self.ar = nc.alloc_sbuf_tensor("arena", [128, nbytes], U8).ap()
        self.nbytes = nbytes
        self.top = 0
        self.peak = 0

    def alloc(self, shape, dtype, parts=128):
        es = ESZ[dtype]
        n = 1
        for s in shape:
            n *= s
        nb = n * es
        off = (self.top + 63) // 64 * 64
        assert off + nb <= self.nbytes, "SBUF arena overflow: need %d have %d" % (off + nb, self.nbytes)
        self.top = off + nb
        self.peak = max(self.peak, self.top)
        ap = self.ar[0:parts, off:off + nb].bitcast(dtype)
        if len(shape) == 2:
            ap = ap.rearrange("p (a b) -> p a b", a=shape[0])
        elif len(shape) == 3:
            ap = ap.rearrange("p (a b c) -> p a b c", a=shape[0], b=shape[1])
        return Tn(ap, off, nb, es)

    def mark(self):
        return self.top

    def release(self, m):
        self.top = m


def dkeys(name, lo, hi, gran):
    return [(name, i) for i in range(lo // gran, (hi - 1) // gran + 1)]


def host_consts():
    c = {}
    c["ident_bf"] = np.eye(128, dtype=np.float32).astype(NPBF)
    c["ident_f"] = np.eye(128, dtype=np.float32)
    c["ones_f"] = np.ones((128, 128), dtype=np.float32)
    c["ones_row"] = np.ones((1, 512), dtype=np.float32)
    tpos = np.arange(TL)
    pos = np.stack([tpos // 64, tpos % 64], 1).astype(np.float32)
    fr = (10000.0 ** (-np.arange(8, dtype=np.float32) / 8.0)).astype(np.float32)
    ang = pos[:, :, None] * fr[None, None, :]
    c["rope"] = np.stack([np.cos(ang), np.sin(ang)], 1).astype(np.float32).reshape(TL, 32)
    gm = np.zeros((128, 4), np.float32)
    for r_ in range(4):
        gm[r_ * 32:(r_ + 1) * 32, r_] = 1.0
    c["gm4"] = gm
    for L in (TL, TC):
        n = 2 * L
        N1 = n // 128
        K1 = N1 // 2
        tg = "%d" % L
        j = np.arange(L, dtype=np.float64)
        tt_ = j / max(L - 1, 1)
        w_ = 2 * np.pi * j / L
        f_ = np.linspace(1e-4, 15, 16)
        feats = np.concatenate([tt_[:, None], np.cos(w_[:, None] * f_), -np.sin(w_[:, None] * f_)], axis=-1)
        c["hy_featsT" + tg] = np.ascontiguousarray(feats.T).astype(np.float32)
        dist = np.abs(j - L // 2) / (L // 2)
        c["hy_negdist" + tg] = np.ascontiguousarray((-dist).reshape(L // 128, 128).T).astype(np.float32)
        t1 = np.arange(K1)[:, None]
        f1 = np.arange(N1)[None, :]
        W1 = np.exp(-2j * np.pi * f1 * t1 / N1 - 1j * np.pi * t1 / N1)
        c["hy_W1_" + tg] = np.concatenate([W1.real, W1.imag], 1).astype(NPBF)
        E = np.exp(2j * np.pi * f1.T * t1.T / N1 + 1j * np.pi * t1.T / N1)
        c["hy_E_" + tg] = np.concatenate([(2.0 / n) * E.real, -(2.0 / n) * E.imag], 0).astype(NPBF)
        t2 = np.arange(128)[:, None]
        f2 = np.arange(64)[None, :]
        PK = np.zeros((N1, 128, 6, 128), np.float64)
        PKH = np.zeros((N1, 128, 4, 128), np.float64)
        for a in range(N1):
            f = a + N1 * f2
            ang = np.pi * (2 * f + 1) * t2 / n
            Gc, Gs = np.cos(ang), -np.sin(ang)
            PK[a, :, 0, :] = np.concatenate([Gc, Gs], 1)
            PK[a, :, 1, :] = np.concatenate([-Gs, Gc], 1)
            PK[a, :, 2, :] = np.concatenate([Gs, Gc], 1)
            PK[a, :, 3, :] = np.concatenate([Gc, -Gs], 1)
            PK[a, :, 4, :] = np.concatenate([Gc.T, Gs.T], 0)
            PK[a, :, 5, :] = np.concatenate([-Gs.T, Gc.T], 0)
            rot = np.exp(1j * np.pi * (2 * f + 1) / 4)
            Gp = (Gc + 1j * Gs) * rot
            Gpc, Gps = Gp.real, Gp.imag
            PKH[a, :, 0, :] = np.concatenate([Gpc, Gpc], 1)
            PKH[a, :, 1, :] = np.concatenate([-Gps, -Gps], 1)
            PKH[a, :, 2, :] = np.concatenate([-Gps, Gps], 1)
            PKH[a, :, 3, :] = np.concatenate([-Gpc, Gpc], 1)
        c["hy_PK_" + tg] = PK.astype(NPBF)
        c["hy_PKH_" + tg] = PKH.astype(NPBF)
    for C, hp in ((128, 4), (32, 2)):
        s_ = np.arange(C)[:, None]
        t_ = np.arange(C)[None, :]
        trif = (s_ <= t_).astype(np.float32)
        trib = (s_ >= t_).astype(np.float32)
        c["tri%d" % C] = np.stack([trif, trib], 1).copy()
        c["msk%d" % C] = np.stack([np.tile(trif, (1, hp)), np.tile(trib, (1, hp))], 1).astype(np.float32)
        dk = 128 // hp
        hm = np.zeros((128, hp, C), np.float32)
        bd = np.zeros((128, hp, 64), np.float32)
        for hh in range(hp):
            hm[hh * dk:(hh + 1) * dk, hh, :] = 1.0
            bd[hh * dk:(hh + 1) * dk, hh, :] = 1.0
        c["hm%d" % C] = hm
        c["bd%d" % C] = bd
    return c


class K:
    pass


def build_program(dbg=(), stop=None, feed=(), plan=None, need=None):
    nc = bass.Bass("TRN2", target_bir_lowering=False)
    S = Sched(nc)
    mem = Mem(nc, 206 * 1024)
    g = K()
    g.nc, g.S, g.mem = nc, S, mem
    g.dbg = {}

    def din(name, shape, dt=F32):
        if need is not None and name not in need:
            return nc.dram_tensor(name, list(shape), dt, kind="Internal").ap()
        return nc.dram_tensor(name, list(shape), dt, kind="ExternalInput").ap()

    def dscr(name, shape, dt=F32):
        kind = "ExternalOutput" if name in dbg else ("ExternalInput" if name in feed else "Internal")
        t = nc.dram_tensor(name, list(shape), dt, kind=kind).ap()
        if name in dbg:
            g.dbg[name] = t
        return t
    g.dscr = dscr

    I = {}
    I["x"] = din("x", [NB, TL, D])
    I["ctx"] = din("ctx", [NB, TC, D])
    I["ccT"] = din("ccT", [128, 8, 3])
    I["ada_w"] = din("ada_w", [DEPTH, D, 6 * D])
    I["ada_b"] = din("ada_b", [DEPTH, 6 * D])
    I["norm1_g"] = din("norm1_g", [DEPTH, D])
    I["norm2_g"] = din("norm2_g", [DEPTH, D])
    I["w_in"] = din("w_in", [DEPTH, D, NIN])
    for nm, shp in (("gla_wa2", [DEPTH, 2, 16, 128]), ("gla_ba", [DEPTH, 2, 128]), ("gla_norm_g", [DEPTH, 64]),
                    ("hy_conv_w", [DEPTH, 3, 768]), ("hy_w1", [DEPTH, 33, 64]), ("hy_b1", [DEPTH, 64]),
                    ("hy_w2", [DEPTH, 64, 64]), ("hy_b2", [DEPTH, 64]), ("hy_w3", [DEPTH, 64, 512]),
                    ("hy_freq", [DEPTH, 2, 64]), ("hy_decay", [DEPTH, 512]), ("hy_bias", [DEPTH, 2, 256]),
                    ("hg_lower", [DEPTH, 2, 256]), ("hg_norm_g", [DEPTH, 64]), ("da_qnorm_g", [DEPTH, 32]),
                    ("da_knorm_g", [DEPTH, 32]), ("da_lam", [DEPTH, 4, 32]), ("da_norm_g", [DEPTH, 64]),
                    ("w_branch", [DEPTH, 4, 256, D]), ("w_out", [DEPTH, D, D]), ("moe_router", [DEPTH, D, 16]),
                    ("moe_w1", [DEPTH, 16, D, D]), ("moe_w3", [DEPTH, 16, D, D]), ("moe_w2", [DEPTH, 16, D, D])):
        I[nm] = din(nm, shp)
    cst = host_consts()
    for k, v in cst.items():
        I[k] = din(k, v.shape, BF16 if v.dtype == NPBF else F32)
    g.I = I
    OUT = nc.dram_tensor("out", [NB, TL, D], F32, kind="ExternalOutput").ap()
    g.OUT = OUT
    g.XC = dscr("XC", [NB, TC, D])
    g.MODS = dscr("MODS", [DEPTH, 3, 6 * D])
    g.HTOK = dscr("HTOK", [TT, D], BF16)
    g.QK_GLA = dscr("QK_GLA", [256, TT])
    g.AFB = dscr("AFB", [32, TT])
    g.V_GLA = dscr("V_GLA", [TT, 256], BF16)
    g.G_GLA = dscr("G_GLA", [TT, 256], BF16)
    g.HY = dscr("HY", [TT, 768])
    g.Q_HG = dscr("Q_HG", [256, TT])
    g.ZT_HG = dscr("ZT_HG", [512, TT])
    g.Z_HG = dscr("Z_HG", [TT, 512])
    g.VG_HG = dscr("VG_HG", [TT, 512], BF16)
    g.QK_DA = dscr("QK_DA", [TT, 512])
    g.V_DA = dscr("V_DA", [TT, 256], BF16)
    g.MG = dscr("MG", [4096, TT], BF16)
    g.O_GLA = dscr("O_GLA", [2, TT, 256])
    g.O_HG = dscr("O_HG", [2, TT, 256])
    g.O_DA = dscr("O_DA", [1, TT, 256])
    g.BRT = dscr("BRT", [4, 256, TT], BF16)
    g.HFILT = {TL: dscr("HFILT_X", [TL, 512], BF16), TC: dscr("HFILT_C", [TC, 512], BF16)}
    g.HAB = {TL: dscr("HAB_X", [2, 64, 128, 512]), TC: dscr("HAB_C", [2, 4, 128, 512])}
    g.UCONV = dscr("UCONV", [TT, 768])
    g.ZB = dscr("ZB", [2, TT, 256], BF16)
    g.ZF = dscr("ZF", [TT, 256])
    g.AD = dscr("AD", [128, 128, 512], BF16)
    g.ZD = dscr("ZD", [128, 128, 256], BF16)

    psall = nc.alloc_psum_tensor("psall", [128, 8 * 512], F32).ap()
    g.PS = [psall[:, b * 512:(b + 1) * 512] for b in range(8)]
    g.PSB = [psall[:, b * 512:(b + 1) * 512].bitcast(BF16) for b in range(8)]
    g.psk = [[("ps", b, 0), ("ps", b, 1)] for b in range(8)]

    g.ident_bf = mem.alloc([128], BF16)
    g.ident_f = mem.alloc([128], F32)
    g.ones_f = mem.alloc([128], F32)
    S.dma(lambda e: e.dma_start(out=g.ident_bf.ap, in_=I["ident_bf"]), [], g.ident_bf.k())
    S.dma(lambda e: e.dma_start(out=g.ident_f.ap, in_=I["ident_f"]), [], g.ident_f.k())
    S.dma(lambda e: e.dma_start(out=g.ones_f.ap, in_=I["ones_f"]), [], g.ones_f.k())
    g.hT = mem.alloc([8, TT], BF16)

    for b in range(NB):
        for q in range(4):
            S.dma(lambda e, b=b, q=q: e.dma_start(out=OUT[b, q * 1024:(q + 1) * 1024, :], in_=I["x"][b, q * 1024:(q + 1) * 1024, :]),
                  [], dkeys(("XX", b), q * 1024, (q + 1) * 1024, 128))
        S.dma(lambda e, b=b: e.dma_start(out=g.XC[b], in_=I["ctx"][b]), [], dkeys(("XC", b), 0, TC, 128))

    if plan is not None:
        for (fn, args) in plan:
            globals()[fn](g, *args)
        return finish(g)
    stage_mods_all(g)
    for l in range(DEPTH):
        last = (l == DEPTH - 1)
        lam_init = 0.8 - 0.6 * math.exp(-0.3 * l)
        S.stage = "hy_filter"
        stage_hy_filter(g, l, TL)
        if not last:
            stage_hy_filter(g, l, TC)
        tok_lo = TC if last else 0
        for b in range(NB):
            S.stage = "norm1"
            stage_norm(g, l, b, 1)
            S.stage = "stage_proj"
            stage_proj(g, l, b)
            S.stage = "recur_gla"
            stage_recur(g, l, b, "gla")
            S.stage = "recur_hg"
            stage_recur(g, l, b, "hg")
            S.stage = "stage_da"
            stage_da(g, l, b, not last)
            S.stage = "stage_hy"
            stage_hy(g, l, b, not last)
            S.stage = "brnorm"
            stage_branch_norm(g, l, b, "O_GLA", "O_GLA", 2, "G_GLA", 0, "G_GLA", "gla_norm_g", 1.0, 0, tok_lo)
            S.stage = "brnorm"
            stage_branch_norm(g, l, b, "O_HG", "O_HG", 2, "VG_HG", 256, "VG_HG", "hg_norm_g", 1.0, 2, tok_lo)
            S.stage = "brnorm"
            stage_branch_norm(g, l, b, "O_DA", "O_DA", 1, None, 0, None, "da_norm_g", 1.0 - lam_init, 3, tok_lo)
            S.stage = "stage_merge"
            stage_merge(g, l, b, tok_lo)
            S.stage = "norm2"
            stage_norm(g, l, b, 2, segs=("x",) if last else ("c", "x"))
            S.stage = "stage_moe"
            stage_moe(g, l, b, not last)
    return finish(g)


def finish(g):
    fk = []
    for b in range(NB):
        fk += dkeys(("XX", b), 0, TL, 128)
    for name in g.dbg:
        fk.append(("DBG", name))
    for k in list(g.S.lastw.keys()):
        if isinstance(k, tuple) and len(k) >= 1 and isinstance(k[0], str) and k[0] in g.dbg:
            fk.append(k)
    g.S.emit(final_keys=fk)
    return g


def stage_mods_all(g):
    nc, S, mem, I = g.nc, g.S, g.mem, g.I
    m0 = mem.mark()
    scT = mem.alloc([8, 3], F32)
    S.dma(lambda e: e.dma_start(out=scT.ap, in_=I["ccT"]), [], scT.k())
    S.act(lambda e: e.activation(out=scT.ap, in_=scT.ap, func=AF.Silu), scT.k(), scT.k())
    modrow = mem.alloc([6 * D], F32, parts=3)
    bias = mem.alloc([6 * D], F32, parts=3)
    gbc = mem.alloc([2, D], F32, parts=3)
    wsl = [mem.alloc([8, 512], F32) for _ in range(2)]
    for l in range(DEPTH):
        S.dma(lambda e, l=l: e.dma_start(out=bias.ap, in_=I["ada_b"][l:l + 1, :].partition_broadcast(3)), [], bias.k())
        S.dma(lambda e, l=l: e.dma_start(out=gbc.ap[:, 0, :], in_=I["norm1_g"][l:l + 1, :].partition_broadcast(3)), [], gbc.k())
        S.dma(lambda e, l=l: e.dma_start(out=gbc.ap[:, 1, :], in_=I["norm2_g"][l:l + 1, :].partition_broadcast(3)), [], gbc.k())
        for n in range(12):
            w = wsl[n % 2]
            src = I["ada_w"][l].rearrange("(k p) n -> p k n", p=128)[:, :, n * 512:(n + 1) * 512]
            S.dma(lambda e, w=w, src=src: e.dma_start(out=w.ap, in_=src), [], w.k())
            bank = n % 2
            for k in range(8):
                S.pe(lambda e, w=w, k=k, bank=bank: e.matmul(g.PS[bank][0:3, :], lhsT=scT.ap[:, k, :], rhs=w.ap[:, k, :],
                                                            start=(k == 0), stop=(k == 7)),
                     scT.k() + w.k(), g.psk[bank])
            S.dve(lambda e, n=n, bank=bank: e.tensor_tensor(out=modrow.ap[:, n * 512:(n + 1) * 512], in0=g.PS[bank][0:3, :],
                                                              in1=bias.ap[:, n * 512:(n + 1) * 512], op=ALU.add),
                  g.psk[bank] + bias.k(), modrow.k(n * 512, (n + 1) * 512))
        for (ch, gi) in ((1, 0), (4, 1)):
            S.dve(lambda e, ch=ch, gi=gi: e.scalar_tensor_tensor(out=modrow.ap[:, ch * D:(ch + 1) * D], in0=modrow.ap[:, ch * D:(ch + 1) * D],
                                                                  scalar=1.0, in1=gbc.ap[:, gi, :], op0=ALU.add, op1=ALU.mult),
                  modrow.k(ch * D, (ch + 1) * D) + gbc.k(), modrow.k(ch * D, (ch + 1) * D))
        S.dma(lambda e, l=l: e.dma_start(out=g.MODS[l], in_=modrow.ap), modrow.k(), [("MODS", l)])
    mem.release(m0)


def load_mod_bc(g, l, j, ch, dst):
    src = g.MODS[l, j:j + 1, ch * D:(ch + 1) * D].partition_broadcast(128)
    g.S.dma(lambda e: e.dma_start(out=dst.ap, in_=src), [("MODS", l)], dst.k())


def stage_norm(g, l, b, which, segs=("c", "x")):
    nc, S, mem = g.nc, g.S, g.mem
    m0 = mem.mark()
    sbc = mem.alloc([D], F32)
    shbc = mem.alloc([D], F32)
    xts = [mem.alloc([D], F32) for _ in range(2)]
    t1s = [mem.alloc([D], F32) for _ in range(2)]
    hbs = [mem.alloc([D], BF16) for _ in range(2)]
    junk = mem.alloc([D], BF16)
    st = [mem.alloc([2], F32) for _ in range(2)]
    cnt = 0
    for seg in segs:
        j = 2 if seg == "c" else b
        load_mod_bc(g, l, j, 1 if which == 1 else 4, sbc)
        load_mod_bc(g, l, j, 0 if which == 1 else 3, shbc)
        ntile = TC // 128 if seg == "c" else TL // 128
        for i in range(ntile):
            tok0 = i * 128 if seg == "c" else TC + i * 128
            xt, t1, hb, s_ = xts[cnt % 2], t1s[cnt % 2], hbs[cnt % 2], st[cnt % 2]
            bank = 2 + cnt % 2
            cnt += 1
            if seg == "c":
                src, sk = g.XC[b, i * 128:(i + 1) * 128, :], [(("XC", b), i)]
            else:
                src, sk = g.OUT[b, i * 128:(i + 1) * 128, :], [(("XX", b), i)]
            S.dma(lambda e, xt=xt, src=src: e.dma_start(out=xt.ap, in_=src), sk, xt.k())
            S.act(lambda e, xt=xt, s_=s_: e.activation(out=junk.ap, in_=xt.ap, func=AF.Square, accum_out=s_.ap[:, 0:1]),
                  xt.k(), junk.k() + s_.k())
            S.act(lambda e, s_=s_: e.activation(out=s_.ap[:, 1:2], in_=s_.ap[:, 0:1], func=AF.Sqrt, bias=EPS, scale=1.0 / D),
                  s_.k(), s_.k())
            S.dve(lambda e, s_=s_: e.reciprocal(out=s_.ap[:, 1:2], in_=s_.ap[:, 1:2]), s_.k(), s_.k())
            S.dve(lambda e, xt=xt, t1=t1, s_=s_: e.scalar_tensor_tensor(out=t1.ap, in0=xt.ap, scalar=s_.ap[:, 1:2], in1=sbc.ap,
                                                                         op0=ALU.mult, op1=ALU.mult),
                  xt.k() + s_.k() + sbc.k(), t1.k())
            S.pool(lambda e, t1=t1, hb=hb: e.tensor_tensor(out=hb.ap, in0=t1.ap, in1=shbc.ap, op=ALU.add),
                   t1.k() + shbc.k(), hb.k())
            if which == 2:
                S.dma(lambda e, hb=hb, tok0=tok0: e.dma_start(out=g.HTOK[tok0:tok0 + 128, :], in_=hb.ap),
                      hb.k(), [("HTOK", tok0 // 128)])
            for c in range(8):
                S.pe(lambda e, hb=hb, c=c, bank=bank: e.transpose(out=g.PSB[bank][:, c * 128:(c + 1) * 128],
                                                                  in_=hb.ap[:, c * 128:(c + 1) * 128], identity=g.ident_bf.ap),
                     hb.k() + g.ident_bf.k(), g.psk[bank])
            dst = g.hT.ap[:, :, tok0:tok0 + 128]
            hk = []
            for c in range(8):
                hk += g.hT.k(c * TT + tok0, c * TT + tok0 + 128)
            srcp = g.PSB[bank].rearrange("p (c t) -> p c t", c=8)
            if cnt % 2 == 0:
                S.act(lambda e, dst=dst, srcp=srcp: e.activation(out=dst, in_=srcp, func=AF.Copy), g.psk[bank], hk)
            else:
                S.dve(lambda e, dst=dst, srcp=srcp: e.tensor_copy(out=dst, in_=srcp), g.psk[bank], hk)
    mem.release(m0)


def load_w_bf16(g, dst, src_ap, nk=8):
    src = src_ap.rearrange("(k p) n -> p k n", p=128)
    g.S.dma(lambda e: e.dma_start(out=dst.ap, in_=src), [], dst.k(), q="pq")


def stage_proj(g, l, b):
    nc, S, mem, I = g.nc, g.S, g.mem, g.I
    m0 = mem.mark()
    W = I["w_in"][l]
    wsl = [mem.alloc([8, 512], BF16) for _ in range(2)]
    stg = [mem.alloc([512], F32) for _ in range(4)]
    wcnt = [0]
    scnt = [0]
    pcnt = [0]
    slabs = [(s * 512, min(512, TT - s * 512)) for s in range((TT + 511) // 512)]

    def next_w(c0, width):
        w = wsl[wcnt[0] % 2]
        wcnt[0] += 1
        wv = Tn(w.ap[:, :, 0:width], w.off, w.nbytes, w.esize)
        src = W[:, c0:c0 + width].rearrange("(k p) n -> p k n", p=128)
        S.dma(lambda e: e.dma_start(out=wv.ap, in_=src), [], w.k(), q="pq")
        return w

    def evac(kind, out_ap, ps_ap, rk, wk, scale=1.0):
        if kind == "copy":
            if scnt[0] % 2 == 0:
                S.dve(lambda e: e.tensor_copy(out=out_ap, in_=ps_ap), rk, wk)
            else:
                S.act(lambda e: e.activation(out=out_ap, in_=ps_ap, func=AF.Copy), rk, wk)
        elif kind == "scale":
            S.act(lambda e: e.activation(out=out_ap, in_=ps_ap, func=AF.Copy, scale=scale), rk, wk)
        elif kind == "silu":
            S.act(lambda e: e.activation(out=out_ap, in_=ps_ap, func=AF.Silu), rk, wk)
        elif kind == "sigmoid":
            S.act(lambda e: e.activation(out=out_ap, in_=ps_ap, func=AF.Sigmoid), rk, wk)

    fm = [
        (OFF["gla_q"][0], 128, g.QK_GLA, 0, "scale", F32, "QK_GLA"),
        (OFF["gla_k"][0], 128, g.QK_GLA, 128, "copy", F32, "QK_GLA"),
        (OFF["gla_af"][0], 32, g.AFB, 0, "copy", F32, "AFB"),
        (OFF["hg_q"][0], 256, g.Q_HG, 0, "silu", F32, "Q_HG"),
        (OFF["hg_ff"][0], 512, g.ZT_HG, 0, "copy", F32, "ZT_HG"),
        (OFF["merge"][0], 4096, g.MG, 0, "sigmoid", BF16, "MG"),
    ]
    for (c0, ncols, dst, r0, kind, dt, kn) in fm:
        for cs in range(0, ncols, 512):
            cw = min(512, ncols - cs)
            w = next_w(c0 + cs, cw)
            for ch in range(0, cw, 128):
                m = min(128, cw - ch)
                for (t0, tw) in slabs:
                    bank = 4 + pcnt[0] % 4
                    pcnt[0] += 1
                    hk = []
                    for k in range(8):
                        hk += g.hT.k(k * TT + t0, k * TT + t0 + tw)
                    for k in range(8):
                        S.pe(lambda e, w=w, k=k, ch=ch, m=m, bank=bank, t0=t0, tw=tw:
                             e.matmul(g.PS[bank][0:m, 0:tw], lhsT=w.ap[:, k, ch:ch + m], rhs=g.hT.ap[:, k, t0:t0 + tw],
                                      start=(k == 0), stop=(k == 7)),
                             w.k() + hk, g.psk[bank])
                    st = stg[scnt[0] % 4]
                    scnt[0] += 1
                    if dt == BF16:
                        sv = st.ap.bitcast(BF16)[0:m, 0:tw]
                    else:
                        sv = st.ap[0:m, 0:tw]
                    evac(kind, sv, g.PS[bank][0:m, 0:tw], g.psk[bank], st.k(), scale=32 ** -0.5)
                    rr = r0 + cs + ch
                    S.dma(lambda e, dst=dst, rr=rr, m=m, t0=t0, tw=tw, sv=sv: e.dma_start(out=dst[rr:rr + m, t0:t0 + tw], in_=sv),
                          st.k(), [(kn, rr // 128, t0 // 512)])
    tmg = [
        (OFF["gla_v"][0], 256, g.V_GLA, 0, "copy", BF16, "V_GLA"),
        (OFF["gla_g"][0], 256, g.G_GLA, 0, "silu", BF16, "G_GLA"),
        (OFF["hy"][0], 512, g.HY, 0, "copy", F32, "HY"),
        (OFF["hy"][0] + 512, 256, g.HY, 512, "copy", F32, "HY"),
        (OFF["hg_ff"][0], 512, g.Z_HG, 0, "copy", F32, "Z_HG"),
        (OFF["hg_i"][0], 256, g.VG_HG, 0, "copy", BF16, "VG_HG"),
        (OFF["hg_g"][0], 256, g.VG_HG, 256, "silu", BF16, "VG_HG"),
        (OFF["da_q"][0], 512, g.QK_DA, 0, "copy", F32, "QK_DA"),
        (OFF["da_v"][0], 256, g.V_DA, 0, "copy", BF16, "V_DA"),
    ]
    for (c0, ncols, dst, dc0, kind, dt, kn) in tmg:
        w = next_w(c0, ncols)
        for i in range(TT // 128):
            t0 = i * 128
            bank = 4 + pcnt[0] % 4
            pcnt[0] += 1
            hk = []
            for k in range(8):
                hk += g.hT.k(k * TT + t0, k * TT + t0 + 128)
            for k in range(8):
                S.pe(lambda e, w=w, k=k, bank=bank, t0=t0, ncols=ncols:
                     e.matmul(g.PS[bank][:, 0:ncols], lhsT=g.hT.ap[:, k, t0:t0 + 128], rhs=w.ap[:, k, 0:ncols],
                              start=(k == 0), stop=(k == 7)),
                     w.k() + hk, g.psk[bank])
            st = stg[scnt[0] % 4]
            scnt[0] += 1
            if dt == BF16:
                sv = st.ap.bitcast(BF16)[:, 0:ncols]
            else:
                sv = st.ap[:, 0:ncols]
            evac(kind, sv, g.PS[bank][:, 0:ncols], g.psk[bank], st.k())
            S.dma(lambda e, dst=dst, t0=t0, dc0=dc0, ncols=ncols, sv=sv: e.dma_start(out=dst[t0:t0 + 128, dc0:dc0 + ncols], in_=sv),
                  st.k(), [(kn, i, dc0)])
    mem.release(m0)


_CACHE = {}


def make_in_maps(inputs):
    cst = host_consts()
    maps = []
    x = np.ascontiguousarray(inputs["x"], dtype=np.float32)
    ctx = np.ascontiguousarray(inputs["ctx"], dtype=np.float32)
    c = np.asarray(inputs["c"], dtype=np.float32)
    c_ctx = np.asarray(inputs["c_ctx"], dtype=np.float32)
    shared = {}
    for k in inputs:
        if k not in ("x", "c", "ctx", "c_ctx"):
            shared[k] = np.ascontiguousarray(inputs[k], dtype=np.float32)
    for core in range(NCORES):
        b0 = core * NB
        cc = np.stack([c[b0], c[b0 + 1], c_ctx], axis=1)
        ccT = np.ascontiguousarray(cc.reshape(8, 128, 3).transpose(1, 0, 2))
        m = {"x": x[b0:b0 + NB], "ctx": ctx[b0:b0 + NB], "ccT": ccT}
        m.update(shared)
        m.update(cst)
        maps.append(m)
    return maps


def kernel(**inputs):
    if "prog" not in _CACHE:
        _CACHE["prog"] = build_program()
    g = _CACHE["prog"]
    maps = make_in_maps(inputs)
    res = run_bass_kernel_spmd(g.nc, maps, core_ids=list(range(NCORES)))
    out = np.concatenate([np.asarray(r["out"]) for r in res.results], axis=0)
    return out.astype(np.float32)


def stage_recur(g, l, b, kind):
    nc, S, mem, I = g.nc, g.S, g.mem, g.I
    m0 = mem.mark()
    if kind == "gla":
        dk, C, hp, nt = 32, 128, 4, 1
        O = g.O_GLA
        okn = "O_GLA"
    else:
        dk, C, hp, nt = 64, 32, 2, 2
        O = g.O_HG
        okn = "O_HG"
    HD = 4 * dk
    BLK = 512
    tri = mem.alloc([2, C], F32, parts=C)
    msk = mem.alloc([2, hp * C], F32, parts=C)
    hm = mem.alloc([hp, C], F32)
    bd = mem.alloc([hp, 64], F32)
    S.dma(lambda e: e.dma_start(out=tri.ap, in_=I["tri%d" % C]), [], tri.k())
    S.dma(lambda e: e.dma_start(out=msk.ap, in_=I["msk%d" % C]), [], msk.k())
    S.dma(lambda e: e.dma_start(out=hm.ap, in_=I["hm%d" % C]), [], hm.k())
    S.dma(lambda e: e.dma_start(out=bd.ap, in_=I["bd%d" % C]), [], bd.k())
    if kind == "gla":
        S.dve(lambda e: e.tensor_scalar(out=tri.ap, in0=tri.ap, scalar1=-1.0 / 16.0, scalar2=None, op0=ALU.mult), tri.k(), tri.k())
        wa2b = mem.alloc([2, 128], F32, parts=17)
        for d_ in range(2):
            S.dma(lambda e, d_=d_: e.dma_start(out=wa2b.ap[0:16, d_, :], in_=I["gla_wa2"][l, d_]), [], wa2b.k())
            S.dma(lambda e, d_=d_: e.dma_start(out=wa2b.ap[16:17, d_, :], in_=I["gla_ba"][l, d_:d_ + 1, :]), [], wa2b.k())
    else:
        lbb = mem.alloc([2, 256], F32, parts=C)
        omb = mem.alloc([2, 256], F32, parts=C)
        lbp = mem.alloc([2, 2], F32)
        omp = mem.alloc([2, 2], F32)
        if l == 0:
            S.dve(lambda e: e.memset(lbb.ap, 0.0), [], lbb.k())
            S.dve(lambda e: e.memset(omb.ap, 1.0), [], omb.k())
            S.dve(lambda e: e.memset(lbp.ap, 0.0), [], lbp.k())
            S.dve(lambda e: e.memset(omp.ap, 1.0), [], omp.k())
        else:
            t0_ = mem.alloc([2, 256], F32, parts=C)
            S.dma(lambda e: e.dma_start(out=lbb.ap, in_=I["hg_lower"][1:2].rearrange("o d e -> o (d e)").partition_broadcast(C)
                                        .rearrange("p o (d e) -> p (o d) e", d=2)), [], lbb.k())
            S.dma(lambda e: e.dma_start(out=t0_.ap, in_=I["hg_lower"][0:1].rearrange("o d e -> o (d e)").partition_broadcast(C)
                                        .rearrange("p o (d e) -> p (o d) e", d=2)), [], t0_.k())
            S.dve(lambda e: e.tensor_tensor(out=lbb.ap, in0=lbb.ap, in1=t0_.ap, op=ALU.subtract), lbb.k() + t0_.k(), lbb.k())
            S.act(lambda e: e.activation(out=lbb.ap, in_=lbb.ap, func=AF.Sigmoid), lbb.k(), lbb.k())
            S.dve(lambda e: e.tensor_scalar(out=omb.ap, in0=lbb.ap, scalar1=-1.0, scalar2=1.0, op0=ALU.mult, op1=ALU.add), lbb.k(), omb.k())
            t1_ = mem.alloc([2, 2], F32)
            S.dma(lambda e: e.dma_start(out=lbp.ap, in_=I["hg_lower"][1].rearrange("d (t p) -> p d t", p=128), allow_slow_non_contiguous=True), [], lbp.k())
            S.dma(lambda e: e.dma_start(out=t1_.ap, in_=I["hg_lower"][0].rearrange("d (t p) -> p d t", p=128), allow_slow_non_contiguous=True), [], t1_.k())
            S.dve(lambda e: e.tensor_tensor(out=lbp.ap, in0=lbp.ap, in1=t1_.ap, op=ALU.subtract), lbp.k() + t1_.k(), lbp.k())
            S.act(lambda e: e.activation(out=lbp.ap, in_=lbp.ap, func=AF.Sigmoid), lbp.k(), lbp.k())
            S.dve(lambda e: e.tensor_scalar(out=omp.ap, in0=lbp.ap, scalar1=-1.0, scalar2=1.0, op0=ALU.mult, op1=ALU.add), lbp.k(), omp.k())
    St = [mem.alloc([64], F32) for _ in range(nt)]
    Sb = [mem.alloc([64], BF16) for _ in range(nt)]
    qTb = [[mem.alloc([BLK], F32) for _ in range(nt)] for _ in range(2)]
    kTb = [[mem.alloc([BLK], F32) for _ in range(nt)] for _ in range(2)]
    nchb = BLK // C
    lgb = [mem.alloc([nchb, HD], F32, parts=C) for _ in range(2)]
    vb = [mem.alloc([nchb, 256], BF16, parts=C) for _ in range(2)]
    if kind == "gla":
        af1 = [mem.alloc([BLK], F32, parts=17) for _ in range(2)]
        lgt = [mem.alloc([128], F32) for _ in range(2)]
    NSET = 4 if kind == "hg" else 2
    E1 = [mem.alloc([C], F32) for _ in range(NSET)]
    E2 = [mem.alloc([C], F32) for _ in range(NSET)]
    qe = [mem.alloc([C], BF16) for _ in range(NSET)]
    ke = [mem.alloc([C], BF16) for _ in range(NSET)]
    qeb = [mem.alloc([hp, C], BF16) for _ in range(NSET)]
    keT = [mem.alloc([128], BF16, parts=C) for _ in range(NSET)]
    Am = [mem.alloc([hp * C], BF16, parts=C) for _ in range(NSET)]
    tmpS = [mem.alloc([64], F32) for _ in range(NSET)]
    tmp2 = [mem.alloc([hp, 64], F32) for _ in range(NSET)]
    osb = [mem.alloc([256], F32, parts=C) for _ in range(2)]
    if kind == "hg":
        def hb_(bank, half):
            return g.PS[bank][:, half * 256:(half + 1) * 256], [("ps", bank, half)]
        bTv = [hb_(0, 0), hb_(0, 1), hb_(1, 0), hb_(1, 1)]
        ATv = [hb_(3, 0), hb_(3, 1), hb_(4, 0), hb_(4, 1)]
        Mv = [hb_(7, 0), hb_(7, 1), hb_(7, 0), hb_(7, 1)]
        kTv = [(g.PSB[2][:, 0:128], [("ps", 2, 0)]), (g.PSB[2][:, 512:640], [("ps", 2, 1)]), (g.PSB[2][:, 0:128], [("ps", 2, 0)]), (g.PSB[2][:, 512:640], [("ps", 2, 1)])]
    else:
        bTv = [(g.PS[0], g.psk[0]), (g.PS[1], g.psk[1])]
        ATv = [(g.PS[3], g.psk[3]), (g.PS[4], g.psk[4])]
        Mv = [(g.PS[7], g.psk[7]), (g.PS[7], g.psk[7])]
        kTv = [(g.PSB[2], g.psk[2]), (g.PSB[2], g.psk[2])]
    cc = [0]
    bcnt = [0]
    ocnt = [0]
    for dr in range(2):
        for t in range(nt):
            S.dve(lambda e, t=t: e.memset(St[t].ap, 0.0), [], St[t].k())
            S.pool(lambda e, t=t: e.memset(Sb[t].ap, 0.0), [], Sb[t].k())
        blocks = [(0, TC)] + [(TC + i * BLK, BLK) for i in range(TL // BLK)]
        if dr == 1:
            blocks = [(0, TC)] + [(TC + i * BLK, BLK) for i in reversed(range(TL // BLK))]
        for (t0, bw) in blocks:
            bi = bcnt[0] % 2
            bcnt[0] += 1
            nch = bw // C
            qT, kT, lg, vv = qTb[bi], kTb[bi], lgb[bi], vb[bi]
            if kind == "gla":
                S.dma(lambda e, qT=qT, t0=t0, bw=bw: e.dma_start(out=qT[0].ap[:, 0:bw], in_=g.QK_GLA[0:128, t0:t0 + bw]),
                      dkeys_fm("QK_GLA", 0, t0, bw), qT[0].k())
                S.dma(lambda e, kT=kT, t0=t0, bw=bw: e.dma_start(out=kT[0].ap[:, 0:bw], in_=g.QK_GLA[128:256, t0:t0 + bw]),
                      dkeys_fm("QK_GLA", 1, t0, bw), kT[0].k())
                a1 = af1[bi]
                S.dma(lambda e, a1=a1, t0=t0, bw=bw, dr=dr: e.dma_start(out=a1.ap[0:16, 0:bw], in_=g.AFB[dr * 16:(dr + 1) * 16, t0:t0 + bw]),
                      dkeys_fm("AFB", 0, t0, bw), a1.k())
                S.dma(lambda e, a1=a1, bw=bw: e.dma_start(out=a1.ap[16:17, 0:bw], in_=I["ones_row"][0:1, 0:bw]), [], a1.k())
                for c in range(nch):
                    pb = cc[0] % 2
                    lt = lgt[cc[0] % 2]
                    S.pe(lambda e, a1=a1, c=c, pb=pb, dr=dr: e.matmul(g.PS[pb][:, 0:128], lhsT=a1.ap[:, c * 128:(c + 1) * 128],
                                                                       rhs=wa2b.ap[:, dr, :], start=True, stop=True),
                         a1.k() + wa2b.k(), g.psk[pb])
                    S.act(lambda e, lt=lt, pb=pb: e.activation(out=lt.ap, in_=g.PS[pb][:, 0:128], func=AF.Exp, scale=-1.0),
                          g.psk[pb], lt.k())
                    S.act(lambda e, lt=lt, lg=lg, c=c: e.activation(out=lg.ap[:, c, :], in_=lt.ap, func=AF.Ln, bias=1.0, scale=1.0),
                          lt.k(), lg.k(c * HD, (c + 1) * HD))
                    cc[0] += 1
                S.dma(lambda e, vv=vv, t0=t0, bw=bw, nch=nch: e.dma_start(out=vv.ap[:, 0:nch, :],
                                                                           in_=g.V_GLA[t0:t0 + bw, :].rearrange("(c s) e -> s c e", s=C)),
                      dkeys_tm("V_GLA", 0, t0, bw), vv.k())
            else:
                for t in range(nt):
                    S.dma(lambda e, qT=qT, t=t, t0=t0, bw=bw: e.dma_start(out=qT[t].ap[:, 0:bw], in_=g.Q_HG[t * 128:(t + 1) * 128, t0:t0 + bw]),
                          dkeys_fm("Q_HG", t, t0, bw), qT[t].k())
                    r0 = dr * 256 + t * 128
                    S.dma(lambda e, kT=kT, t=t, t0=t0, bw=bw, r0=r0: e.dma_start(out=kT[t].ap[:, 0:bw], in_=g.ZT_HG[r0:r0 + 128, t0:t0 + bw]),
                          dkeys_fm("ZT_HG", r0 // 128, t0, bw), kT[t].k())
                    S.act(lambda e, kT=kT, t=t, bw=bw: e.activation(out=kT[t].ap[:, 0:bw], in_=kT[t].ap[:, 0:bw], func=AF.Sigmoid, scale=-1.0),
                          kT[t].k(), kT[t].k())
                    S.dve(lambda e, kT=kT, t=t, bw=bw, dr=dr: e.tensor_scalar(out=kT[t].ap[:, 0:bw], in0=kT[t].ap[:, 0:bw],
                                                                               scalar1=omp.ap[:, dr, t:t + 1], scalar2=None, op0=ALU.mult),
                          kT[t].k() + omp.k(), kT[t].k())
                S.dma(lambda e, lg=lg, t0=t0, bw=bw, nch=nch, dr=dr: e.dma_start(out=lg.ap[:, 0:nch, :],
                      in_=g.Z_HG[t0:t0 + bw, dr * 256:(dr + 1) * 256].rearrange("(c s) e -> s c e", s=C)),
                      dkeys_tm("Z_HG", 0, t0, bw), lg.k())
                lgv = lg.ap[:, 0:nch, :]
                S.act(lambda e, lgv=lgv: e.activation(out=lgv, in_=lgv, func=AF.Sigmoid), lg.k(), lg.k())
                S.dve(lambda e, lgv=lgv, nch=nch, dr=dr: e.tensor_tensor(out=lgv, in0=lgv, in1=omb.ap[:, dr:dr + 1, :].broadcast_to([C, nch, 256]),
                                                                          op=ALU.mult), lg.k() + omb.k(), lg.k())
                S.dve(lambda e, lgv=lgv, nch=nch, dr=dr: e.tensor_tensor(out=lgv, in0=lgv, in1=lbb.ap[:, dr:dr + 1, :].broadcast_to([C, nch, 256]),
                                                                          op=ALU.add), lg.k() + lbb.k(), lg.k())
                S.dve(lambda e, lgv=lgv: e.tensor_scalar(out=lgv, in0=lgv, scalar1=1e-20, scalar2=None, op0=ALU.max), lg.k(), lg.k())
                S.act(lambda e, lgv=lgv: e.activation(out=lgv, in_=lgv, func=AF.Ln), lg.k(), lg.k())
                S.dma(lambda e, vv=vv, t0=t0, bw=bw, nch=nch: e.dma_start(out=vv.ap[:, 0:nch, :],
                                                                           in_=g.VG_HG[t0:t0 + bw, 0:256].rearrange("(c s) e -> s c e", s=C)),
                      dkeys_tm("VG_HG", 0, t0, bw), vv.k())
            chs = list(range(nch)) if dr == 0 else list(reversed(range(nch)))
            for c in chs:
                o_bank = 5 + ocnt[0] % 2
                ob = osb[ocnt[0] % 2]
                ocnt[0] += 1
                for t in range(nt):
                    i2 = cc[0] % NSET
                    cc[0] += 1
                    bT_ap, bT_k = bTv[i2]
                    AT_ap, AT_k = ATv[i2]
                    M_ap, M_k = Mv[i2]
                    kT_ap, kT_k = kTv[i2]
                    e1, e2, qe_, ke_, qb_, kt_, am_, ts_, t2_ = E1[i2], E2[i2], qe[i2], ke[i2], qeb[i2], keT[i2], Am[i2], tmpS[i2], tmp2[i2]
                    S.pe(lambda e, lg=lg, c=c, t=t, bT_ap=bT_ap, dr=dr: e.matmul(bT_ap[:, 0:C], lhsT=lg.ap[:, c, t * 128:(t + 1) * 128],
                                                                            rhs=tri.ap[:, dr, :], start=True, stop=True),
                         lg.k(c * HD, (c + 1) * HD) + tri.k(), bT_k)
                    S.act(lambda e, e1=e1, bT_ap=bT_ap: e.activation(out=e1.ap, in_=bT_ap[:, 0:C], func=AF.Exp), bT_k, e1.k())
                    S.act(lambda e, e2=e2, bT_ap=bT_ap: e.activation(out=e2.ap, in_=bT_ap[:, 0:C], func=AF.Exp, scale=-1.0), bT_k, e2.k())
                    S.dve(lambda e, qe_=qe_, e1=e1, qT=qT, t=t, c=c: e.tensor_tensor(out=qe_.ap, in0=qT[t].ap[:, c * C:(c + 1) * C], in1=e1.ap, op=ALU.mult),
                          qT[t].k() + e1.k(), qe_.k())
                    S.dve(lambda e, ke_=ke_, e2=e2, kT=kT, t=t, c=c: e.tensor_tensor(out=ke_.ap, in0=kT[t].ap[:, c * C:(c + 1) * C], in1=e2.ap, op=ALU.mult),
                          kT[t].k() + e2.k(), ke_.k())
                    S.pool(lambda e, qb_=qb_, qe_=qe_: e.tensor_tensor(out=qb_.ap, in0=qe_.ap[:, None, :].broadcast_to([128, hp, C]), in1=hm.ap, op=ALU.mult),
                           qe_.k() + hm.k(), qb_.k())
                    S.pe(lambda e, ke_=ke_, kT_ap=kT_ap: e.transpose(out=kT_ap[0:C, 0:128], in_=ke_.ap, identity=g.ident_bf.ap),
                         ke_.k() + g.ident_bf.k(), kT_k)
                    S.act(lambda e, kt_=kt_, kT_ap=kT_ap: e.activation(out=kt_.ap, in_=kT_ap[0:C, 0:128], func=AF.Copy), kT_k, kt_.k())
                    S.pe(lambda e, ke_=ke_, qb_=qb_, AT_ap=AT_ap: e.matmul(AT_ap[0:C, 0:hp * C], lhsT=ke_.ap, rhs=qb_.ap.rearrange("p h c -> p (h c)"),
                                                                       start=True, stop=True),
                         ke_.k() + qb_.k(), AT_k)
                    S.dve(lambda e, am_=am_, AT_ap=AT_ap, dr=dr: e.tensor_tensor(out=am_.ap, in0=AT_ap[0:C, 0:hp * C], in1=msk.ap[:, dr, :], op=ALU.mult),
                          AT_k + msk.k(), am_.k())
                    for hh in range(hp):
                        hcol = (t * hp + hh) * 64
                        S.pe(lambda e, am_=am_, hh=hh, hcol=hcol, vv=vv, c=c, o_bank=o_bank:
                             e.matmul(g.PS[o_bank][0:C, hcol:hcol + 64], lhsT=am_.ap[:, hh * C:(hh + 1) * C], rhs=vv.ap[:, c, hcol:hcol + 64],
                                      start=True, stop=False),
                             am_.k() + vv.k(), g.psk[o_bank])
                        S.pe(lambda e, qb_=qb_, hh=hh, hcol=hcol, t=t, o_bank=o_bank:
                             e.matmul(g.PS[o_bank][0:C, hcol:hcol + 64], lhsT=qb_.ap[:, hh, :], rhs=Sb[t].ap,
                                      start=False, stop=True),
                             qb_.k() + Sb[t].k(), g.psk[o_bank])
                    S.pe(lambda e, kt_=kt_, vv=vv, c=c, t=t, M_ap=M_ap: e.matmul(M_ap[:, 0:hp * 64], lhsT=kt_.ap, rhs=vv.ap[:, c, t * hp * 64:(t + 1) * hp * 64],
                                                                       start=True, stop=True),
                         kt_.k() + vv.k(), M_k)
                    ecol = (C - 1) if dr == 0 else 0
                    S.dve(lambda e, ts_=ts_, t=t, e1=e1, ecol=ecol: e.tensor_scalar(out=ts_.ap, in0=St[t].ap, scalar1=e1.ap[:, ecol:ecol + 1], scalar2=None, op0=ALU.mult),
                          St[t].k() + e1.k(), ts_.k())
                    S.dve(lambda e, t2_=t2_, M_ap=M_ap: e.tensor_tensor(out=t2_.ap.rearrange("p h e -> p (h e)"), in0=M_ap[:, 0:hp * 64], in1=bd.ap.rearrange("p h e -> p (h e)"), op=ALU.mult),
                          M_k + bd.k(), t2_.k())
                    for hh in range(hp):
                        src1 = ts_ if hh == 0 else St[t]
                        S.dve(lambda e, t2_=t2_, hh=hh, t=t, e1=e1, ecol=ecol, src1=src1:
                              e.scalar_tensor_tensor(out=St[t].ap, in0=t2_.ap[:, hh, :], scalar=e1.ap[:, ecol:ecol + 1], in1=src1.ap, op0=ALU.mult, op1=ALU.add),
                              t2_.k() + e1.k() + src1.k() + St[t].k(), St[t].k())
                    S.act(lambda e, t=t: e.activation(out=Sb[t].ap, in_=St[t].ap, func=AF.Copy), St[t].k(), Sb[t].k())
                S.dve(lambda e, ob=ob, o_bank=o_bank: e.tensor_copy(out=ob.ap, in_=g.PS[o_bank][0:C, 0:256]), g.psk[o_bank], ob.k())
                tt = t0 + c * C
                S.dma(lambda e, ob=ob, tt=tt, dr=dr: e.dma_start(out=O[dr, tt:tt + C, :], in_=ob.ap), ob.k(), [(okn, dr, u) for u in range(tt // 32, (tt + C) // 32)])
    mem.release(m0)


def dkeys_fm(name, rowtile, t0, bw):
    return [(name, rowtile, s) for s in range(t0 // 512, (t0 + bw - 1) // 512 + 1)]


def dkeys_tm(name, dc0, t0, bw):
    return [(name, i, dc0) for i in range(t0 // 128, (t0 + bw - 1) // 128 + 1)]


def stage_branch_norm(g, l, b, Osrc, okn, ndir, gate, gate_c0, gkn, gname, cscale, br, tok_lo=0):
    nc, S, mem, I = g.nc, g.S, g.mem, g.I
    if isinstance(Osrc, str):
        Osrc = getattr(g, Osrc)
    if isinstance(gate, str):
        gate = getattr(g, gate)
    m0 = mem.mark()
    gbc = mem.alloc([64], F32)
    S.dma(lambda e: e.dma_start(out=gbc.ap, in_=I[gname][l:l + 1, :].partition_broadcast(128)), [], gbc.k())
    if cscale != 1.0:
        S.dve(lambda e: e.tensor_scalar(out=gbc.ap, in0=gbc.ap, scalar1=float(cscale), scalar2=None, op0=ALU.mult), gbc.k(), gbc.k())
    of = [mem.alloc([256], F32) for _ in range(4)]
    ob = [mem.alloc([256], F32) for _ in range(4)]
    gt = [mem.alloc([256], BF16) for _ in range(4)]
    sq = [mem.alloc([256], F32) for _ in range(4)]
    ss = [mem.alloc([8], F32) for _ in range(4)]
    yb = [mem.alloc([256], BF16) for _ in range(4)]
    stg = [mem.alloc([2, 128], BF16) for _ in range(4)]
    for i in range(tok_lo // 128, TT // 128):
        t0 = i * 128
        u = i % 4
        o1, o2, g_, sq_, ss_, y_, st_ = of[u], ob[u], gt[u], sq[u], ss[u], yb[u], stg[u]
        okeys = lambda dr: [(okn, dr, v) for v in range(t0 // 32, t0 // 32 + 4)]
        S.dma(lambda e, o1=o1, t0=t0: e.dma_start(out=o1.ap, in_=Osrc[0, t0:t0 + 128, :]), okeys(0), o1.k())
        if ndir == 2:
            S.dma(lambda e, o2=o2, t0=t0: e.dma_start(out=o2.ap, in_=Osrc[1, t0:t0 + 128, :]), okeys(1), o2.k())
            S.pool(lambda e, o1=o1, o2=o2: e.tensor_tensor(out=o1.ap, in0=o1.ap, in1=o2.ap, op=ALU.add), o1.k() + o2.k(), o1.k())
        if gate is not None:
            S.dma(lambda e, g_=g_, t0=t0: e.dma_start(out=g_.ap, in_=gate[t0:t0 + 128, gate_c0:gate_c0 + 256]), [(gkn, i, gate_c0)], g_.k())
        S.dve(lambda e, o1=o1, sq_=sq_: e.tensor_tensor(out=sq_.ap, in0=o1.ap, in1=o1.ap, op=ALU.mult), o1.k(), sq_.k())
        S.dve(lambda e, sq_=sq_, ss_=ss_: e.tensor_reduce(out=ss_.ap[:, 0:4], in_=sq_.ap.rearrange("p (h e) -> p h e", h=4), axis=AX.X, op=ALU.add),
              sq_.k(), ss_.k())
        S.act(lambda e, ss_=ss_: e.activation(out=ss_.ap[:, 4:8], in_=ss_.ap[:, 0:4], func=AF.Sqrt, bias=EPS, scale=1.0 / 64), ss_.k(), ss_.k())
        S.dve(lambda e, ss_=ss_: e.reciprocal(out=ss_.ap[:, 4:8], in_=ss_.ap[:, 4:8]), ss_.k(), ss_.k())
        S.dve(lambda e, o1=o1, ss_=ss_, sq_=sq_: e.tensor_tensor(out=sq_.ap.rearrange("p (h e) -> p h e", h=4), in0=o1.ap.rearrange("p (h e) -> p h e", h=4),
                                                                in1=ss_.ap[:, 4:8, None].broadcast_to([128, 4, 64]), op=ALU.mult),
              o1.k() + ss_.k(), sq_.k())
        if gate is not None:
            S.pool(lambda e, sq_=sq_: e.tensor_tensor(out=sq_.ap.rearrange("p (h e) -> p h e", h=4), in0=sq_.ap.rearrange("p (h e) -> p h e", h=4),
                                                     in1=gbc.ap[:, None, :].broadcast_to([128, 4, 64]), op=ALU.mult), sq_.k() + gbc.k(), sq_.k())
            S.dve(lambda e, sq_=sq_, g_=g_, y_=y_: e.tensor_tensor(out=y_.ap, in0=sq_.ap, in1=g_.ap, op=ALU.mult), sq_.k() + g_.k(), y_.k())
        else:
            S.pool(lambda e, sq_=sq_, y_=y_: e.tensor_tensor(out=y_.ap.rearrange("p (h e) -> p h e", h=4), in0=sq_.ap.rearrange("p (h e) -> p h e", h=4),
                                                            in1=gbc.ap[:, None, :].broadcast_to([128, 4, 64]), op=ALU.mult), sq_.k() + gbc.k(), y_.k())
        bank = 2 + u % 2
        for c in range(2):
            S.pe(lambda e, y_=y_, c=c, bank=bank: e.transpose(out=g.PSB[bank][:, c * 128:(c + 1) * 128], in_=y_.ap[:, c * 128:(c + 1) * 128],
                                                              identity=g.ident_bf.ap), y_.k() + g.ident_bf.k(), g.psk[bank])
        S.act(lambda e, st_=st_, bank=bank: e.activation(out=st_.ap, in_=g.PSB[bank][:, 0:256].rearrange("p (c t) -> p c t", c=2), func=AF.Copy),
              g.psk[bank], st_.k())
        S.dma(lambda e, st_=st_, t0=t0: e.dma_start(out=g.BRT[br].rearrange("(c p) t -> p c t", p=128)[:, :, t0:t0 + 128], in_=st_.ap),
              st_.k(), [("BRT", br, i)])
    mem.release(m0)


def stage_merge(g, l, b, tok_lo=0):
    nc, S, mem, I = g.nc, g.S, g.mem, g.I
    m0 = mem.mark()
    wb = mem.alloc([4, 2, D], BF16)
    for i in range(4):
        S.dma(lambda e, i=i: e.dma_start(out=wb.ap[:, i], in_=I["w_branch"][l, i].rearrange("(k p) n -> p k n", p=128)), [], wb.k(), q="pq")
    wo = mem.alloc([8, D], BF16)
    S.dma(lambda e: e.dma_start(out=wo.ap, in_=I["w_out"][l].rearrange("(k p) n -> p k n", p=128)), [], wo.k(), q="pq")
    mT = g.hT
    brt = [mem.alloc([4, 2, 512], BF16) for _ in range(2)]
    gts = [mem.alloc([512], BF16) for _ in range(4)]
    tm = [mem.alloc([512], F32) for _ in range(4)]
    slabs = [(s * 512, min(512, TT - s * 512)) for s in range((TT + 511) // 512) if s * 512 + 512 > tok_lo]
    gc = [0]
    for si, (t0, tw) in enumerate(slabs):
        bt = brt[si % 2]
        for i in range(4):
            S.dma(lambda e, bt=bt, i=i, t0=t0, tw=tw: e.dma_start(out=bt.ap[:, i, :, 0:tw], in_=g.BRT[i].rearrange("(c p) t -> p c t", p=128)[:, :, t0:t0 + tw]),
                  [("BRT", i, v) for v in range(t0 // 128, (t0 + tw) // 128)], bt.k())
        for c in range(8):
            for i in range(4):
                bank = 4 + i
                for kc in range(2):
                    S.pe(lambda e, bt=bt, i=i, kc=kc, c=c, bank=bank, tw=tw: e.matmul(g.PS[bank][:, 0:tw], lhsT=wb.ap[:, i, kc, c * 128:(c + 1) * 128],
                                                                                     rhs=bt.ap[:, i, kc, 0:tw], start=(kc == 0), stop=(kc == 1)),
                         wb.k() + bt.k(), g.psk[bank])
                gt = gts[gc[0] % 4]
                gc[0] += 1
                r0 = i * 1024 + c * 128
                S.dma(lambda e, gt=gt, r0=r0, t0=t0, tw=tw: e.dma_start(out=gt.ap[:, 0:tw], in_=g.MG[r0:r0 + 128, t0:t0 + tw]),
                      [("MG", r0 // 128, t0 // 512)], gt.k())
                S.dve(lambda e, i=i, gt=gt, bank=bank, tw=tw: e.tensor_tensor(out=tm[i].ap[:, 0:tw], in0=g.PS[bank][:, 0:tw], in1=gt.ap[:, 0:tw], op=ALU.mult),
                      g.psk[bank] + gt.k(), tm[i].k())
            S.pool(lambda e, tw=tw: e.tensor_tensor(out=tm[0].ap[:, 0:tw], in0=tm[0].ap[:, 0:tw], in1=tm[1].ap[:, 0:tw], op=ALU.add), tm[0].k() + tm[1].k(), tm[0].k())
            S.pool(lambda e, tw=tw: e.tensor_tensor(out=tm[2].ap[:, 0:tw], in0=tm[2].ap[:, 0:tw], in1=tm[3].ap[:, 0:tw], op=ALU.add), tm[2].k() + tm[3].k(), tm[2].k())
            S.pool(lambda e, c=c, t0=t0, tw=tw: e.tensor_tensor(out=mT.ap[:, c, t0:t0 + tw], in0=tm[0].ap[:, 0:tw], in1=tm[2].ap[:, 0:tw], op=ALU.add),
                   tm[0].k() + tm[2].k(), mT.k(c * TT + t0, c * TT + t0 + tw))
    gbc = [mem.alloc([D], F32) for _ in range(2)]
    load_mod_bc(g, l, 2, 2, gbc[0])
    load_mod_bc(g, l, b, 2, gbc[1])
    xr = [mem.alloc([D], F32) for _ in range(2)]
    tp = [mem.alloc([D], F32) for _ in range(2)]
    for i in range(tok_lo // 128, TT // 128):
        t0 = i * 128
        u = i % 2
        isc = t0 < TC
        gb = gbc[0] if isc else gbc[1]
        if isc:
            dst, xk = g.XC[b, t0:t0 + 128, :], [(("XC", b), i)]
        else:
            dst, xk = g.OUT[b, t0 - TC:t0 - TC + 128, :], [(("XX", b), (t0 - TC) // 128)]
        x_, t_ = xr[u], tp[u]
        S.dma(lambda e, x_=x_, dst=dst: e.dma_start(out=x_.ap, in_=dst), xk, x_.k())
        mk = []
        for k in range(8):
            mk += mT.k(k * TT + t0, k * TT + t0 + 128)
        for hf in range(2):
            bank = 2 * u + hf
            for k in range(8):
                S.pe(lambda e, k=k, hf=hf, bank=bank, t0=t0: e.matmul(g.PS[bank], lhsT=mT.ap[:, k, t0:t0 + 128], rhs=wo.ap[:, k, hf * 512:(hf + 1) * 512],
                                                                      start=(k == 0), stop=(k == 7)), mk + wo.k(), g.psk[bank])
            S.dve(lambda e, t_=t_, hf=hf, bank=bank, gb=gb: e.tensor_tensor(out=t_.ap[:, hf * 512:(hf + 1) * 512], in0=g.PS[bank], in1=gb.ap[:, hf * 512:(hf + 1) * 512], op=ALU.mult),
                  g.psk[bank] + gb.k(), t_.k(hf * 512, (hf + 1) * 512))
        S.pool(lambda e, x_=x_, t_=t_: e.tensor_tensor(out=x_.ap, in0=x_.ap, in1=t_.ap, op=ALU.add), x_.k() + t_.k(), x_.k())
        S.dma(lambda e, x_=x_, dst=dst: e.dma_start(out=dst, in_=x_.ap), x_.k(), xk)
    mem.release(m0)


def bound_reg(g, e, bound):
    if not hasattr(g, "_bregs"):
        g._bregs = {}
    if bound not in g._bregs:
        g._bregs[bound] = e.to_reg(bound)
    return g._bregs[bound]


def stage_moe(g, l, b, do_ctx):
    nc, S, mem, I = g.nc, g.S, g.mem, g.I
    m0 = mem.mark()
    segs = [("x", TC, TL, 512, b, b * TL)]
    if do_ctx:
        segs.append(("c", 0, TC, 32, 2, b * TC))
    rw = mem.alloc([8, 16], BF16)
    S.dma(lambda e: e.dma_start(out=rw.ap, in_=I["moe_router"][l].rearrange("(k p) n -> p k n", p=128)), [], rw.k(), q="pq")
    NS = 5
    idxg = mem.alloc([NS, 16], I32)
    idxs = mem.alloc([NS, 16], I32)
    gT = mem.alloc([NS, 16], F32)
    S.dve(lambda e: e.memset(idxg.ap, 0), [], idxg.k())
    S.dve(lambda e: e.memset(idxs.ap, 0), [], idxs.k())
    m1 = mem.mark()
    affT = mem.alloc([TT], F32, parts=16)
    vals = mem.alloc([512], F32, parts=16)
    idx = mem.alloc([512], U32, parts=16)
    idxf = mem.alloc([512], F32, parts=16)
    sm = [mem.alloc([4], F32) for _ in range(2)]
    ex = [mem.alloc([16], F32) for _ in range(2)]
    tlo = 0 if do_ctx else TC
    for i in range(tlo // 128, TT // 128):
        t0 = i * 128
        u = i % 2
        s_, e_ = sm[u], ex[u]
        hk = []
        for k in range(8):
            hk += g.hT.k(k * TT + t0, k * TT + t0 + 128)
        for k in range(8):
            S.pe(lambda e, k=k, u=u, t0=t0: e.matmul(g.PS[u][:, 0:16], lhsT=g.hT.ap[:, k, t0:t0 + 128], rhs=rw.ap[:, k, :], start=(k == 0), stop=(k == 7)),
                 hk + rw.k(), g.psk[u])
        S.dve(lambda e, s_=s_, u=u: e.tensor_reduce(out=s_.ap[:, 0:1], in_=g.PS[u][:, 0:16], axis=AX.X, op=ALU.max), g.psk[u], s_.k())
        S.dve(lambda e, s_=s_: e.tensor_scalar(out=s_.ap[:, 1:2], in0=s_.ap[:, 0:1], scalar1=-1.0, scalar2=None, op0=ALU.mult), s_.k(), s_.k())
        S.act(lambda e, s_=s_, e_=e_, u=u: e.activation(out=e_.ap, in_=g.PS[u][:, 0:16], func=AF.Exp, bias=s_.ap[:, 1:2], scale=1.0, accum_out=s_.ap[:, 2:3]),
              g.psk[u] + s_.k(), e_.k() + s_.k())
        S.dve(lambda e, s_=s_: e.reciprocal(out=s_.ap[:, 3:4], in_=s_.ap[:, 2:3]), s_.k(), s_.k())
        S.dve(lambda e, s_=s_, e_=e_: e.tensor_scalar(out=e_.ap, in0=e_.ap, scalar1=s_.ap[:, 3:4], scalar2=None, op0=ALU.mult), e_.k() + s_.k(), e_.k())
        S.pe(lambda e, e_=e_: e.transpose(out=g.PS[2][0:16, 0:128], in_=e_.ap, identity=g.ident_f.ap), e_.k() + g.ident_f.k(), g.psk[2])
        S.act(lambda e, t0=t0: e.activation(out=affT.ap[:, t0:t0 + 128], in_=g.PS[2][0:16, 0:128], func=AF.Copy), g.psk[2], affT.k(t0, t0 + 128))
    for (sg, s0, slen, cap, jm, soff) in segs:
        work = affT.ap[:, s0:s0 + slen]
        wk = affT.k(s0, s0 + slen)
        for r in range(cap // 8):
            S.dve(lambda e, r=r, work=work: e.max(out=vals.ap[:, r * 8:(r + 1) * 8], in_=work), wk, vals.k(r * 8, (r + 1) * 8))
            S.dve(lambda e, r=r, work=work: e.max_index(out=idx.ap[:, r * 8:(r + 1) * 8], in_max=vals.ap[:, r * 8:(r + 1) * 8], in_values=work),
                  wk + vals.k(r * 8, (r + 1) * 8), idx.k(r * 8, (r + 1) * 8))
            S.dve(lambda e, r=r, work=work: e.match_replace(out=work, in_to_replace=vals.ap[:, r * 8:(r + 1) * 8], in_values=work, imm_value=-1.0),
                  wk + vals.k(r * 8, (r + 1) * 8), wk)
        S.dve(lambda e, cap=cap: e.tensor_copy(out=idxf.ap[:, 0:cap], in_=idx.ap[:, 0:cap]), idx.k(), idxf.k())
        nblk = (cap + 127) // 128
        for jb in range(nblk):
            ns = min(128, cap - jb * 128)
            j = jb if sg == "x" else 4
            S.pe(lambda e, jb=jb, ns=ns: e.transpose(out=g.PS[2][0:ns, 0:16], in_=idxf.ap[:, jb * 128:jb * 128 + ns], identity=g.ident_f.ap[0:16, 0:16]),
                 idxf.k() + g.ident_f.k(), g.psk[2])
            S.dve(lambda e, j=j, ns=ns, s0=s0: e.tensor_scalar(out=idxg.ap[0:ns, j, :], in0=g.PS[2][0:ns, 0:16], scalar1=float(s0), scalar2=None, op0=ALU.add),
                  g.psk[2], idxg.k())
            S.dve(lambda e, j=j, ns=ns, soff=soff: e.tensor_scalar(out=idxs.ap[0:ns, j, :], in0=g.PS[2][0:ns, 0:16], scalar1=float(soff), scalar2=None, op0=ALU.add),
                  g.psk[2], idxs.k())
            S.pe(lambda e, jb=jb, ns=ns: e.transpose(out=g.PS[3][0:ns, 0:16], in_=vals.ap[:, jb * 128:jb * 128 + ns], identity=g.ident_f.ap[0:16, 0:16]),
                 vals.k() + g.ident_f.k(), g.psk[3])
            S.act(lambda e, j=j, ns=ns: e.activation(out=gT.ap[0:ns, j, :], in_=g.PS[3][0:ns, 0:16], func=AF.Copy), g.psk[3], gT.k())
    mem.release(m1)
    wbuf = [mem.alloc([8, D], BF16) for _ in range(4)]
    wcnt = [0]
    NSL = 544
    xg = [mem.alloc([D], BF16) for _ in range(2)]
    xgT = mem.alloc([8, NSL], BF16)
    uT = mem.alloc([8, NSL], BF16)
    tmpa = [mem.alloc([512], F32) for _ in range(2)]
    ysb = [mem.alloc([D], F32) for _ in range(2)]
    mbc = {}
    for (sg, s0, slen, cap, jm, soff) in segs:
        mbc[sg] = mem.alloc([D], F32)
        load_mod_bc(g, l, jm, 5, mbc[sg])
    outflat = g.OUT.rearrange("a t d -> (a t) d")
    xcflat = g.XC.rearrange("a t d -> (a t) d")
    xxkeys = dkeys(("XX", b), 0, TL, 128)
    xckeys = dkeys(("XC", b), 0, TC, 128)
    htk = [("HTOK", i) for i in range(TT // 128)]

    def next_w(src):
        w = wbuf[wcnt[0] % 4]
        wcnt[0] += 1
        S.dma(lambda e: e.dma_start(out=w.ap, in_=src.rearrange("(k p) n -> p k n", p=128)), [], w.k(), q="pq")
        return w

    blocks = [("x", j, 128, j * 128) for j in range(4)]
    slabs = [(0, 512)]
    if do_ctx:
        blocks.append(("c", 4, 32, 512))
        slabs.append((512, 32))
    gcnt = [0]
    for ex_ in range(16):
        w1 = next_w(I["moe_w1"][l, ex_])
        w3 = next_w(I["moe_w3"][l, ex_])
        for (sg, j, ns, col0) in blocks:
            xg_ = xg[gcnt[0] % 2]
            gcnt[0] += 1
            S.dma(lambda e, xg_=xg_, j=j, ns=ns, ex_=ex_: e.indirect_dma_start(out=xg_.ap[0:ns, :], out_offset=None, in_=g.HTOK,
                                                                               in_offset=bass.IndirectOffsetOnAxis(ap=idxg.ap[0:ns, j, ex_:ex_ + 1], axis=0)),
                  idxg.k() + htk, xg_.k(), q="pq")
            for c in range(8):
                S.pe(lambda e, xg_=xg_, c=c, ns=ns: e.transpose(out=g.PSB[3][:, c * 128:c * 128 + ns], in_=xg_.ap[0:ns, c * 128:(c + 1) * 128],
                                                                identity=g.ident_bf.ap[0:ns, 0:ns]), xg_.k() + g.ident_bf.k(), g.psk[3])
            S.act(lambda e, ns=ns, col0=col0: e.activation(out=xgT.ap[:, :, col0:col0 + ns], in_=g.PSB[3].rearrange("p (c t) -> p c t", c=8)[:, :, 0:ns], func=AF.Copy),
                  g.psk[3], xgT.k())
        for fc in range(8):
            for (c0, cw) in slabs:
                ba, bb_ = (4, 5) if c0 == 0 else (2, 2)
                oa, ob_ = (0, 0) if c0 == 0 else (0, 64)
                for k in range(8):
                    S.pe(lambda e, k=k, fc=fc, c0=c0, cw=cw, ba=ba, oa=oa, w1=w1: e.matmul(g.PS[ba][:, oa:oa + cw], lhsT=w1.ap[:, k, fc * 128:(fc + 1) * 128],
                                                                                       rhs=xgT.ap[:, k, c0:c0 + cw], start=(k == 0), stop=(k == 7)),
                         w1.k() + xgT.k(), g.psk[ba])
                ta = tmpa[fc % 2]
                S.act(lambda e, ta=ta, ba=ba, oa=oa, cw=cw: e.activation(out=ta.ap[:, 0:cw], in_=g.PS[ba][:, oa:oa + cw], func=AF.Silu), g.psk[ba], ta.k())
                for k in range(8):
                    S.pe(lambda e, k=k, fc=fc, c0=c0, cw=cw, bb_=bb_, ob_=ob_, w3=w3: e.matmul(g.PS[bb_][:, ob_:ob_ + cw], lhsT=w3.ap[:, k, fc * 128:(fc + 1) * 128],
                                                                                            rhs=xgT.ap[:, k, c0:c0 + cw], start=(k == 0), stop=(k == 7)),
                         w3.k() + xgT.k(), g.psk[bb_])
                S.dve(lambda e, ta=ta, bb_=bb_, ob_=ob_, cw=cw, fc=fc, c0=c0: e.tensor_tensor(out=uT.ap[:, fc, c0:c0 + cw], in0=ta.ap[:, 0:cw], in1=g.PS[bb_][:, ob_:ob_ + cw], op=ALU.mult),
                      ta.k() + g.psk[bb_], uT.k(fc * NSL + c0, fc * NSL + c0 + cw))
        w2 = next_w(I["moe_w2"][l, ex_])
        for (sg, j, ns, col0) in blocks:
            y_ = ysb[j % 2]
            for hf in range(2):
                bank = 6 + hf
                for fc in range(8):
                    S.pe(lambda e, fc=fc, hf=hf, bank=bank, ns=ns, col0=col0, w2=w2: e.matmul(g.PS[bank][0:ns, :], lhsT=uT.ap[:, fc, col0:col0 + ns],
                                                                                         rhs=w2.ap[:, fc, hf * 512:(hf + 1) * 512], start=(fc == 0), stop=(fc == 7)),
                         uT.k() + w2.k(), g.psk[bank])
                S.dve(lambda e, y_=y_, hf=hf, bank=bank, ns=ns, j=j, ex_=ex_, sg=sg: e.scalar_tensor_tensor(out=y_.ap[0:ns, hf * 512:(hf + 1) * 512], in0=g.PS[bank][0:ns, :],
                      scalar=gT.ap[0:ns, j, ex_:ex_ + 1], in1=mbc[sg].ap[0:ns, hf * 512:(hf + 1) * 512], op0=ALU.mult, op1=ALU.mult),
                      g.psk[bank] + gT.k() + mbc[sg].k(), y_.k(hf * 512, (hf + 1) * 512))
            dstf, dkk, bound = (outflat, xxkeys, NB * TL - 1) if sg == "x" else (xcflat, xckeys, NB * TC - 1)
            S.dma(lambda e, y_=y_, ns=ns, j=j, ex_=ex_, dstf=dstf, bound=bound: e.indirect_dma_start(
                out=dstf[:, :], out_offset=bass.IndirectOffsetOnAxis(ap=idxs.ap[0:ns, j, ex_:ex_ + 1], axis=0), in_=y_.ap[0:ns, :], in_offset=None,
                bounds_check=bound_reg(g, e, bound), oob_is_err=True, compute_op=ALU.add), y_.k() + idxs.k() + dkk, dkk, q="pq")
    mem.release(m0)


def stage_da(g, l, b, do_ctx):
    nc, S, mem, I = g.nc, g.S, g.mem, g.I
    lam_init = 0.8 - 0.6 * math.exp(-0.3 * l)
    m0 = mem.mark()
    qT = mem.alloc([2, TT], BF16)
    kT = mem.alloc([2, TT], BF16)
    v1 = mem.alloc([TT // 128, 4, 65], BF16)
    gm4 = mem.alloc([4], F32)
    S.dma(lambda e: e.dma_start(out=gm4.ap, in_=I["gm4"]), [], gm4.k())
    S.dve(lambda e: e.memset(v1.ap, 1.0), [], v1.k())
    lamt = mem.alloc([4, 32], F32)
    lams = mem.alloc([8], F32)
    S.dma(lambda e: e.dma_start(out=lamt.ap, in_=I["da_lam"][l:l + 1].rearrange("o a d -> o (a d)").partition_broadcast(128)
                                .rearrange("p o (a d) -> p (o a) d", a=4)), [], lamt.k())
    S.dve(lambda e: e.tensor_tensor(out=lamt.ap[:, 0, :], in0=lamt.ap[:, 0, :], in1=lamt.ap[:, 1, :], op=ALU.mult), lamt.k(), lamt.k())
    S.dve(lambda e: e.tensor_tensor(out=lamt.ap[:, 2, :], in0=lamt.ap[:, 2, :], in1=lamt.ap[:, 3, :], op=ALU.mult), lamt.k(), lamt.k())
    S.dve(lambda e: e.tensor_reduce(out=lams.ap[:, 0:1], in_=lamt.ap[:, 0, :], axis=AX.X, op=ALU.add), lamt.k(), lams.k())
    S.dve(lambda e: e.tensor_reduce(out=lams.ap[:, 1:2], in_=lamt.ap[:, 2, :], axis=AX.X, op=ALU.add), lamt.k(), lams.k())
    S.act(lambda e: e.activation(out=lams.ap[:, 2:4], in_=lams.ap[:, 0:2], func=AF.Exp), lams.k(), lams.k())
    S.dve(lambda e: e.tensor_tensor(out=lams.ap[:, 4:5], in0=lams.ap[:, 3:4], in1=lams.ap[:, 2:3], op=ALU.subtract), lams.k(), lams.k())
    S.dve(lambda e: e.tensor_scalar(out=lams.ap[:, 4:5], in0=lams.ap[:, 4:5], scalar1=-float(lam_init), scalar2=None, op0=ALU.add), lams.k(), lams.k())
    m1 = mem.mark()
    gqk = mem.alloc([2, 32], F32)
    S.dma(lambda e: e.dma_start(out=gqk.ap[:, 0, :], in_=I["da_qnorm_g"][l:l + 1, :].partition_broadcast(128)), [], gqk.k())
    S.dma(lambda e: e.dma_start(out=gqk.ap[:, 1, :], in_=I["da_knorm_g"][l:l + 1, :].partition_broadcast(128)), [], gqk.k())
    qk = [mem.alloc([512], F32) for _ in range(2)]
    sq = [mem.alloc([512], F32) for _ in range(2)]
    ss = [mem.alloc([32], F32) for _ in range(2)]
    rp = [mem.alloc([2, 2, 8], F32) for _ in range(2)]
    ra = [mem.alloc([16, 2, 8], F32) for _ in range(2)]
    rb = [mem.alloc([16, 2, 8], F32) for _ in range(2)]
    qb = [mem.alloc([512], BF16) for _ in range(2)]
    for i in range(TT // 128):
        t0 = i * 128
        u = i % 2
        q_, s_, ss_, rp_, ra_, rb_, qb_ = qk[u], sq[u], ss[u], rp[u], ra[u], rb[u], qb[u]
        S.dma(lambda e, q_=q_, t0=t0: e.dma_start(out=q_.ap, in_=g.QK_DA[t0:t0 + 128, :]), [("QK_DA", i, 0)], q_.k())
        S.dma(lambda e, t0=t0, i=i: e.dma_start(out=v1.ap[:, i, :, 0:64], in_=g.V_DA[t0:t0 + 128, :].rearrange("p (h e) -> p h e", h=4)),
              [("V_DA", i, 0)], v1.k())
        S.dve(lambda e, q_=q_, s_=s_: e.tensor_tensor(out=s_.ap, in0=q_.ap, in1=q_.ap, op=ALU.mult), q_.k(), s_.k())
        S.dve(lambda e, s_=s_, ss_=ss_: e.tensor_reduce(out=ss_.ap[:, 0:16], in_=s_.ap.rearrange("p (g d) -> p g d", g=16), axis=AX.X, op=ALU.add), s_.k(), ss_.k())
        S.act(lambda e, ss_=ss_: e.activation(out=ss_.ap[:, 16:32], in_=ss_.ap[:, 0:16], func=AF.Sqrt, bias=EPS, scale=1.0 / 32), ss_.k(), ss_.k())
        S.dve(lambda e, ss_=ss_: e.reciprocal(out=ss_.ap[:, 16:32], in_=ss_.ap[:, 16:32]), ss_.k(), ss_.k())
        S.dve(lambda e, q_=q_, ss_=ss_, s_=s_: e.tensor_tensor(out=s_.ap.rearrange("p (g d) -> p g d", g=16), in0=q_.ap.rearrange("p (g d) -> p g d", g=16),
                                                               in1=ss_.ap[:, 16:32, None].broadcast_to([128, 16, 32]), op=ALU.mult), q_.k() + ss_.k(), s_.k())
        for hf in range(2):
            S.pool(lambda e, s_=s_, hf=hf: e.tensor_tensor(out=s_.ap[:, hf * 256:(hf + 1) * 256].rearrange("p (g d) -> p g d", g=8),
                                                          in0=s_.ap[:, hf * 256:(hf + 1) * 256].rearrange("p (g d) -> p g d", g=8),
                                                          in1=gqk.ap[:, hf:hf + 1, :].broadcast_to([128, 8, 32]), op=ALU.mult), s_.k() + gqk.k(), s_.k())
        if t0 >= TC:
            S.dma(lambda e, rp_=rp_, t0=t0: e.dma_start(out=rp_.ap.rearrange("p a b c -> p (a b c)"), in_=I["rope"][t0 - TC:t0 - TC + 128, :]), [], rp_.k())
            xv = s_.ap.rearrange("p (g a h e) -> p g a h e", g=16, a=2, h=2)
            x1, x2 = xv[:, :, :, 0, :], xv[:, :, :, 1, :]
            cosb = rp_.ap[:, 0:1, :, :].broadcast_to([128, 16, 2, 8])
            sinb = rp_.ap[:, 1:2, :, :].broadcast_to([128, 16, 2, 8])
            qv = qb_.ap.rearrange("p (g a h e) -> p g a h e", g=16, a=2, h=2)
            S.dve(lambda e, ra_=ra_, x1=x1, cosb=cosb: e.tensor_tensor(out=ra_.ap, in0=x1, in1=cosb, op=ALU.mult), s_.k() + rp_.k(), ra_.k())
            S.pool(lambda e, rb_=rb_, x2=x2, sinb=sinb: e.tensor_tensor(out=rb_.ap, in0=x2, in1=sinb, op=ALU.mult), s_.k() + rp_.k(), rb_.k())
            S.dve(lambda e, ra_=ra_, rb_=rb_, qv=qv: e.tensor_tensor(out=qv[:, :, :, 0, :], in0=ra_.ap, in1=rb_.ap, op=ALU.subtract), ra_.k() + rb_.k(), qb_.k())
            S.dve(lambda e, ra_=ra_, x1=x1, sinb=sinb: e.tensor_tensor(out=ra_.ap, in0=x1, in1=sinb, op=ALU.mult), s_.k() + rp_.k() + qb_.k(), ra_.k())
            S.pool(lambda e, rb_=rb_, x2=x2, cosb=cosb: e.tensor_tensor(out=rb_.ap, in0=x2, in1=cosb, op=ALU.mult), s_.k() + rp_.k() + qb_.k(), rb_.k())
            S.dve(lambda e, ra_=ra_, rb_=rb_, qv=qv: e.tensor_tensor(out=qv[:, :, :, 1, :], in0=ra_.ap, in1=rb_.ap, op=ALU.add), ra_.k() + rb_.k(), qb_.k())
        else:
            S.dve(lambda e, s_=s_, qb_=qb_: e.tensor_copy(out=qb_.ap, in_=s_.ap), s_.k(), qb_.k())
        bank = 2 + u
        for c in range(4):
            S.pe(lambda e, qb_=qb_, c=c, bank=bank: e.transpose(out=g.PSB[bank][:, c * 128:(c + 1) * 128], in_=qb_.ap[:, c * 128:(c + 1) * 128],
                                                                identity=g.ident_bf.ap), qb_.k() + g.ident_bf.k(), g.psk[bank])
        S.act(lambda e, bank=bank, t0=t0: e.activation(out=qT.ap[:, :, t0:t0 + 128], in_=g.PSB[bank][:, 0:256].rearrange("p (c t) -> p c t", c=2), func=AF.Copy),
              g.psk[bank], qT.k())
        S.dve(lambda e, bank=bank, t0=t0: e.tensor_copy(out=kT.ap[:, :, t0:t0 + 128], in_=g.PSB[bank][:, 256:512].rearrange("p (c t) -> p c t", c=2)),
              g.psk[bank], kT.k())
    mem.release(m1)
    qm = [mem.alloc([512], BF16) for _ in range(2)]
    PT = [mem.alloc([512], BF16) for _ in range(3)]
    o1 = mem.alloc([4, 64], F32)
    rs = mem.alloc([8], F32)
    osb = [mem.alloc([4, 256], F32) for _ in range(2)]
    slabs = []
    if do_ctx:
        slabs.append((0, TC, [0, 1]))
    for s_i in range(TL // 512):
        slabs.append((TC + s_i * 512, 512, list(range(TT // 128))))
    cnt = [0]
    SCALE = 32 ** -0.5
    for si, (q0, qw, ktiles) in enumerate(slabs):
        nqt = qw // 128
        ob = osb[si % 2]
        for h in range(4):
            for j in range(2):
                gi = h * 2 + j
                c, r = gi // 4, gi % 4
                qm_ = qm[gi % 2]
                S.dve(lambda e, qm_=qm_, c=c, r=r, q0=q0, qw=qw: e.tensor_scalar(out=qm_.ap[:, 0:qw], in0=qT.ap[:, c, q0:q0 + qw], scalar1=gm4.ap[:, r:r + 1], scalar2=None, op0=ALU.mult),
                      qT.k() + gm4.k(), qm_.k())
                nk = len(ktiles)

                def emit_pv(pt, ki, kt, h=h, nk=nk, nqt=nqt):
                    for qt in range(nqt):
                        S.pe(lambda e, pt=pt, qt=qt, kt=kt, h=h, ki=ki, nk=nk: e.matmul(g.PS[4 + qt][:, 0:65], lhsT=pt.ap[:, qt * 128:(qt + 1) * 128], rhs=v1.ap[:, kt, h, :],
                                                                                        start=(ki == 0), stop=(ki == nk - 1)),
                             pt.k() + v1.k(), g.psk[4 + qt])

                pend = None
                for ki, kt in enumerate(ktiles):
                    sb = cnt[0] % 2
                    pt = PT[cnt[0] % 3]
                    cnt[0] += 1
                    S.pe(lambda e, qm_=qm_, c=c, kt=kt, sb=sb, qw=qw: e.matmul(g.PS[sb][:, 0:qw], lhsT=kT.ap[:, c, kt * 128:(kt + 1) * 128], rhs=qm_.ap[:, 0:qw], start=True, stop=True),
                         kT.k() + qm_.k(), g.psk[sb])
                    S.act(lambda e, pt=pt, sb=sb, qw=qw: e.activation(out=pt.ap[:, 0:qw], in_=g.PS[sb][:, 0:qw], func=AF.Exp, scale=SCALE), g.psk[sb], pt.k())
                    if pend is not None:
                        emit_pv(*pend)
                    pend = (pt, ki, kt)
                emit_pv(*pend)
                for qt in range(nqt):
                    S.dve(lambda e, qt=qt, j=j: e.reciprocal(out=rs.ap[:, j * 4 + qt:j * 4 + qt + 1], in_=g.PS[4 + qt][:, 64:65]), g.psk[4 + qt], rs.k())
                    if j == 0:
                        S.dve(lambda e, qt=qt: e.tensor_scalar(out=o1.ap[:, qt, :], in0=g.PS[4 + qt][:, 0:64], scalar1=rs.ap[:, qt:qt + 1], scalar2=None, op0=ALU.mult),
                              g.psk[4 + qt] + rs.k(), o1.k())
                    else:
                        S.dve(lambda e, qt=qt: e.tensor_tensor(out=rs.ap[:, 4 + qt:5 + qt], in0=rs.ap[:, 4 + qt:5 + qt], in1=lams.ap[:, 4:5], op=ALU.mult), rs.k() + lams.k(), rs.k())
                        S.dve(lambda e, qt=qt, h=h, ob=ob: e.scalar_tensor_tensor(out=ob.ap[:, qt, h * 64:(h + 1) * 64], in0=g.PS[4 + qt][:, 0:64], scalar=rs.ap[:, 4 + qt:5 + qt],
                                                                                  in1=o1.ap[:, qt, :], op0=ALU.mult, op1=ALU.add),
                              g.psk[4 + qt] + rs.k() + o1.k(), ob.k())
        for qt in range(nqt):
            tt = q0 + qt * 128
            S.dma(lambda e, ob=ob, qt=qt, tt=tt: e.dma_start(out=g.O_DA[0, tt:tt + 128, :], in_=ob.ap[:, qt, :]), ob.k(), [("O_DA", 0, v) for v in range(tt // 32, tt // 32 + 4)])
    mem.release(m0)


def hy_params(L):
    n = 2 * L
    N1 = n // 128
    return n, N1, N1 // 2


def hy_stage1(g, L, X, N, in_keys, W1):
    S, mem = g.S, g.mem
    n, N1, K1 = hy_params(L)
    m0 = mem.mark()
    G = 16
    pcs = [mem.alloc([G, N], BF16, parts=K1) for _ in range(2)]
    stg = [mem.alloc([G, N], BF16) for _ in range(2)]
    Xv = X.rearrange("(a b) n -> a b n", b=128)
    nmm = G * N // 512
    for gi in range(128 // G):
        pc, st = pcs[gi % 2], stg[gi % 2]
        S.dma(lambda e, pc=pc, gi=gi: e.dma_start(out=pc.ap, in_=Xv[:, gi * G:(gi + 1) * G, :]), in_keys, pc.k())
        pcf = pc.ap.rearrange("p a n -> p (a n)")
        stf = st.ap.rearrange("p a n -> p (a n)")
        for m in range(nmm):
            bank = m % 2
            S.pe(lambda e, pcf=pcf, m=m, bank=bank: e.matmul(g.PS[bank][0:2 * N1, :], lhsT=W1.ap, rhs=pcf[:, m * 512:(m + 1) * 512], start=True, stop=True),
                 pc.k() + W1.k(), g.psk[bank])
            if m % 2 == 0:
                S.act(lambda e, stf=stf, m=m, bank=bank: e.activation(out=stf[0:2 * N1, m * 512:(m + 1) * 512], in_=g.PS[bank][0:2 * N1, :], func=AF.Copy), g.psk[bank], st.k())
            else:
                S.dve(lambda e, stf=stf, m=m, bank=bank: e.tensor_copy(out=stf[0:2 * N1, m * 512:(m + 1) * 512], in_=g.PS[bank][0:2 * N1, :]), g.psk[bank], st.k())
        S.dma(lambda e, st=st, gi=gi: e.dma_start(out=g.AD[0:2 * N1, gi * G:(gi + 1) * G, 0:N], in_=st.ap[0:2 * N1]), st.k(), [("AD", gi)])
    mem.release(m0)


def hy_load_consts(g, L):
    S, mem, I = g.S, g.mem, g.I
    n, N1, K1 = hy_params(L)
    tg = "%d" % L
    W1 = mem.alloc([2 * N1], BF16, parts=K1)
    E = mem.alloc([K1], BF16, parts=2 * N1)
    S.dma(lambda e: e.dma_start(out=W1.ap, in_=I["hy_W1_" + tg]), [], W1.k())
    S.dma(lambda e: e.dma_start(out=E.ap, in_=I["hy_E_" + tg]), [], E.k())
    return W1, E


def stage_hy_filter(g, l, L, upto=99):
    nc, S, mem, I = g.nc, g.S, g.mem, g.I
    n, N1, K1 = hy_params(L)
    tg = "%d" % L
    m0 = mem.mark()
    W1, E = hy_load_consts(g, L)
    m1 = mem.mark()
    NTL = L // 128
    w3 = mem.alloc([512], F32, parts=64)
    S.dma(lambda e: e.dma_start(out=w3.ap, in_=I["hy_w3"][l]), [], w3.k())
    hB = mem.alloc([L], F32, parts=64)
    hpi = mem.alloc([1], F32)
    S.dve(lambda e: e.memset(hpi.ap, math.pi / 2), [], hpi.k())
    m_s = mem.mark()
    fe = mem.alloc([L], F32, parts=33)
    S.dma(lambda e: e.dma_start(out=fe.ap, in_=I["hy_featsT" + tg]), [], fe.k())
    w1 = mem.alloc([64], F32, parts=33)
    w2 = mem.alloc([64], F32, parts=64)
    S.dma(lambda e: e.dma_start(out=w1.ap, in_=I["hy_w1"][l]), [], w1.k())
    S.dma(lambda e: e.dma_start(out=w2.ap, in_=I["hy_w2"][l]), [], w2.k())
    pc = mem.alloc([12], F32, parts=64)
    S.dma(lambda e: e.dma_start(out=pc.ap[:, 0:2], in_=I["hy_freq"][l].rearrange("a p -> p a"), allow_slow_non_contiguous=True), [], pc.k())
    S.dma(lambda e: e.dma_start(out=pc.ap[:, 2:3], in_=I["hy_b1"][l:l + 1, :].rearrange("a p -> p a"), allow_slow_non_contiguous=True), [], pc.k())
    S.dma(lambda e: e.dma_start(out=pc.ap[:, 3:4], in_=I["hy_b2"][l:l + 1, :].rearrange("a p -> p a"), allow_slow_non_contiguous=True), [], pc.k())
    S.dve(lambda e: e.tensor_scalar(out=pc.ap[:, 4:6], in0=pc.ap[:, 0:2], scalar1=1.0 / (2 * math.pi), scalar2=None, op0=ALU.mult), pc.k(), pc.k())
    S.dve(lambda e: e.tensor_tensor(out=pc.ap[:, 8:10], in0=pc.ap[:, 0:2], in1=pc.ap[:, 2:4], op=ALU.mult), pc.k(), pc.k())
    S.dve(lambda e: e.tensor_scalar(out=pc.ap[:, 6:8], in0=pc.ap[:, 8:10], scalar1=1.0 / (2 * math.pi), scalar2=64.5, op0=ALU.mult, op1=ALU.add), pc.k(), pc.k())
    hA = mem.alloc([L], F32, parts=64)
    ki = [mem.alloc([512], I32, parts=64) for _ in range(2)]
    tf = [mem.alloc([512], F32, parts=64) for _ in range(2)]
    xf = [mem.alloc([512], F32, parts=64) for _ in range(2)]
    sn = [mem.alloc([512], F32, parts=64) for _ in range(2)]
    cs = [mem.alloc([512], F32, parts=64) for _ in range(2)]

    def sine_layer(wt, kparts, src, dst, li):
        for si in range((L + 511) // 512):
            c0 = si * 512
            cw = min(512, L - c0)
            u = si % 2
            bank = u
            S.pe(lambda e, c0=c0, cw=cw, bank=bank: e.matmul(g.PS[bank][0:64, 0:cw], lhsT=wt.ap, rhs=src.ap[0:kparts, c0:c0 + cw], start=True, stop=True),
                 wt.k() + src.k(), g.psk[bank])
            k_, t_, x_, s_, c_ = ki[u], tf[u], xf[u], sn[u], cs[u]
            S.dve(lambda e, k_=k_, bank=bank, cw=cw: e.tensor_scalar(out=k_.ap[:, 0:cw], in0=g.PS[bank][0:64, 0:cw], scalar1=pc.ap[:, 4 + li:5 + li], scalar2=pc.ap[:, 6 + li:7 + li],
                                                                     op0=ALU.mult, op1=ALU.add), g.psk[bank] + pc.k(), k_.k())
            S.dve(lambda e, k_=k_, t_=t_, cw=cw: e.tensor_scalar(out=t_.ap[:, 0:cw], in0=k_.ap[:, 0:cw], scalar1=-64.0, scalar2=-2 * math.pi, op0=ALU.add, op1=ALU.mult), k_.k(), t_.k())
            S.dve(lambda e, x_=x_, bank=bank, cw=cw: e.tensor_scalar(out=x_.ap[:, 0:cw], in0=g.PS[bank][0:64, 0:cw], scalar1=pc.ap[:, li:li + 1], scalar2=pc.ap[:, 8 + li:9 + li],
                                                                     op0=ALU.mult, op1=ALU.add), g.psk[bank] + pc.k(), x_.k())
            S.pool(lambda e, x_=x_, t_=t_, cw=cw: e.tensor_tensor(out=x_.ap[:, 0:cw], in0=x_.ap[:, 0:cw], in1=t_.ap[:, 0:cw], op=ALU.add), x_.k() + t_.k(), x_.k())
            S.act(lambda e, x_=x_, s_=s_, cw=cw: e.activation(out=s_.ap[:, 0:cw], in_=x_.ap[:, 0:cw], func=AF.Sin, scale=0.5), x_.k(), s_.k())
            S.act(lambda e, x_=x_, c_=c_, cw=cw: e.activation(out=c_.ap[:, 0:cw], in_=x_.ap[:, 0:cw], func=AF.Sin, scale=0.5, bias=hpi.ap[0:64, 0:1]), x_.k() + hpi.k(), c_.k())
            S.dve(lambda e, s_=s_, c_=c_, c0=c0, cw=cw: e.scalar_tensor_tensor(out=dst.ap[:, c0:c0 + cw], in0=s_.ap[:, 0:cw], scalar=2.0, in1=c_.ap[:, 0:cw], op0=ALU.mult, op1=ALU.mult),
                  s_.k() + c_.k(), dst.k(c0, c0 + cw))

    sine_layer(w1, 33, fe, hA, 0)
    if upto == 1:
        S.dma(lambda e: e.dma_start(out=g.ZF[0:64, 0:256], in_=hA.ap[:, 0:256]), hA.k(), [("ZF", 0)])
        mem.release(m0)
        return
    sine_layer(w2, 64, hA, hB, 1)
    mem.release(m_s)
    if upto == 2:
        S.dma(lambda e: e.dma_start(out=g.ZF[0:64, 0:256], in_=hB.ap[:, 0:256]), hB.k(), [("ZF", 0)])
        mem.release(m0)
        return
    decb = mem.alloc([512], F32)
    S.dma(lambda e: e.dma_start(out=decb.ap, in_=I["hy_decay"][l:l + 1, :].partition_broadcast(128)), [], decb.k())
    S.act(lambda e: e.activation(out=decb.ap, in_=decb.ap, func=AF.Abs), decb.k(), decb.k())
    nd = mem.alloc([NTL], F32)
    S.dma(lambda e: e.dma_start(out=nd.ap, in_=I["hy_negdist" + tg]), [], nd.k())
    hw = mem.alloc([NTL, 512], F32)
    win = [mem.alloc([512], F32) for _ in range(2)]
    ab = [mem.alloc([512], F32) for _ in range(2)]
    for i in range(NTL):
        u = i % 2
        bank = 2 + u
        S.pe(lambda e, i=i, bank=bank: e.matmul(g.PS[bank], lhsT=hB.ap[:, i * 128:(i + 1) * 128], rhs=w3.ap, start=True, stop=True), hB.k() + w3.k(), g.psk[bank])
        S.act(lambda e, i=i, u=u: e.activation(out=win[u].ap, in_=decb.ap, func=AF.Exp, scale=nd.ap[:, i:i + 1]), decb.k() + nd.k(), win[u].k())
        S.dve(lambda e, i=i, u=u, bank=bank: e.scalar_tensor_tensor(out=hw.ap[:, i, :], in0=win[u].ap, scalar=0.05, in1=g.PS[bank], op0=ALU.add, op1=ALU.mult),
              win[u].k() + g.psk[bank], hw.k(i * 512, (i + 1) * 512))
        S.act(lambda e, i=i, u=u: e.activation(out=ab[u].ap, in_=hw.ap[:, i, :], func=AF.Abs), hw.k(i * 512, (i + 1) * 512), ab[u].k())
        S.pe(lambda e, i=i, u=u: e.matmul(g.PS[7], lhsT=g.ones_f.ap, rhs=ab[u].ap, start=(i == 0), stop=(i == NTL - 1)), g.ones_f.k() + ab[u].k(), g.psk[7])
    rcs = mem.alloc([512], F32)
    S.dve(lambda e: e.reciprocal(out=rcs.ap, in_=g.PS[7]), g.psk[7], rcs.k())
    hn = [mem.alloc([512], BF16) for _ in range(2)]
    for i in range(NTL):
        u = i % 2
        S.dve(lambda e, i=i, u=u: e.tensor_tensor(out=hn[u].ap, in0=hw.ap[:, i, :], in1=rcs.ap, op=ALU.mult), hw.k(i * 512, (i + 1) * 512) + rcs.k(), hn[u].k())
        S.dma(lambda e, i=i, u=u: e.dma_start(out=g.HFILT[L][i * 128:(i + 1) * 128, :], in_=hn[u].ap), hn[u].k(), [("HFILT", L, i)])
    mem.release(m1)
    if upto == 3:
        mem.release(m0)
        return
    hy_stage1(g, L, g.HFILT[L], 512, [("HFILT", L, i) for i in range(NTL)], W1)
    if upto == 4:
        g.S.dma(lambda e: e.dma_start(out=g.ZF[0:128, 0:256], in_=g.AD[5, :, 0:256].bitcast(BF16)) if False else e.dma_start(out=g.ZD[0], in_=g.AD[5, :, 0:256]), [("AD", gi) for gi in range(8)], [("ZD", 0)])
        mem.release(m0)
        return
    adk = [("AD", gi) for gi in range(8)]
    are = [mem.alloc([512], BF16) for _ in range(2)]
    aim = [mem.alloc([512], BF16) for _ in range(2)]
    pk = [mem.alloc([4, 128], BF16) for _ in range(2)]
    hs = [mem.alloc([2, 512], F32) for _ in range(2)]
    for f1 in range(N1 if upto > 10 else min(N1, 2 ** upto)):
        u = f1 % 2
        S.dma(lambda e, f1=f1, u=u: e.dma_start(out=are[u].ap, in_=g.AD[f1, :, 0:512]), adk, are[u].k())
        S.dma(lambda e, f1=f1, u=u: e.dma_start(out=aim[u].ap, in_=g.AD[N1 + f1, :, 0:512]), adk, aim[u].k())
        S.dma(lambda e, f1=f1, u=u: e.dma_start(out=pk[u].ap, in_=I["hy_PKH_" + tg][f1]), [], pk[u].k())
        for hb in range(2):
            bank = 4 + hb
            S.pe(lambda e, u=u, hb=hb, bank=bank: e.matmul(g.PS[bank], lhsT=pk[u].ap[:, 2 * hb, :], rhs=are[u].ap, start=True, stop=False), pk[u].k() + are[u].k(), g.psk[bank])
            S.pe(lambda e, u=u, hb=hb, bank=bank: e.matmul(g.PS[bank], lhsT=pk[u].ap[:, 2 * hb + 1, :], rhs=aim[u].ap, start=False, stop=True), pk[u].k() + aim[u].k(), g.psk[bank])
            if hb == 0:
                S.act(lambda e, u=u, hb=hb, bank=bank: e.activation(out=hs[u].ap[:, hb, :], in_=g.PS[bank], func=AF.Copy), g.psk[bank], hs[u].k())
            else:
                S.dve(lambda e, u=u, hb=hb, bank=bank: e.tensor_copy(out=hs[u].ap[:, hb, :], in_=g.PS[bank]), g.psk[bank], hs[u].k())
        S.dma(lambda e, f1=f1, u=u: e.dma_start(out=g.HAB[L][:, f1].rearrange("a p n -> p a n"), in_=hs[u].ap), hs[u].k(), [("HAB", L, f1)])
    mem.release(m0)


def stage_hy(g, l, b, do_ctx):
    nc, S, mem, I = g.nc, g.S, g.mem, g.I
    m0 = mem.mark()
    segs = [(TC, TL)]
    if do_ctx:
        segs.append((0, TC))
    m1 = mem.mark()
    wc = mem.alloc([3, 768], F32)
    S.dma(lambda e: e.dma_start(out=wc.ap, in_=I["hy_conv_w"][l:l + 1].rearrange("o a c -> o (a c)").partition_broadcast(128).rearrange("p o (a c) -> p (o a) c", a=3)), [], wc.k())
    cur = [mem.alloc([768], F32) for _ in range(2)]
    prv = [mem.alloc([768], F32) for _ in range(2)]
    nxt = [mem.alloc([768], F32) for _ in range(2)]
    zb = [mem.alloc([256], BF16) for _ in range(2)]
    cnt = 0
    for (s0, L) in segs:
        for i in range(L // 128):
            t0 = s0 + i * 128
            u = cnt % 2
            cnt += 1
            c_, p_, n_, z_ = cur[u], prv[u], nxt[u], zb[u]
            ti = t0 // 128
            S.dma(lambda e, c_=c_, t0=t0: e.dma_start(out=c_.ap, in_=g.HY[t0:t0 + 128, :]), [("HY", ti, 0), ("HY", ti, 512)], c_.k())
            if i == 0:
                S.pool(lambda e, p_=p_: e.memset(p_.ap, 0.0), [], p_.k())
                S.dma(lambda e, p_=p_, t0=t0: e.dma_start(out=p_.ap[1:128, :], in_=g.HY[t0:t0 + 127, :]), [("HY", ti, 0), ("HY", ti, 512)], p_.k())
            else:
                S.dma(lambda e, p_=p_, t0=t0: e.dma_start(out=p_.ap, in_=g.HY[t0 - 1:t0 + 127, :]),
                      [("HY", ti, 0), ("HY", ti, 512), ("HY", ti - 1, 0), ("HY", ti - 1, 512)], p_.k())
            if i == L // 128 - 1:
                S.pool(lambda e, n_=n_: e.memset(n_.ap, 0.0), [], n_.k())
                S.dma(lambda e, n_=n_, t0=t0: e.dma_start(out=n_.ap[0:127, :], in_=g.HY[t0 + 1:t0 + 128, :]), [("HY", ti, 0), ("HY", ti, 512)], n_.k())
            else:
                S.dma(lambda e, n_=n_, t0=t0: e.dma_start(out=n_.ap, in_=g.HY[t0 + 1:t0 + 129, :]),
                      [("HY", ti, 0), ("HY", ti, 512), ("HY", ti + 1, 0), ("HY", ti + 1, 512)], n_.k())
            S.dve(lambda e, c_=c_: e.tensor_tensor(out=c_.ap, in0=c_.ap, in1=wc.ap[:, 1, :], op=ALU.mult), c_.k() + wc.k(), c_.k())
            S.pool(lambda e, p_=p_: e.tensor_tensor(out=p_.ap, in0=p_.ap, in1=wc.ap[:, 0, :], op=ALU.mult), p_.k() + wc.k(), p_.k())
            S.pool(lambda e, n_=n_: e.tensor_tensor(out=n_.ap, in0=n_.ap, in1=wc.ap[:, 2, :], op=ALU.mult), n_.k() + wc.k(), n_.k())
            S.dve(lambda e, c_=c_, p_=p_: e.tensor_tensor(out=c_.ap, in0=c_.ap, in1=p_.ap, op=ALU.add), c_.k() + p_.k(), c_.k())
            S.dve(lambda e, c_=c_, n_=n_: e.tensor_tensor(out=c_.ap, in0=c_.ap, in1=n_.ap, op=ALU.add), c_.k() + n_.k(), c_.k())
            S.act(lambda e, c_=c_, z_=z_: e.activation(out=z_.ap, in_=c_.ap[:, 0:256], func=AF.Copy), c_.k(), z_.k())
            S.dma(lambda e, c_=c_, t0=t0: e.dma_start(out=g.UCONV[t0:t0 + 128, :], in_=c_.ap), c_.k(), [("UCONV", ti)])
            S.dma(lambda e, c_=c_, t0=t0: e.dma_start(out=g.ZF[t0:t0 + 128, :], in_=c_.ap[:, 0:256]), c_.k(), [("ZF", ti)])
            S.dma(lambda e, z_=z_, t0=t0: e.dma_start(out=g.ZB[0, t0:t0 + 128, :], in_=z_.ap), z_.k(), [("ZB", 0, ti)])
    mem.release(m1)
    def do_seg(s0, L):
        n, N1, K1 = hy_params(L)
        tg = "%d" % L
        m2 = mem.mark()
        W1, E = hy_load_consts(g, L)
        tiles = list(range(s0 // 128, (s0 + L) // 128))
        def do_order(o):
            zin = g.ZB[o % 2, s0:s0 + L, :]
            hy_stage1(g, L, zin, 256, [("ZB", o % 2, ti) for ti in tiles], W1)
            m3 = mem.mark()
            adk = [("AD", gi) for gi in range(8)]
            are = [mem.alloc([256], BF16) for _ in range(4)]
            aim = [mem.alloc([256], BF16) for _ in range(4)]
            pk = [mem.alloc([6, 128], BF16) for _ in range(4)]
            hab = [mem.alloc([2, 256], F32) for _ in range(4)]
            ta = [mem.alloc([256], F32) for _ in range(4)]
            tb = [mem.alloc([256], F32) for _ in range(4)]
            yb = [mem.alloc([256], BF16) for _ in range(4)]
            zs = [mem.alloc([2, 256], BF16) for _ in range(4)]
            for f1 in range(N1):
                u = f1 % 4
                hv = u % 2
                S.dma(lambda e, f1=f1, u=u: e.dma_start(out=are[u].ap, in_=g.AD[f1, :, 0:256]), adk, are[u].k())
                S.dma(lambda e, f1=f1, u=u: e.dma_start(out=aim[u].ap, in_=g.AD[N1 + f1, :, 0:256]), adk, aim[u].k())
                S.dma(lambda e, f1=f1, u=u: e.dma_start(out=pk[u].ap, in_=I["hy_PK_" + tg][f1]), [], pk[u].k())
                S.dma(lambda e, f1=f1, u=u, o=o: e.dma_start(out=hab[u].ap, in_=g.HAB[L][:, f1, :, o * 256:(o + 1) * 256].rearrange("a p n -> p a n")),
                      [("HAB", L, f1)], hab[u].k())
                for hb in range(2):
                    bank = 2 + hb
                    S.pe(lambda e, u=u, hb=hb, bank=bank, hv=hv: e.matmul(g.PS[bank][:, hv * 256:(hv + 1) * 256], lhsT=pk[u].ap[:, 2 * hb, :], rhs=are[u].ap, start=True, stop=False), pk[u].k() + are[u].k(), [("ps", bank, hv)])
                    S.pe(lambda e, u=u, hb=hb, bank=bank, hv=hv: e.matmul(g.PS[bank][:, hv * 256:(hv + 1) * 256], lhsT=pk[u].ap[:, 2 * hb + 1, :], rhs=aim[u].ap, start=False, stop=True), pk[u].k() + aim[u].k(), [("ps", bank, hv)])
                S.dve(lambda e, u=u, hv=hv: e.tensor_tensor(out=ta[u].ap, in0=g.PS[2][:, hv * 256:(hv + 1) * 256], in1=hab[u].ap[:, 0, :], op=ALU.mult), [("ps", 2, hv)] + hab[u].k(), ta[u].k())
                S.dve(lambda e, u=u, hv=hv: e.tensor_tensor(out=tb[u].ap, in0=g.PS[3][:, hv * 256:(hv + 1) * 256], in1=hab[u].ap[:, 1, :], op=ALU.mult), [("ps", 3, hv)] + hab[u].k(), tb[u].k())
                S.pool(lambda e, u=u: e.tensor_tensor(out=yb[u].ap, in0=ta[u].ap, in1=tb[u].ap, op=ALU.add), ta[u].k() + tb[u].k(), yb[u].k())
                for hb in range(2):
                    bank = 4 + hb
                    S.pe(lambda e, u=u, hb=hb, bank=bank, hv=hv: e.matmul(g.PS[bank][:, hv * 256:(hv + 1) * 256], lhsT=pk[u].ap[:, 4 + hb, :], rhs=yb[u].ap, start=True, stop=True), pk[u].k() + yb[u].k(), [("ps", bank, hv)])
                    if hb == 0:
                        S.act(lambda e, u=u, hb=hb, bank=bank, hv=hv: e.activation(out=zs[u].ap[:, hb, :], in_=g.PS[bank][:, hv * 256:(hv + 1) * 256], func=AF.Copy), [("ps", bank, hv)], zs[u].k())
                    else:
                        S.dve(lambda e, u=u, hb=hb, bank=bank, hv=hv: e.tensor_copy(out=zs[u].ap[:, hb, :], in_=g.PS[bank][:, hv * 256:(hv + 1) * 256]), [("ps", bank, hv)], zs[u].k())
                S.dma(lambda e, f1=f1, u=u: e.dma_start(out=g.ZD[f1, :, :], in_=zs[u].ap[:, 0, :]), zs[u].k(), [("ZD", f1)])
                S.dma(lambda e, f1=f1, u=u: e.dma_start(out=g.ZD[N1 + f1, :, :], in_=zs[u].ap[:, 1, :]), zs[u].k(), [("ZD", N1 + f1)])
            mem.release(m3)
            m4 = mem.mark()
            G = 16
            zin_ = [mem.alloc([G, 256], BF16, parts=2 * N1) for _ in range(2)]
            gt = [mem.alloc([G, 256], F32, parts=K1) for _ in range(2)]
            zp = [mem.alloc([G, 256], F32, parts=K1) for _ in range(2)]
            zn = [mem.alloc([G, 256], F32, parts=K1) for _ in range(2)]
            znb = [mem.alloc([G, 256], BF16, parts=K1) for _ in range(2)]
            tq = [mem.alloc([512], F32, parts=K1) for _ in range(2)]
            bb = mem.alloc([256], F32, parts=K1)
            S.dma(lambda e, o=o: e.dma_start(out=bb.ap, in_=I["hy_bias"][l, o:o + 1, :].partition_broadcast(K1)), [], bb.k())
            zdk = [("ZD", r_) for r_ in range(2 * N1)]
            Uv = g.UCONV[s0:s0 + L, :].rearrange("(a b) n -> a b n", b=128)
            ZFv = g.ZF[s0:s0 + L, :].rearrange("(a b) n -> a b n", b=128)
            ZBv = g.ZB[(o + 1) % 2, s0:s0 + L, :].rearrange("(a b) n -> a b n", b=128)
            ukeys = [("UCONV", ti) for ti in tiles]
            zfk = [("ZF", ti) for ti in tiles]
            zbk = [("ZB", (o + 1) % 2, ti) for ti in tiles]
            for gi in range(128 // G):
                u = gi % 2
                S.dma(lambda e, u=u, gi=gi: e.dma_start(out=zin_[u].ap, in_=g.ZD[0:2 * N1, gi * G:(gi + 1) * G, :]), zdk, zin_[u].k())
                S.dma(lambda e, u=u, gi=gi, o=o: e.dma_start(out=gt[u].ap, in_=Uv[:, gi * G:(gi + 1) * G, 256 * (o + 1):256 * (o + 2)]), ukeys, gt[u].k())
                S.dma(lambda e, u=u, gi=gi: e.dma_start(out=zp[u].ap, in_=ZFv[:, gi * G:(gi + 1) * G, :]), zfk, zp[u].k())
                zf_ = zin_[u].ap.rearrange("p a n -> p (a n)")
                for pr in range(G // 2):
                    bank = 6 + pr % 2
                    tq_ = tq[pr % 2]
                    S.pe(lambda e, zf_=zf_, pr=pr, bank=bank: e.matmul(g.PS[bank][0:K1, :], lhsT=E.ap, rhs=zf_[:, pr * 512:(pr + 1) * 512], start=True, stop=True),
                         E.k() + zin_[u].k(), g.psk[bank])
                    zpv = zp[u].ap[:, 2 * pr:2 * pr + 2, :]
                    S.pool(lambda e, tq_=tq_, zpv=zpv: e.tensor_tensor(out=tq_.ap.rearrange("p (a n) -> p a n", a=2), in0=zpv, in1=bb.ap[:, None, :].broadcast_to([K1, 2, 256]), op=ALU.mult),
                           zp[u].k() + bb.k(), tq_.k())
                    S.dve(lambda e, tq_=tq_, bank=bank: e.tensor_tensor(out=tq_.ap, in0=tq_.ap, in1=g.PS[bank][0:K1, :], op=ALU.add), tq_.k() + g.psk[bank], tq_.k())
                    S.dve(lambda e, tq_=tq_, u=u, pr=pr: e.tensor_tensor(out=zn[u].ap[:, 2 * pr:2 * pr + 2, :], in0=tq_.ap.rearrange("p (a n) -> p a n", a=2), in1=gt[u].ap[:, 2 * pr:2 * pr + 2, :], op=ALU.mult),
                          tq_.k() + gt[u].k(), zn[u].k())
                S.act(lambda e, u=u: e.activation(out=znb[u].ap, in_=zn[u].ap, func=AF.Copy), zn[u].k(), znb[u].k())
                S.dma(lambda e, u=u, gi=gi: e.dma_start(out=ZFv[:, gi * G:(gi + 1) * G, :], in_=zn[u].ap), zn[u].k(), zfk)
                S.dma(lambda e, u=u, gi=gi: e.dma_start(out=ZBv[:, gi * G:(gi + 1) * G, :], in_=znb[u].ap), znb[u].k(), zbk)
            mem.release(m4)

        for o in range(2):
            do_order(o)
        mem.release(m2)

    for (s0, L) in segs:
        do_seg(s0, L)
    zt = [mem.alloc([256], BF16) for _ in range(2)]
    stg = [mem.alloc([2, 128], BF16) for _ in range(2)]
    cnt = 0
    for (s0, L) in segs:
        for i in range(L // 128):
            t0 = s0 + i * 128
            ti = t0 // 128
            u = cnt % 2
            cnt += 1
            S.dma(lambda e, u=u, t0=t0: e.dma_start(out=zt[u].ap, in_=g.ZB[0, t0:t0 + 128, :]), [("ZB", 0, ti)], zt[u].k())
            bank = 2 + u
            for c in range(2):
                S.pe(lambda e, u=u, c=c, bank=bank: e.transpose(out=g.PSB[bank][:, c * 128:(c + 1) * 128], in_=zt[u].ap[:, c * 128:(c + 1) * 128], identity=g.ident_bf.ap),
                     zt[u].k() + g.ident_bf.k(), g.psk[bank])
            S.act(lambda e, u=u, bank=bank: e.activation(out=stg[u].ap, in_=g.PSB[bank][:, 0:256].rearrange("p (c t) -> p c t", c=2), func=AF.Copy), g.psk[bank], stg[u].k())
            S.dma(lambda e, u=u, t0=t0: e.dma_start(out=g.BRT[1].rearrange("(c p) t -> p c t", p=128)[:, :, t0:t0 + 128], in_=stg[u].ap), stg[u].k(), [("BRT", 1, ti)])
    mem.release(m0)
```
